# Optimizing a Trainium2 kernel written in Bass

```python
import math
import jax, jax.numpy as jnp
from jax import lax
import numpy as np

D_MODEL = 1024
BATCH = 8
SEQ = 4096
DEPTH = 4

GRID_W = 64
CTX_LEN = 256
N_MIXERS = 4
DEEPNORM_ALPHA = (2 * DEPTH) ** 0.25
DEEPNORM_BETA = (8 * DEPTH) ** -0.25
LN_EPS = 1e-5
RMS_EPS = 1e-6
NEG_INF = -1e30
ROPE_BASE = 10000.0
Q_BLOCK = 128
SC_WIDTH = 3
SWA_HEADS = 16
SWA_KV_HEADS = 4
SWA_GROUP = SWA_HEADS // SWA_KV_HEADS
SWA_HEAD_DIM = D_MODEL // SWA_HEADS
SWA_SCALE = SWA_HEAD_DIM ** -0.5
SWA_WINDOW = 128
SWA_BLOCK = 128
DIFF_HEADS = 8
DIFF_HEAD_DIM = D_MODEL // (2 * DIFF_HEADS)
DIFF_SCALE = DIFF_HEAD_DIM ** -0.5
DN_QK_HEADS = 8
DN_V_HEADS = 16
DN_HEAD_DIM = 128
DN_CONV = 5
DN_CHUNK = 64
N_EXPERTS = 32
TOP_K = 4
D_FF = D_MODEL
SWIGLU_LIMIT = 7.0
SWIGLU_ALPHA = 1.702
EXPERT_BLOCK = 128

kernel_name = 'hybrid_flow_trunk_conv_swa_diff_deltanet_moe'


def layer_norm(x, g, b):
    xf = x.astype(jnp.float32)
    mu = jnp.mean(xf, axis=-1, keepdims=True)
    var = jnp.mean(jnp.square(xf - mu), axis=-1, keepdims=True)
    y = (xf - mu) * lax.rsqrt(var + LN_EPS) * g.astype(jnp.float32) + b.astype(jnp.float32)
    return y.astype(x.dtype)


def rms_norm(x, g):
    xf = x.astype(jnp.float32)
    y = xf * lax.rsqrt(jnp.mean(jnp.square(xf), axis=-1, keepdims=True) + RMS_EPS)
    return (y * g.astype(jnp.float32)).astype(x.dtype)


def l2_normalize(x):
    xf = x.astype(jnp.float32)
    return (xf * lax.rsqrt(jnp.sum(jnp.square(xf), axis=-1, keepdims=True) + RMS_EPS)).astype(x.dtype)


def modulate(x, shift, scale):
    return x * (1.0 + scale) + shift


def centred_dwconv(x, w):
    pad = (w.shape[0] - 1) // 2
    return lax.conv_general_dilated(x, w[:, None, :].astype(x.dtype), window_strides=(1,),
                                    padding=[(pad, pad)], dimension_numbers=('NWC', 'WIO', 'NWC'),
                                    feature_group_count=x.shape[-1])


def axial_rope_tables(rows, head_dim):
    row = jnp.repeat(jnp.arange(rows, dtype=jnp.float32), GRID_W)
    col = jnp.tile(jnp.arange(GRID_W, dtype=jnp.float32), rows)
    half = head_dim // 2
    inv = ROPE_BASE ** (-jnp.arange(0, half, 2, dtype=jnp.float32) / half)
    ar = row[:, None] * inv
    ac = col[:, None] * inv
    ang = jnp.concatenate([ar, ar, ac, ac], axis=-1)
    return jnp.cos(ang), jnp.sin(ang)


def apply_rope(x, cos, sin):
    x1, x2, x3, x4 = jnp.split(x, 4, axis=-1)
    rot = jnp.concatenate([-x2, x1, -x4, x3], axis=-1)
    return (x * cos[:, None, :] + rot * sin[:, None, :]).astype(x.dtype)


def short_conv_mixer(hl, hc, w_in, w_conv, w_out, ctx_out):
    def mix(h):
        b_gate, c_gate, u = jnp.split(h @ w_in, 3, axis=-1)
        return (b_gate * centred_dwconv(c_gate * u, w_conv)) @ w_out
    return mix(hl), (mix(hc) if ctx_out else None)


def sink_attend(q, k, v, sink_logit, mask):
    s = jnp.einsum('bqkgd,bskd->bkgqs', q, k).astype(jnp.float32) * SWA_SCALE
    if mask is not None:
        s = jnp.where(mask, s, NEG_INF)
    sink_col = jnp.broadcast_to(sink_logit, s.shape[:-1] + (1,))
    p = jax.nn.softmax(jnp.concatenate([s, sink_col], axis=-1), axis=-1)[..., :-1]
    return jnp.einsum('bkgqs,bskd->bqkgd', p.astype(v.dtype), v)


def swa_mixer(hl, hc, w_qkv, b_qkv, sink, w_o, b_o, cos, sin, ctx_out):
    bsz, n_lat, _ = hl.shape
    nq = SWA_HEADS * SWA_HEAD_DIM
    nkv = SWA_KV_HEADS * SWA_HEAD_DIM

    def proj(h):
        qkv = h @ w_qkv + b_qkv
        sh = h.shape[:2]
        q = qkv[..., :nq].reshape(sh + (SWA_HEADS, SWA_HEAD_DIM))
        k = qkv[..., nq:nq + nkv].reshape(sh + (SWA_KV_HEADS, SWA_HEAD_DIM))
        v = qkv[..., nq + nkv:].reshape(sh + (SWA_KV_HEADS, SWA_HEAD_DIM))
        return q, k, v

    ql, kl, vl = proj(hl)
    qc, kc, vc = proj(hc)
    ql = apply_rope(ql, cos, sin)
    kl = apply_rope(kl, cos, sin)
    group = lambda q: q.reshape(q.shape[:2] + (SWA_KV_HEADS, SWA_GROUP, SWA_HEAD_DIM))
    ql, qc = group(ql), group(qc)
    sink_logit = sink.astype(jnp.float32).reshape(SWA_KV_HEADS, SWA_GROUP, 1, 1)

    nb = n_lat // SWA_BLOCK

    def windows(a):
        ap = jnp.pad(a, ((0, 0), (SWA_BLOCK, SWA_BLOCK), (0, 0), (0, 0)))
        ap = ap.reshape(bsz, nb + 2, SWA_BLOCK, SWA_KV_HEADS, SWA_HEAD_DIM)
        w = jnp.concatenate([ap[:, :-2], ap[:, 1:-1], ap[:, 2:]], axis=2)
        return jnp.moveaxis(w, 1, 0)

    kw, vw = windows(kl), windows(vl)
    qb = jnp.moveaxis(ql.reshape(bsz, nb, SWA_BLOCK, SWA_KV_HEADS, SWA_GROUP, SWA_HEAD_DIM), 1, 0)
    blk = jnp.arange(nb)[:, None, None]
    qpos = blk * SWA_BLOCK + jnp.arange(SWA_BLOCK)[None, :, None]
    kpos = (blk - 1) * SWA_BLOCK + jnp.arange(3 * SWA_BLOCK)[None, None, :]
    band = (jnp.abs(qpos - kpos) <= SWA_WINDOW) & (kpos >= 0) & (kpos < n_lat)
    band = jnp.concatenate([band, jnp.ones((nb, SWA_BLOCK, kc.shape[1]), bool)], axis=-1)

    def block(args):
        q_b, k_b, v_b, m_b = args
        return sink_attend(q_b, jnp.concatenate([k_b, kc], axis=1),
                           jnp.concatenate([v_b, vc], axis=1), sink_logit, m_b)

    ol = lax.map(block, (qb, kw, vw, band))
    ol = jnp.moveaxis(ol, 0, 1).reshape(bsz, n_lat, nq)
    yl = ol @ w_o + b_o
    yc = None
    if ctx_out:
        oc = sink_attend(qc, kc, vc, sink_logit, None).reshape(bsz, kc.shape[1], nq)
        yc = oc @ w_o + b_o
    return yl, yc


def diff_mixer(hl, hc, w_qkv, lam_p, subln_g, w_o, lam_init, cos, sin, ctx_out):
    bsz, n_lat, _ = hl.shape

    def proj(h):
        q, k, v = jnp.split(h @ w_qkv, 3, axis=-1)
        sh = h.shape[:2]
        return (q.reshape(sh + (2 * DIFF_HEADS, DIFF_HEAD_DIM)),
                k.reshape(sh + (2 * DIFF_HEADS, DIFF_HEAD_DIM)),
                v.reshape(sh + (DIFF_HEADS, 2 * DIFF_HEAD_DIM)))

    ql, kl, vl = proj(hl)
    qc, kc, vc = proj(hc)
    ql = apply_rope(ql, cos, sin)
    kl = apply_rope(kl, cos, sin)
    split2 = lambda a: a.reshape(a.shape[:2] + (DIFF_HEADS, 2, DIFF_HEAD_DIM))
    lp = lam_p.astype(jnp.float32)
    lam = jnp.exp(jnp.sum(lp[0] * lp[1])) - jnp.exp(jnp.sum(lp[2] * lp[3])) + lam_init

    def diff_attend(q, k, v):
        s = jnp.einsum('bqhid,bshid->bhiqs', q, k).astype(jnp.float32) * DIFF_SCALE
        p = jax.nn.softmax(s, axis=-1)
        a = p[:, :, 0] - lam * p[:, :, 1]
        return jnp.einsum('bhqs,bshe->bqhe', a.astype(v.dtype), v)

    def head_out(o):
        o = rms_norm(o, subln_g) * (1.0 - lam_init)
        return o.reshape(o.shape[:2] + (DIFF_HEADS * 2 * DIFF_HEAD_DIM,)) @ w_o

    kc2 = split2(kc)
    k_all = jnp.concatenate([split2(kl), kc2], axis=1)
    v_all = jnp.concatenate([vl, vc], axis=1)
    nb = n_lat // Q_BLOCK
    qb = jnp.moveaxis(split2(ql).reshape(bsz, nb, Q_BLOCK, DIFF_HEADS, 2, DIFF_HEAD_DIM), 1, 0)
    ol = lax.map(lambda q_b: diff_attend(q_b, k_all, v_all), qb)
    ol = jnp.moveaxis(ol, 0, 1).reshape(bsz, n_lat, DIFF_HEADS, 2 * DIFF_HEAD_DIM)
    yl = head_out(ol)
    yc = head_out(diff_attend(split2(qc), kc2, vc)) if ctx_out else None
    return yl, yc


def chunk_gated_delta(q, k, v, g, beta, s0):
    bsz, t, h, _ = q.shape
    dv = v.shape[-1]
    n = t // DN_CHUNK
    f32 = jnp.float32

    def chunks(a):
        a = a.astype(f32).reshape((bsz, n, DN_CHUNK, h) + a.shape[3:])
        return jnp.moveaxis(a, (1, 2), (0, 3))

    qc, kc, vc, gch, bc = chunks(q), chunks(k), chunks(v), chunks(g), chunks(beta)
    gcum = jnp.cumsum(gch, axis=-1)
    idx = jnp.arange(DN_CHUNK)
    incl = idx[:, None] >= idx[None, :]
    strict = idx[:, None] > idx[None, :]
    decay = jnp.exp(jnp.where(incl, gcum[..., :, None] - gcum[..., None, :], -jnp.inf))
    kb = kc * bc[..., None]
    vb = vc * bc[..., None]
    lower = jnp.where(strict, jnp.einsum('nbhid,nbhjd->nbhij', kb, kc) * decay, 0.0)
    u = lax.linalg.triangular_solve(lower, vb, left_side=True, lower=True, unit_diagonal=True)
    w = lax.linalg.triangular_solve(lower, kb * jnp.exp(gcum)[..., None], left_side=True,
                                    lower=True, unit_diagonal=True)
    intra = jnp.where(incl, jnp.einsum('nbhid,nbhjd->nbhij', qc, kc) * decay, 0.0)

    def step(s, inp):
        q_i, k_i, u_i, w_i, g_i, a_i = inp
        v_new = u_i - jnp.einsum('bhcd,bhde->bhce', w_i, s)
        o = (jnp.einsum('bhcd,bhde->bhce', q_i * jnp.exp(g_i)[..., None], s)
             + jnp.einsum('bhij,bhje->bhie', a_i, v_new))
        g_last = g_i[..., -1]
        s = (s * jnp.exp(g_last)[..., None, None]
             + jnp.einsum('bhcd,bhce->bhde', k_i * jnp.exp(g_last[..., None] - g_i)[..., None], v_new))
        return s, o

    s_fin, o = lax.scan(step, s0.astype(f32), (qc, kc, u, w, gcum, intra))
    o = jnp.moveaxis(o, (0, 3), (1, 2)).reshape(bsz, t, h, dv)
    return o.astype(v.dtype), s_fin


def delta_mixer(hl, hc, w_qkvz, w_ba, a_log, dt_bias, w_conv, norm_g, w_o, ctx_out):
    nqk = DN_QK_HEADS * DN_HEAD_DIM
    nv = DN_V_HEADS * DN_HEAD_DIM
    rep = DN_V_HEADS // DN_QK_HEADS

    def prep(h):
        bsz, t, _ = h.shape
        proj = h @ w_qkvz
        qkv = jax.nn.silu(centred_dwconv(proj[..., :2 * nqk + nv], w_conv))
        z = proj[..., 2 * nqk + nv:].reshape(bsz, t, DN_V_HEADS, DN_HEAD_DIM)
        q = qkv[..., :nqk].reshape(bsz, t, DN_QK_HEADS, DN_HEAD_DIM)
        k = qkv[..., nqk:2 * nqk].reshape(bsz, t, DN_QK_HEADS, DN_HEAD_DIM)
        v = qkv[..., 2 * nqk:].reshape(bsz, t, DN_V_HEADS, DN_HEAD_DIM)
        q = jnp.repeat(l2_normalize(q), rep, axis=2) * (DN_HEAD_DIM ** -0.5)
        k = jnp.repeat(l2_normalize(k), rep, axis=2)
        ba = (h @ w_ba).astype(jnp.float32).reshape(bsz, t, 2, 2, DN_V_HEADS)
        beta = jax.nn.sigmoid(ba[:, :, :, 0])
        g = -jnp.exp(a_log.astype(jnp.float32)) * jax.nn.softplus(ba[:, :, :, 1] + dt_bias.astype(jnp.float32))
        return q, k, v, z, beta, g

    qc, kc, vc, zc, bc, gc = prep(hc)
    ql, kl, vl, zl, bl, gl = prep(hl)
    rev = lambda a: a[:, ::-1]
    s0 = jnp.zeros((hc.shape[0], DN_V_HEADS, DN_HEAD_DIM, DN_HEAD_DIM), jnp.float32)
    oc_f, sc_f = chunk_gated_delta(qc, kc, vc, gc[:, :, 0], bc[:, :, 0], s0)
    ol_f, _ = chunk_gated_delta(ql, kl, vl, gl[:, :, 0], bl[:, :, 0], sc_f)
    oc_b, sc_b = chunk_gated_delta(rev(qc), rev(kc), rev(vc), rev(gc[:, :, 1]), rev(bc[:, :, 1]), s0)
    ol_b, _ = chunk_gated_delta(rev(ql), rev(kl), rev(vl), rev(gl[:, :, 1]), rev(bl[:, :, 1]), sc_b)

    def out(o, z):
        o = rms_norm(o, norm_g) * jax.nn.silu(z)
        return o.reshape(o.shape[:2] + (nv,)) @ w_o

    yl = out(ol_f + rev(ol_b), zl)
    yc = out(oc_f + rev(oc_b), zc) if ctx_out else None
    return yl, yc


def moe_ffn(h, w_r, b_r, w1, b1, w2, b2):
    n_tok, d = h.shape
    logits = (h @ w_r + b_r).astype(jnp.float32)
    top_logit, top_idx = lax.top_k(logits, TOP_K)
    top_w = jax.nn.softmax(top_logit, axis=-1).astype(h.dtype)
    n_assign = n_tok * TOP_K
    flat_e = top_idx.reshape(-1)
    flat_w = top_w.reshape(-1)
    flat_tok = jnp.arange(n_assign, dtype=jnp.int32) // TOP_K
    order = jnp.argsort(flat_e)
    sorted_e = flat_e[order]
    counts = jnp.bincount(flat_e, length=N_EXPERTS)
    starts = jnp.cumsum(counts) - counts
    padded = (counts + EXPERT_BLOCK - 1) // EXPERT_BLOCK * EXPERT_BLOCK
    pends = jnp.cumsum(padded)
    pstarts = pends - padded
    dest = pstarts[sorted_e] + jnp.arange(n_assign, dtype=jnp.int32) - starts[sorted_e]
    n_rows = (-(-n_assign // EXPERT_BLOCK) + N_EXPERTS) * EXPERT_BLOCK
    n_blocks = n_rows // EXPERT_BLOCK
    row_tok = jnp.full((n_rows,), n_tok, jnp.int32).at[dest].set(flat_tok[order])
    row_w = jnp.zeros((n_rows,), h.dtype).at[dest].set(flat_w[order])
    block_e = jnp.minimum(jnp.searchsorted(pends, jnp.arange(n_blocks) * EXPERT_BLOCK, side='right'),
                          N_EXPERTS - 1)
    h_pad = jnp.concatenate([h, jnp.zeros((1, d), h.dtype)], axis=0)

    def expert_block(args):
        tok, e = args
        hh = h_pad[tok] @ w1[e] + b1[e]
        gate = jnp.minimum(hh[:, :D_FF], SWIGLU_LIMIT)
        up = jnp.clip(hh[:, D_FF:], -SWIGLU_LIMIT, SWIGLU_LIMIT)
        act = (up + 1.0) * gate * jax.nn.sigmoid(SWIGLU_ALPHA * gate)
        return act @ w2[e] + b2[e]

    rows = lax.map(expert_block, (row_tok.reshape(n_blocks, EXPERT_BLOCK), block_e))
    rows = rows.reshape(n_rows, d) * row_w[:, None]
    return jnp.zeros((n_tok + 1, d), h.dtype).at[row_tok].add(rows)[:n_tok]


def setup_inputs(seed: int = 0) -> dict:
    key = jax.random.key(seed)
    ks = list(jax.random.split(key, 40))
    D = D_MODEL

    def nrm(i, shape, scale):
        return jax.random.normal(ks[i], shape, jnp.float32) * scale

    na, nb, nc, nd = [len(range(m, DEPTH, N_MIXERS)) for m in range(N_MIXERS)]
    nq = SWA_HEADS * SWA_HEAD_DIM
    n_swa = nq + 2 * SWA_KV_HEADS * SWA_HEAD_DIM
    nv = DN_V_HEADS * DN_HEAD_DIM
    n_qkvz = 2 * DN_QK_HEADS * DN_HEAD_DIM + 2 * nv
    n_conv = 2 * DN_QK_HEADS * DN_HEAD_DIM + nv
    a_log = jnp.log(jax.random.uniform(ks[33], (nd, 2, DN_V_HEADS), jnp.float32, minval=1.0, maxval=16.0))
    dt = jnp.exp(jax.random.uniform(ks[34], (nd, 2, DN_V_HEADS), jnp.float32,
                                    minval=math.log(1e-3), maxval=math.log(1e-1)))
    dt_bias = dt + jnp.log(-jnp.expm1(-dt))
    return {
        'x': nrm(0, (BATCH, SEQ, D), 1.0),
        'c': nrm(1, (BATCH, D), 1.0),
        'ctx': nrm(2, (BATCH, CTX_LEN, D), 1.0),
        'c_ctx': nrm(3, (D,), 1.0),
        'mod_w': nrm(4, (DEPTH, D, 6 * D), 0.5 * D ** -0.5),
        'mod_b': nrm(5, (DEPTH, 6 * D), 0.02),
        'ln1_g': 1.0 + nrm(6, (DEPTH, D), 0.02),
        'ln1_b': nrm(7, (DEPTH, D), 0.02),
        'ln2_g': 1.0 + nrm(8, (DEPTH, D), 0.02),
        'ln2_b': nrm(9, (DEPTH, D), 0.02),
        'router_w': nrm(10, (DEPTH, D, N_EXPERTS), D ** -0.5),
        'router_b': nrm(11, (DEPTH, N_EXPERTS), 0.01),
        'exp_w1': nrm(12, (DEPTH, N_EXPERTS, D, 2 * D_FF), D ** -0.5),
        'exp_b1': nrm(13, (DEPTH, N_EXPERTS, 2 * D_FF), 0.01),
        'exp_w2': nrm(14, (DEPTH, N_EXPERTS, D_FF, D), DEEPNORM_BETA * D_FF ** -0.5),
        'exp_b2': nrm(15, (DEPTH, N_EXPERTS, D), 0.01),
        'conv_in_w': nrm(16, (na, D, 3 * D), D ** -0.5),
        'conv_w': nrm(17, (na, SC_WIDTH, D), SC_WIDTH ** -0.5),
        'conv_out_w': nrm(18, (na, D, D), DEEPNORM_BETA * D ** -0.5),
        'swa_qkv_w': nrm(19, (nb, D, n_swa), D ** -0.5),
        'swa_qkv_b': nrm(20, (nb, n_swa), 0.01),
        'swa_sink': nrm(21, (nb, SWA_HEADS), 0.5),
        'swa_out_w': nrm(22, (nb, nq, D), DEEPNORM_BETA * nq ** -0.5),
        'swa_out_b': nrm(23, (nb, D), 0.01),
        'diff_qkv_w': nrm(24, (nc, D, 3 * D), D ** -0.5),
        'diff_lambda': nrm(25, (nc, 4, DIFF_HEAD_DIM), 0.1),
        'diff_subln_g': 1.0 + nrm(26, (nc, 2 * DIFF_HEAD_DIM), 0.02),
        'diff_out_w': nrm(27, (nc, D, D), DEEPNORM_BETA * D ** -0.5),
        'delta_qkvz_w': nrm(28, (nd, D, n_qkvz), D ** -0.5),
        'delta_ba_w': nrm(29, (nd, D, 4 * DN_V_HEADS), D ** -0.5),
        'delta_a_log': a_log,
        'delta_dt_bias': dt_bias,
        'delta_conv_w': nrm(30, (nd, DN_CONV, n_conv), DN_CONV ** -0.5),
        'delta_norm_g': 1.0 + nrm(31, (nd, DN_HEAD_DIM), 0.02),
        'delta_out_w': nrm(32, (nd, nv, D), DEEPNORM_BETA * nv ** -0.5),
    }


def reference(x, c, ctx, c_ctx, mod_w, mod_b, ln1_g, ln1_b, ln2_g, ln2_b,
              router_w, router_b, exp_w1, exp_b1, exp_w2, exp_b2,
              conv_in_w, conv_w, conv_out_w,
              swa_qkv_w, swa_qkv_b, swa_sink, swa_out_w, swa_out_b,
              diff_qkv_w, diff_lambda, diff_subln_g, diff_out_w,
              delta_qkvz_w, delta_ba_w, delta_a_log, delta_dt_bias, delta_conv_w, delta_norm_g, delta_out_w):
    n_lat = x.shape[1]
    rows = n_lat // GRID_W
    cos_swa, sin_swa = axial_rope_tables(rows, SWA_HEAD_DIM)
    cos_diff, sin_diff = axial_rope_tables(rows, DIFF_HEAD_DIM)
    silu_c = jax.nn.silu(c)
    silu_cc = jax.nn.silu(c_ctx)
    xl, xc = x, ctx
    for i in range(DEPTH):
        last = i == DEPTH - 1
        kind, j = i % N_MIXERS, i // N_MIXERS
        mod_l = jnp.split((silu_c @ mod_w[i] + mod_b[i])[:, None, :], 6, axis=-1)
        mod_c = jnp.split(silu_cc @ mod_w[i] + mod_b[i], 6, axis=-1)
        hl = modulate(xl, mod_l[0], mod_l[1])
        hc = modulate(xc, mod_c[0], mod_c[1])
        if kind == 0:
            yl, yc = short_conv_mixer(hl, hc, conv_in_w[j], conv_w[j], conv_out_w[j], not last)
        elif kind == 1:
            yl, yc = swa_mixer(hl, hc, swa_qkv_w[j], swa_qkv_b[j], swa_sink[j], swa_out_w[j], swa_out_b[j],
                               cos_swa, sin_swa, not last)
        elif kind == 2:
            lam_init = 0.8 - 0.6 * math.exp(-0.3 * i)
            yl, yc = diff_mixer(hl, hc, diff_qkv_w[j], diff_lambda[j], diff_subln_g[j], diff_out_w[j],
                                lam_init, cos_diff, sin_diff, not last)
        else:
            yl, yc = delta_mixer(hl, hc, delta_qkvz_w[j], delta_ba_w[j], delta_a_log[j], delta_dt_bias[j],
                                 delta_conv_w[j], delta_norm_g[j], delta_out_w[j], not last)
        xl = layer_norm(DEEPNORM_ALPHA * xl + mod_l[2] * yl, ln1_g[i], ln1_b[i])
        if not last:
            xc = layer_norm(DEEPNORM_ALPHA * xc + mod_c[2] * yc, ln1_g[i], ln1_b[i])
        moe_args = (router_w[i], router_b[i], exp_w1[i], exp_b1[i], exp_w2[i], exp_b2[i])
        hl = modulate(xl, mod_l[3], mod_l[4]).reshape(-1, D_MODEL)
        if last:
            fl = moe_ffn(hl, *moe_args)
        else:
            hc = modulate(xc, mod_c[3], mod_c[4]).reshape(-1, D_MODEL)
            f = moe_ffn(jnp.concatenate([hl, hc], axis=0), *moe_args)
            fl, fc = f[:hl.shape[0]], f[hl.shape[0]:]
            xc = layer_norm(DEEPNORM_ALPHA * xc + mod_c[5] * fc.reshape(xc.shape), ln2_g[i], ln2_b[i])
        xl = layer_norm(DEEPNORM_ALPHA * xl + mod_l[5] * fl.reshape(xl.shape), ln2_g[i], ln2_b[i])
    return xl
```

```python
import math
import numpy as np
from contextlib import ExitStack
import concourse.bass as bass
import concourse.mybir as mybir
from concourse.bass_utils import run_bass_kernel_spmd

F32 = mybir.dt.float32
BF16 = mybir.dt.bfloat16
AF = mybir.ActivationFunctionType
ALU = mybir.AluOpType

D = 1024
SEQ = 4096
CTX = 256
T = SEQ + CTX
NT = T // 128
NTL = SEQ // 128
DEPTH = 4
ALPHA = (2 * DEPTH) ** 0.25
LN_EPS = 1e-5
RMS_EPS = 1e-6
NE = 32
ENGS = ("pe", "act", "dve", "pool", "sp")
N_DMA_SEMS = 12


class Dep:
    __slots__ = ("w", "r")

    def __init__(self):
        self.w = None
        self.r = {}


class MK:
    def __init__(self, nc):
        self.nc = nc
        self.es = ExitStack()
        self.prog = {e: [] for e in ENGS}
        self.cnt = {e: 0 for e in ENGS}
        self.sems = {e: self.es.enter_context(nc.semaphore("sem_" + e)) for e in ENGS}
        self.dsem, self.dsem_uses, self.dq_count = {}, {}, {}
        for q in ("sp", "pool", "act"):
            self.dsem[q] = [self.es.enter_context(nc.semaphore(f"dma_{q}_{i}")) for i in range(N_DMA_SEMS)]
            self.dsem_uses[q] = [0] * N_DMA_SEMS
            self.dq_count[q] = 0
        self.waited = {e: {} for e in ENGS}
        self.semobj = {("e", e): self.sems[e] for e in ENGS}
        for q in self.dsem:
            for i, s in enumerate(self.dsem[q]):
                self.semobj[("d", q, i)] = s
        self.n_inst = 0
        self.n_wait = 0
        self.ps_banks = None
        self.ps_deps = None
        self.ps_i = 0

    def sbuf(self, name, shape, dtype):
        return self.es.enter_context(self.nc.sbuf_tensor(name, list(shape), dtype))

    def init_psum(self):
        self.ps_banks = [self.es.enter_context(self.nc.psum_tensor(f"psb{i}", [128, 512], F32)) for i in range(8)]
        self.ps_deps = [Dep() for _ in range(8)]

    def next_ps(self):
        i = self.ps_i % 8
        self.ps_i += 1
        return self.ps_banks[i], self.ps_deps[i]

    def ps_rot(self, key, banks):
        if not hasattr(self, "_rot"):
            self._rot = {}
        k = self._rot.get(key, 0)
        self._rot[key] = k + 1
        i = banks[k % len(banks)]
        return self.ps_banks[i], self.ps_deps[i]

    def _waits(self, eng, deps, force_same=False):
        hard, soft = deps
        out = []
        for sk in set(hard) | set(soft):
            hv, sv = hard.get(sk, 0), soft.get(sk, 0)
            if sk == ("e", eng) and not force_same:
                if eng == "pe":
                    continue
                val = hv
            else:
                val = max(hv, sv)
            if val <= 0 or self.waited[eng].get(sk, 0) >= val:
                continue
            self.waited[eng][sk] = val
            out.append((self.semobj[sk], val))
        return out

    def _collect(self, r, w):
        hard, soft = {}, {}
        for b in r:
            if b.w is not None and hard.get(b.w[0], 0) < b.w[1]:
                hard[b.w[0]] = b.w[1]
        for b in w:
            if b.w is not None and hard.get(b.w[0], 0) < b.w[1]:
                hard[b.w[0]] = b.w[1]
            for sk, val in b.r.items():
                if soft.get(sk, 0) < val:
                    soft[sk] = val
        return hard, soft

    def _commit(self, ev, r, w):
        sk, val = ev
        for b in r:
            if b.r.get(sk, 0) < val:
                b.r[sk] = val
        for b in w:
            b.w = ev
            b.r = {}

    def op(self, eng, fn, r=(), w=()):
        waits = self._waits(eng, self._collect(r, w))
        self.cnt[eng] += 1
        ev = (("e", eng), self.cnt[eng])
        sem = self.sems[eng]
        self.n_wait += len(waits)
        self.n_inst += 1

        def emit(E, waits=waits, fn=fn, sem=sem):
            for s, v in waits:
                E.wait_ge(s, v)
            fn(E).then_inc(sem, 1)
        self.prog[eng].append(emit)
        self._commit(ev, r, w)
        return ev

    def dma(self, q, out, in_, r=(), w=(), **kw):
        k = self.dq_count[q]
        self.dq_count[q] += 1
        i = k % N_DMA_SEMS
        sk = ("d", q, i)
        deps = self._collect(r, w)
        prev = self.dsem_uses[q][i]
        if prev > 0:
            deps[0][sk] = max(deps[0].get(sk, 0), 16 * prev)
        waits = self._waits(q, deps, force_same=True)
        self.dsem_uses[q][i] += 1
        ev = (sk, 16 * self.dsem_uses[q][i])
        sem = self.dsem[q][i]
        self.n_wait += len(waits)
        self.n_inst += 1

        def emit(E, waits=waits, sem=sem, out=out, in_=in_, kw=kw):
            for s, v in waits:
                E.wait_ge(s, v)
            E.dma_start(out=out, in_=in_, **kw).then_inc(sem, 16)
        self.prog[q].append(emit)
        self._commit(ev, r, w)
        return ev

    def barrier(self):
        evs = []
        for e in ENGS:
            if self.cnt[e] > 0:
                evs.append((("e", e), self.cnt[e]))
        for q in self.dsem:
            for i in range(N_DMA_SEMS):
                if self.dsem_uses[q][i] > 0:
                    evs.append((("d", q, i), 16 * self.dsem_uses[q][i]))
        for e in ENGS:
            waits = []
            for sk, val in evs:
                if sk == ("e", e):
                    continue
                if self.waited[e].get(sk, 0) >= val:
                    continue
                self.waited[e][sk] = val
                waits.append((self.semobj[sk], val))
            if waits:
                def emit(E, waits=waits):
                    for s, v in waits:
                        E.wait_ge(s, v)
                self.prog[e].append(emit)

    def finish(self):
        self.barrier()
        prog = self.prog
        with self.nc.Block() as block:
            @block.tensor
            def _(E):
                for f in prog["pe"]:
                    f(E)

            @block.scalar
            def _(E):
                for f in prog["act"]:
                    f(E)

            @block.vector
            def _(E):
                for f in prog["dve"]:
                    f(E)

            @block.gpsimd
            def _(E):
                for f in prog["pool"]:
                    f(E)

            @block.sync
            def _(E):
                for f in prog["sp"]:
                    f(E)
        self.es.close()

    def mm(self, out, lhsT, rhs, start, stop, r=(), w=(), sgc=False):
        if sgc:
            return self.op("pe", lambda E: E.matmul(out, lhsT, rhs, start=start, stop=stop, skip_group_check=True), r, w)
        return self.op("pe", lambda E: E.matmul(out, lhsT, rhs, start=start, stop=stop), r, w)

    def tr(self, out, in_, ident, r=(), w=()):
        return self.op("pe", lambda E: E.transpose(out, in_, ident), r, w)

    def ts(self, eng, out, in0, s1, s2, op0, op1=None, r=(), w=(), accum_out=None):
        if op1 is None:
            if accum_out is None:
                return self.op(eng, lambda E: E.tensor_scalar(out, in0, s1, None, op0), r, w)
        if accum_out is not None:
            return self.op(eng, lambda E: E.tensor_scalar(out, in0, s1, s2, op0, op1, accum_out=accum_out), r, w)
        return self.op(eng, lambda E: E.tensor_scalar(out, in0, s1, s2, op0, op1), r, w)

    def tt(self, eng, out, in0, in1, op, r=(), w=()):
        return self.op(eng, lambda E: E.tensor_tensor(out, in0, in1, op), r, w)

    def stt(self, eng, out, in0, scalar, in1, op0, op1, r=(), w=()):
        return self.op(eng, lambda E: E.scalar_tensor_tensor(out, in0, scalar, in1, op0, op1), r, w)

    def act(self, out, in_, func, bias=None, scale=1.0, r=(), w=(), accum_out=None):
        kw = {}
        if bias is not None:
            kw["bias"] = bias
        if accum_out is not None:
            kw["accum_out"] = accum_out
        return self.op("act", lambda E: E.activation(out, in_, func, scale=scale, **kw), r, w)

    def copy(self, eng, out, in_, r=(), w=()):
        if eng == "act":
            return self.op("act", lambda E: E.copy(out, in_), r, w)
        return self.op(eng, lambda E: E.tensor_copy(out, in_), r, w)

    def memset(self, eng, ap, val, r=(), w=()):
        return self.op(eng, lambda E: E.memset(ap, val), r, w)


class Arena:
    def __init__(self, mk, words):
        self.mk = mk
        self.t = mk.sbuf("arena", [128, words], F32)
        self.cap = words
        self.off = 0

    def mark(self):
        return self.off

    def release(self, m):
        self.mk.barrier()
        self.off = m

    def alloc(self, shape, dtype=F32, parts=128):
        n = 1
        for s in shape:
            n *= s
        size = 4 if dtype == F32 else 2
        words = (n * size + 3) // 4
        words = (words + 7) // 8 * 8
        if self.off + words > self.cap:
            raise RuntimeError(f"arena overflow: need {words} at {self.off} cap {self.cap}")
        ap = self.t[0:parts, self.off:self.off + words]
        self.off += words
        if dtype != F32:
            ap = ap.bitcast(dtype)
        ap = ap[:, 0:n]
        if len(shape) == 2:
            ap = ap.rearrange("p (a b) -> p a b", a=shape[0])
        elif len(shape) == 3:
            ap = ap.rearrange("p (a b c) -> p a b c", a=shape[0], b=shape[1])
        elif len(shape) == 4:
            ap = ap.rearrange("p (a b c d) -> p a b c d", a=shape[0], b=shape[1], c=shape[2])
        return ap


W_SPECS = [
    ("mod_w", [4, 1024, 6144]), ("mod_b", [4, 6144]), ("ln1_g", [4, 1024]), ("ln1_b", [4, 1024]),
    ("ln2_g", [4, 1024]), ("ln2_b", [4, 1024]), ("router_w", [4, 1024, 32]), ("router_b", [4, 32]),
    ("exp_w1", [4, 32, 1024, 2048]), ("exp_b1", [4, 32, 2048]), ("exp_w2", [4, 32, 1024, 1024]),
    ("exp_b2", [4, 32, 1024]), ("conv_in_w", [1, 1024, 3072]), ("conv_w", [1, 3, 1024]),
    ("conv_out_w", [1, 1024, 1024]), ("swa_qkv_w", [1, 1024, 1536]), ("swa_qkv_b", [1, 1536]),
    ("swa_sink", [1, 16]), ("swa_out_w", [1, 1024, 1024]), ("swa_out_b", [1, 1024]),
    ("diff_qkv_w", [1, 1024, 3072]), ("diff_lambda", [1, 4, 64]), ("diff_subln_g", [1, 128]),
    ("diff_out_w", [1, 1024, 1024]), ("delta_qkvz_w", [1, 1024, 6144]), ("delta_ba_w", [1, 1024, 64]),
    ("delta_a_log", [1, 2, 16]), ("delta_dt_bias", [1, 2, 16]), ("delta_conv_w", [1, 5, 4096]),
    ("delta_norm_g", [1, 128]), ("delta_out_w", [1, 2048, 1024]),
]


def host_constants():
    c = {}
    c["ident"] = np.eye(128, dtype=np.float32)
    rows = SEQ // 64
    row = np.repeat(np.arange(rows, dtype=np.float32), 64)
    col = np.tile(np.arange(64, dtype=np.float32), rows)
    half = 32
    inv = (10000.0 ** (-np.arange(0, half, 2, dtype=np.float32) / half)).astype(np.float32)
    ar = row[:, None] * inv
    ac = col[:, None] * inv
    ang = np.concatenate([ar, ar, ac, ac], axis=-1).astype(np.float32)
    cos = np.cos(ang).astype(np.float32).T
    sin = np.sin(ang).astype(np.float32).T
    sign = np.ones((64, 1), np.float32)
    sign[0:16] = -1.0
    sign[32:48] = -1.0
    c["cosT"] = np.ascontiguousarray(np.concatenate([cos, cos], axis=0))
    c["sinT"] = np.ascontiguousarray(np.concatenate([sin, sin], axis=0))
    Rm = np.zeros((128, 128), np.float32)
    for hh in range(2):
        for p in range(64):
            blk = p // 16
            if blk % 2 == 0:
                Rm[hh * 64 + p + 16, hh * 64 + p] = -1.0
            else:
                Rm[hh * 64 + p - 16, hh * 64 + p] = 1.0
    c["rotm"] = Rm
    kj = np.arange(128)[:, None]
    qi = np.arange(128)[None, :]
    c["mprev"] = np.tile((kj >= qi).astype(np.float32), (1, 4))
    c["mnext"] = np.tile((kj <= qi).astype(np.float32), (1, 4))
    k_ = np.arange(128)[:, None]
    m_ = np.arange(128)[None, :]
    c["triF"] = (k_ <= m_).astype(np.float32)
    c["triB"] = (k_ >= m_).astype(np.float32)
    c["m2F"] = (k_ > m_).astype(np.float32)
    c["m2B"] = (k_ < m_).astype(np.float32)
    c["selF"] = np.repeat((np.arange(128) == 127).astype(np.float32)[:, None], 128, axis=1)
    c["selB"] = np.repeat((np.arange(128) == 0).astype(np.float32)[:, None], 128, axis=1)
    return c


class Prog:
    def __init__(self, nc, layers, test_mode=False, do_moe=True):
        self.nc = nc
        self.do_moe = do_moe
        self.dbg_names = []
        self.layers = list(layers)
        self.test_mode = test_mode
        self.mk = MK(nc)
        mk = self.mk
        mk.init_psum()
        self.A = Arena(mk, 51 * 1024)
        dt = nc.dram_tensor
        self.x_d = dt("x", [SEQ, D], F32, kind="ExternalInput").ap()
        self.ctx_d = dt("ctx", [CTX, D], F32, kind="ExternalInput").ap()
        self.c_d = dt("c", [D], F32, kind="ExternalInput").ap()
        self.cctx_d = dt("c_ctx", [D], F32, kind="ExternalInput").ap()
        wshapes = dict(W_SPECS)

        class _LazyW(dict):
            def __missing__(d, name):
                ap = dt(name, wshapes[name], F32, kind="ExternalInput").ap()
                d[name] = ap
                return ap
        self.W = _LazyW()
        if not test_mode:
            for name, shape in W_SPECS:
                self.W[name]
        self.C = {}
        for name, arr in host_constants().items():
            self.C[name] = dt(name, list(arr.shape), F32, kind="ExternalInput").ap()
        out_rows = T if test_mode else SEQ
        self.out_d = dt("out", [out_rows, D], F32, kind="ExternalOutput").ap()
        self.xres = dt("xres", [T, D], F32).ap()
        self.xdep = [Dep() for _ in range(NT)]
        self.outdep = [Dep() for _ in range(NT)]
        self.src = [self.x_d[t * 128:(t + 1) * 128, :] for t in range(NTL)] + \
                   [self.ctx_d[t * 128:(t + 1) * 128, :] for t in range(NT - NTL)]
        self.setup_persistent()

    def setup_persistent(self):
        mk, A = self.mk, self.A
        self.ident = A.alloc((128,), F32)
        self.d_ident = Dep()
        mk.dma("sp", self.ident, self.C["ident"][:, :], w=[self.d_ident])
        self.identb = A.alloc((128,), BF16)
        self.d_identb = Dep()
        mk.copy("dve", self.identb, self.ident, r=[self.d_ident], w=[self.d_identb])
        self.ones = A.alloc((128,), F32)
        self.d_ones = Dep()
        mk.memset("dve", self.ones, 1.0, w=[self.d_ones])
        self.modF = A.alloc((2, 6, 8), F32)
        self.d_modF = Dep()
        self.modbc = A.alloc((2, 2, 1024), F32)
        self.d_modbc = Dep()
        self.lnbc = A.alloc((4, 1024), F32)
        self.d_lnbc = Dep()
        self.xt = [A.alloc((1024,), F32) for _ in range(2)]
        self.d_xt = [Dep() for _ in range(2)]
        self.zt = [A.alloc((1024,), F32) for _ in range(2)]
        self.d_zt = [Dep() for _ in range(2)]
        self.st = [A.alloc((2, 6), F32) for _ in range(2)]
        self.mv = [A.alloc((4,), F32) for _ in range(2)]
        self.xt_i = 0
        self.zt_i = 0
        self.fm_tmp = A.alloc((128,), F32, parts=64)
        self.d_fm_tmp = Dep()

    def load_featmajor(self, dst, d_dst, vec_ap, n):
        mk = self.mk
        tmp, d_tmp = self.fm_tmp[0:n, :], self.d_fm_tmp
        mk.dma("sp", tmp, vec_ap.rearrange("(j p) -> j p", p=128), w=[d_tmp])
        ps, dps = mk.next_ps()
        mk.tr(ps[:, 0:n], tmp, self.ident[0:n, 0:n], r=[d_tmp, self.d_ident], w=[dps])
        mk.copy("dve", dst, ps[:, 0:n], r=[dps], w=[d_dst])

    def phase_mod(self, i):
        mk, A, W = self.mk, self.A, self.W
        m0 = A.mark()
        cT = A.alloc((8, 2), F32)
        d_cT = Dep()
        ctmp = A.alloc((8,), F32)
        d_ctmp = Dep()
        self.load_featmajor(ctmp, d_ctmp, self.c_d, 8)
        mk.act(cT[:, :, 0], ctmp, AF.Silu, r=[d_ctmp], w=[d_cT])
        self.load_featmajor(ctmp, d_ctmp, self.cctx_d, 8)
        mk.act(cT[:, :, 1], ctmp, AF.Silu, r=[d_ctmp], w=[d_cT])
        sbc = A.alloc((2, 8, 128), F32)
        d_sbc = Dep()
        for lc in range(2):
            for k in range(8):
                mk.ts("dve", sbc[:, lc, k, :], self.ones, cT[:, k, lc:lc + 1], None, ALU.mult,
                      r=[self.d_ones, d_cT], w=[d_sbc])
        mbF = A.alloc((48,), F32)
        d_mbF = Dep()
        self.load_featmajor(mbF, d_mbF, W["mod_b"][i], 48)
        mbbc = A.alloc((1024,), F32)
        d_mbbc = Dep()
        wsl = [A.alloc((8, 1024), F32) for _ in range(2)]
        d_wsl = [Dep() for _ in range(2)]
        mw = W["mod_w"][i].rearrange("(k p) n -> p k n", p=128)
        for m in range(6):
            ws, dws = wsl[m % 2], d_wsl[m % 2]
            mk.dma("sp", ws, mw[:, :, m * 1024:(m + 1) * 1024], w=[dws])
            for kc in range(8):
                ps, dps = mk.next_ps()
                for kk in range(8):
                    mk.mm(ps[:, 0:2], ws[:, kk, kc * 128:(kc + 1) * 128], cT[:, kk, 0:2], kk == 0, kk == 7,
                          r=[dws, d_cT], w=[dps])
                mk.ts("dve", self.modF[:, :, m, kc], ps[:, 0:2], mbF[:, m * 8 + kc:m * 8 + kc + 1], None, ALU.add,
                      r=[dps, d_mbF], w=[self.d_modF])
            if m in (2, 5):
                j = 0 if m == 2 else 1
                mk.dma("sp", mbbc, W["mod_b"][i][m * 1024:(m + 1) * 1024].partition_broadcast(128), w=[d_mbbc])
                for lc in range(2):
                    for hf in range(2):
                        ps, dps = mk.next_ps()
                        for kk in range(8):
                            mk.mm(ps[:, :], sbc[:, lc, kk, :], ws[:, kk, hf * 512:(hf + 1) * 512], kk == 0, kk == 7,
                                  r=[dws, d_sbc], w=[dps])
                        mk.tt("dve", self.modbc[:, lc, j, hf * 512:(hf + 1) * 512], ps[:, :],
                              mbbc[:, hf * 512:(hf + 1) * 512], ALU.add, r=[dps, d_mbbc], w=[self.d_modbc])
        for m in (1, 4):
            mk.ts("dve", self.modF[:, :, m, :], self.modF[:, :, m, :], 1.0, None, ALU.add, r=[], w=[self.d_modF])
        for q, nm in enumerate(("ln1_g", "ln1_b", "ln2_g", "ln2_b")):
            mk.dma("sp", self.lnbc[:, q, :], W[nm][i].partition_broadcast(128), w=[self.d_lnbc])
        A.release(m0)

    def prep_tile(self, tt, mi, hT, d_hT, col0, h32=None, d_h32=None):
        mk = self.mk
        lc = 0 if tt < NTL else 1
        b = self.xt_i % 2
        self.xt_i += 1
        xt, dxt = self.xt[b], self.d_xt[b]
        mk.dma("sp", xt, self.src[tt], r=[self.xdep[tt]], w=[dxt])
        for hf in range(2):
            ps, dps = mk.next_ps()
            for q in range(4):
                k = hf * 4 + q
                mk.tr(ps[:, q * 128:(q + 1) * 128], xt[:, k * 128:(k + 1) * 128], self.ident,
                      r=[dxt, self.d_ident], w=[dps])
            for q in range(4):
                k = hf * 4 + q
                o = hT[:, k, col0:col0 + 128]
                sc = self.modF[:, lc, mi + 1, k:k + 1]
                sh = self.modF[:, lc, mi, k:k + 1]
                if h32 is not None:
                    o32 = h32[:, k, :]
                    mk.ts("dve", o32, ps[:, q * 128:(q + 1) * 128], sc, sh, ALU.mult, ALU.add,
                          r=[dps, self.d_modF], w=[d_h32])
                    mk.copy("act", o, o32, r=[d_h32], w=[d_hT])
                elif q % 2 == 0:
                    mk.ts("dve", o, ps[:, q * 128:(q + 1) * 128], sc, sh, ALU.mult, ALU.add,
                          r=[dps, self.d_modF], w=[d_hT])
                else:
                    mk.act(o, ps[:, q * 128:(q + 1) * 128], AF.Identity, bias=sh, scale=sc,
                           r=[dps, self.d_modF], w=[d_hT])

    def post_tile(self, tt, y, d_y, j, dst, d_dst, ybias=None, d_ybias=None):
        mk = self.mk
        lc = 0 if tt < NTL else 1
        b = self.xt_i % 2
        self.xt_i += 1
        xt, dxt = self.xt[b], self.d_xt[b]
        mk.dma("sp", xt, self.src[tt], r=[self.xdep[tt]], w=[dxt])
        zb = self.zt_i % 2
        self.zt_i += 1
        z, dz = self.zt[zb], self.d_zt[zb]
        st, mv = self.st[zb], self.mv[zb]
        for hf in range(2):
            sl = slice(hf * 512, (hf + 1) * 512)
            if ybias is not None:
                mk.tt("dve", z[:, sl], y[hf], ybias[:, sl], ALU.add, r=list(d_y) + [d_ybias], w=[dz])
                mk.tt("dve", z[:, sl], z[:, sl], self.modbc[:, lc, j, sl], ALU.mult, r=[self.d_modbc], w=[dz])
            else:
                mk.tt("dve", z[:, sl], y[hf], self.modbc[:, lc, j, sl], ALU.mult, r=list(d_y) + [self.d_modbc], w=[dz])
        mk.stt("dve", z, xt, ALPHA, z, ALU.mult, ALU.add, r=[dxt], w=[dz])
        mk.op("dve", lambda E: E.bn_stats(st[:, 0, :], z[:, 0:512]), r=[], w=[dz])
        mk.op("dve", lambda E: E.bn_stats(st[:, 1, :], z[:, 512:1024]), r=[], w=[dz])
        mk.op("dve", lambda E: E.bn_aggr(mv[:, 0:2], st), r=[], w=[dz])
        mk.ts("dve", mv[:, 2:3], mv[:, 1:2], LN_EPS, None, ALU.add, r=[], w=[dz])
        mk.act(mv[:, 2:3], mv[:, 2:3], AF.Sqrt, r=[dz], w=[dz])
        mk.op("dve", lambda E: E.reciprocal(mv[:, 2:3], mv[:, 2:3]), r=[dz], w=[dz])
        mk.ts("dve", z, z, mv[:, 0:1], mv[:, 2:3], ALU.subtract, ALU.mult, r=[], w=[dz])
        mk.tt("pool", z, z, self.lnbc[:, 2 * j, :], ALU.mult, r=[self.d_lnbc], w=[dz])
        mk.tt("pool", z, z, self.lnbc[:, 2 * j + 1, :], ALU.add, r=[self.d_lnbc], w=[dz])
        mk.dma("sp", dst, z, r=[dz], w=[d_dst])

    def dbg(self, name, ap, deps, dtype=F32):
        if not self.test_mode:
            return
        shape = list(ap.shape)
        t = self.nc.dram_tensor("dbg_" + name, shape, dtype, kind="ExternalOutput").ap()
        self.mk.dma("sp", t, ap, r=list(deps), w=[Dep()])
        self.dbg_names.append("dbg_" + name)

    def set_src_xres(self, tiles):
        for tt in tiles:
            self.src[tt] = self.xres[tt * 128:(tt + 1) * 128, :]

    def load_w_bf16(self, dst, d_dst, src_ap):
        self.mk.dma("pool", dst, src_ap, w=[d_dst])

    def mixer_conv(self, i, ctx_out):
        mk, A, W = self.mk, self.A, self.W
        ntiles = NT
        m0 = A.mark()
        hT = A.alloc((8, T), BF16)
        d_hT = [Dep() for _ in range(NT)]
        for tt in range(NT):
            self.prep_tile(tt, 0, hT, d_hT[tt], tt * 128)
        self.dbg("hT", hT, d_hT, BF16)
        self.dbg("modF", self.modF, [self.d_modF])
        self.dbg("modbc", self.modbc, [self.d_modbc])
        gT_d = self.nc.dram_tensor("conv_gT", [D, T], BF16).ap()
        d_gTd = [Dep() for _ in range(8)]
        wc = A.alloc((8, 3), F32)
        d_wc = Dep()
        wtmp = A.alloc((8,), F32)
        d_wtmp = Dep()
        for k in range(3):
            self.load_featmajor(wtmp, d_wtmp, W["conv_w"][0, k], 8)
            mk.copy("dve", wc[:, :, k], wtmp, r=[d_wtmp], w=[d_wc])
        wj = [A.alloc((3, 8, 128), BF16) for _ in range(2)]
        d_wj = [Dep() for _ in range(2)]
        LP = SEQ + 2 + CTX + 2
        cu = A.alloc((LP,), F32)
        d_cu = Dep()
        bsb = A.alloc((T,), F32)
        d_bsb = Dep()
        t1 = A.alloc((SEQ,), F32)
        d_t1 = Dep()
        gj = [A.alloc((T,), BF16)]
        d_gj = [Dep()]
        csb = [A.alloc((512,), F32) for _ in range(2)]
        d_csb = [Dep() for _ in range(2)]
        mk.memset("dve", cu, 0.0, w=[d_cu])
        win = W["conv_in_w"][0].rearrange("(k p) n -> p k n", p=128)
        tts = [(q * 512, 512, 1 + q * 512) for q in range(8)] + [(SEQ, CTX, SEQ + 3)]
        ci = 0
        for j in range(8):
            wjb, dwj = wj[j % 2], d_wj[j % 2]
            for s in range(3):
                self.load_w_bf16(wjb[:, s], dwj, win[:, :, s * 1024 + j * 128:s * 1024 + (j + 1) * 128])
            for (t0, n, co) in tts:
                pss = []
                for s in range(3):
                    ps, dps = mk.next_ps()
                    for kk in range(8):
                        mk.mm(ps[:, 0:n], wjb[:, s, kk, :], hT[:, kk, t0:t0 + n], kk == 0, kk == 7,
                              r=[dwj] + d_hT[t0 // 128:(t0 + n) // 128], w=[dps])
                    pss.append((ps, dps))
                cb, dcb = csb[ci % 2], d_csb[ci % 2]
                ci += 1
                mk.copy("act", bsb[:, t0:t0 + n], pss[0][0][:, 0:n], r=[pss[0][1]], w=[d_bsb])
                mk.copy("act", cb[:, 0:n], pss[1][0][:, 0:n], r=[pss[1][1]], w=[dcb])
                mk.tt("dve", cu[:, co:co + n], cb[:, 0:n], pss[2][0][:, 0:n], ALU.mult, r=[dcb, pss[2][1]], w=[d_cu])
            g, dg = gj[0], d_gj[0]
            for (s0, L, c0) in ((0, SEQ, 0), (SEQ, CTX, SEQ + 2)):
                tv = t1[:, 0:L]
                mk.ts("dve", tv, cu[:, c0 + 1:c0 + 1 + L], wc[:, j, 1:2], None, ALU.mult, r=[d_cu, d_wc], w=[d_t1])
                mk.stt("dve", tv, cu[:, c0:c0 + L], wc[:, j, 0:1], tv, ALU.mult, ALU.add, r=[d_cu], w=[d_t1])
                mk.stt("dve", tv, cu[:, c0 + 2:c0 + 2 + L], wc[:, j, 2:3], tv, ALU.mult, ALU.add, r=[d_cu], w=[d_t1])
                mk.tt("dve", g[:, s0:s0 + L], tv, bsb[:, s0:s0 + L], ALU.mult, r=[d_bsb], w=[dg])
            mk.dma("sp", gT_d[j * 128:(j + 1) * 128, :], g, r=[dg], w=[d_gTd[j]])
        A.release(m0)
        m0 = A.mark()
        gT = A.alloc((8, T), BF16)
        d_gT = Dep()
        for j in range(8):
            mk.dma("sp", gT[:, j, :], gT_d[j * 128:(j + 1) * 128, :], r=[d_gTd[j]], w=[d_gT])
        wo = A.alloc((8, 1024), BF16)
        d_wo = Dep()
        self.load_w_bf16(wo, d_wo, W["conv_out_w"][0].rearrange("(k p) n -> p k n", p=128))
        self.dbg("gT", gT, [d_gT], BF16)
        for tt in range(NT):
            ys, dys = [], []
            for hf in range(2):
                ps, dps = mk.next_ps()
                for j in range(8):
                    mk.mm(ps[:, :], gT[:, j, tt * 128:(tt + 1) * 128], wo[:, j, hf * 512:(hf + 1) * 512], j == 0, j == 7,
                          r=[d_gT, d_wo], w=[dps])
                ys.append(ps[:, :])
                dys.append(dps)
            self.post_tile(tt, ys, dys, 0, self.xres[tt * 128:(tt + 1) * 128, :], self.xdep[tt])
        self.set_src_xres(range(NT))
        A.release(m0)


    def mixer_attn(self, i, kind, ctx_out):
        mk, A, W, C = self.mk, self.A, self.W, self.C
        nc = self.nc
        swa = (kind == 1)
        qtiles = list(range(NT)) if ctx_out else list(range(NTL))
        if swa:
            wqkv, bqkv = W["swa_qkv_w"][0], W["swa_qkv_b"][0]
            nkc, nvh, vd = 4, 4, 64
            wo_ap, kcol0, vcol0 = W["swa_out_w"][0], 1024, 1280
        else:
            wqkv, bqkv = W["diff_qkv_w"][0], None
            nkc, nvh, vd = 8, 8, 128
            wo_ap, kcol0, vcol0 = W["diff_out_w"][0], 1024, 2048
        va = vd + 1
        qT_d = nc.dram_tensor(f"att{i}_qT", [1024, T], BF16).ap()
        kT_d = nc.dram_tensor(f"att{i}_kT", [nkc * 128, T], BF16).ap()
        v_d = nc.dram_tensor(f"att{i}_v", [T, nvh * va], BF16).ap()
        if self.test_mode:
            o_d = nc.dram_tensor(f"dbg_att{i}_o", [T, 1024], BF16, kind="ExternalOutput").ap()
            self.dbg_names.append(f"dbg_att{i}_o")
            qT_dbg = nc.dram_tensor(f"dbg_att{i}_qT", [1024, T], BF16, kind="ExternalOutput").ap()
            self.dbg_names.append(f"dbg_att{i}_qT")
        else:
            o_d = nc.dram_tensor(f"att{i}_o", [T, 1024], BF16).ap()
        d_qd = [Dep() for _ in range(8)]
        d_kd = [Dep() for _ in range(nkc)]
        d_vd = [Dep() for _ in range(NT)]
        d_od = [Dep() for _ in range(NT)]
        wv = wqkv.rearrange("(k p) n -> p k n", p=128)
        m0 = A.mark()
        hT = A.alloc((8, T), BF16)
        d_hT = [Dep() for _ in range(NT)]
        for tt in range(NT):
            self.prep_tile(tt, 0, hT, d_hT[tt], tt * 128)
        cosT = A.alloc((SEQ,), F32)
        sinT = A.alloc((SEQ,), F32)
        rotm = A.alloc((128,), F32)
        d_tab = Dep()
        mk.dma("sp", cosT, C["cosT"][:, :], w=[d_tab])
        mk.dma("sp", sinT, C["sinT"][:, :], w=[d_tab])
        mk.dma("sp", rotm, C["rotm"][:, :], w=[d_tab])
        bF = A.alloc((16,), F32)
        d_bF = Dep()
        if swa:
            self.load_featmajor(bF[:, 0:12], d_bF, bqkv, 12)
            bkd = A.alloc((4,), F32)
            d_bkd = Dep()
            for g in range(4):
                for hh in range(2):
                    mk.dma("sp", bkd[hh * 64:(hh + 1) * 64, g:g + 1],
                           bqkv[1024 + g * 64:1024 + (g + 1) * 64].rearrange("(p o) -> p o", o=1), w=[d_bkd])
        else:
            mk.memset("dve", bF, 0.0, w=[d_bF])
        wc = [A.alloc((8, 128), BF16) for _ in range(2)]
        d_wc = [Dep() for _ in range(2)]
        qsb = [A.alloc((512,), F32) for _ in range(2)]
        d_qsb = [Dep() for _ in range(2)]
        t1 = [A.alloc((512,), F32) for _ in range(2)]
        d_t1 = [Dep() for _ in range(2)]
        och = [A.alloc((T,), BF16) for _ in range(2)]
        d_och = [Dep() for _ in range(2)]
        tts = [(q * 512, 512) for q in range(8)] + [(SEQ, CTX)]
        chunks = []
        for c in range(8):
            chunks.append(("q", c, qT_d[c * 128:(c + 1) * 128, :], d_qd[c], [(c * 128, 128, 0)], bF[:, c:c + 1]))
        if swa:
            for g in range(4):
                chunks.append(("k", g, kT_d[g * 128:(g + 1) * 128, :], d_kd[g],
                               [(kcol0 + g * 64, 64, 0), (kcol0 + g * 64, 64, 64)], bkd[:, g:g + 1]))
        else:
            for c in range(8):
                chunks.append(("k", c, kT_d[c * 128:(c + 1) * 128, :], d_kd[c], [(kcol0 + c * 128, 128, 0)], bF[:, 8:9]))
        ri = 0
        for ci, (nm, c, dst, d_dst, wcols, bias) in enumerate(chunks):
            wcb, dwc = wc[ci % 2], d_wc[ci % 2]
            for (c0, ncol, o0) in wcols:
                self.load_w_bf16(wcb[:, :, o0:o0 + ncol], dwc, wv[:, :, c0:c0 + ncol])
            ob, dob = och[ci % 2], d_och[ci % 2]
            for (t0, n) in tts:
                ps, dps = mk.next_ps()
                for kk in range(8):
                    mk.mm(ps[:, 0:n], wcb[:, kk, :], hT[:, kk, t0:t0 + n], kk == 0, kk == 7,
                          r=[dwc] + d_hT[t0 // 128:(t0 + n) // 128], w=[dps])
                if t0 >= SEQ:
                    mk.act(ob[:, t0:t0 + n], ps[:, 0:n], AF.Identity, bias=bias, scale=1.0,
                           r=[dps, d_bF] + ([d_bkd] if swa else []), w=[dob])
                    continue
                rb = ri % 2
                ri += 1
                q_, dq_, t_, dt_ = qsb[rb], d_qsb[rb], t1[rb], d_t1[rb]
                mk.act(q_[:, 0:n], ps[:, 0:n], AF.Identity, bias=bias, scale=1.0,
                       r=[dps, d_bF] + ([d_bkd] if swa else []), w=[dq_])
                ps2, dps2 = mk.next_ps()
                mk.mm(ps2[:, 0:n], rotm, q_[:, 0:n], True, True, r=[d_tab, dq_], w=[dps2])
                mk.tt("dve", t_[:, 0:n], ps2[:, 0:n], sinT[:, t0:t0 + n], ALU.mult, r=[dps2, d_tab], w=[dt_])
                mk.tt("pool", q_[:, 0:n], q_[:, 0:n], cosT[:, t0:t0 + n], ALU.mult, r=[d_tab], w=[dq_])
                mk.tt("dve", ob[:, t0:t0 + n], q_[:, 0:n], t_[:, 0:n], ALU.add, r=[dq_, dt_], w=[dob])
            mk.dma("sp", dst, ob, r=[dob], w=[d_dst])
            if self.test_mode and nm == "q":
                mk.dma("sp", qT_dbg[c * 128:(c + 1) * 128, :], ob, r=[dob], w=[Dep()])
        nvc = nvh * vd
        wvb = A.alloc((8, nvc), BF16)
        d_wvb = Dep()
        self.load_w_bf16(wvb, d_wvb, wv[:, :, vcol0:vcol0 + nvc])
        vbias = None
        if swa:
            vbias = A.alloc((nvc,), F32)
            d_vbias = Dep()
            mk.dma("sp", vbias, bqkv[vcol0:vcol0 + nvc].partition_broadcast(128), w=[d_vbias])
        vt = [A.alloc((nvh, va), BF16) for _ in range(2)]
        d_vt = [Dep() for _ in range(2)]
        for b in range(2):
            mk.memset("dve", vt[b], 1.0, w=[d_vt[b]])
        for tt in range(NT):
            vb, dvb = vt[tt % 2], d_vt[tt % 2]
            for c0 in range(0, nvc, 512):
                ncol = min(512, nvc - c0)
                ps, dps = mk.next_ps()
                for kk in range(8):
                    mk.mm(ps[:, 0:ncol], hT[:, kk, tt * 128:(tt + 1) * 128], wvb[:, kk, c0:c0 + ncol], kk == 0, kk == 7,
                          r=[d_wvb, d_hT[tt]], w=[dps])
                h0, nh = c0 // vd, ncol // vd
                src = ps[:, 0:ncol].rearrange("p (h d) -> p h d", d=vd)
                if swa:
                    mk.tt("dve", vb[:, h0:h0 + nh, 0:vd], src,
                          vbias[:, c0:c0 + ncol].rearrange("p (h d) -> p h d", d=vd), ALU.add,
                          r=[dps, d_vbias], w=[dvb])
                else:
                    mk.copy("act", vb[:, h0:h0 + nh, 0:vd], src, r=[dps], w=[dvb])
            mk.dma("sp", v_d[tt * 128:(tt + 1) * 128, :].rearrange("p (h d) -> p h d", d=va), vb, r=[dvb], w=[d_vd[tt]])
        A.release(m0)
        import os as _os
        if _os.environ.get("KSTOP") == "p1":
            return
        m0 = A.mark()
        SCALE = 0.125
        S_BANKS, O_BANKS = [0, 1, 2, 3], [4, 5, 6, 7]
        qt = [A.alloc((8, 512), BF16) for _ in range(2)]
        d_qt = [Dep() for _ in range(2)]
        et = [A.alloc((512,), BF16) for _ in range(4)]
        d_et = [Dep() for _ in range(4)]
        ot = [A.alloc((1024,), BF16) for _ in range(2)]
        d_ot = [Dep() for _ in range(2)]
        sm = [A.alloc((8,), F32) for _ in range(4)]
        d_sm = [Dep() for _ in range(4)]
        ei = 0
        si = 0
        if swa:
            kT = A.alloc((4, T), BF16)
            d_kT = Dep()
            for g in range(4):
                mk.dma("sp", kT[:, g, :], kT_d[g * 128:(g + 1) * 128, :], r=[d_kd[g]], w=[d_kT])
            vv = A.alloc((NT, nvh, va), BF16)
            d_vv = Dep()
            for tt in range(NT):
                mk.dma("sp", vv[:, tt], v_d[tt * 128:(tt + 1) * 128, :].rearrange("p (h d) -> p h d", d=va),
                       r=[d_vd[tt]], w=[d_vv])
            mprev = A.alloc((512,), BF16)
            mnext = A.alloc((512,), BF16)
            d_msk = Dep()
            mk.dma("pool", mprev, C["mprev"][:, :], w=[d_msk])
            mk.dma("pool", mnext, C["mnext"][:, :], w=[d_msk])
            esink = A.alloc((16,), F32)
            d_esink = Dep()
            mk.dma("sp", esink, W["swa_sink"][0].partition_broadcast(128), w=[d_esink])
            mk.act(esink, esink, AF.Exp, r=[d_esink], w=[d_esink])
            for qi_, qb in enumerate(qtiles):
                b = qi_ % 2
                q_, dq_ = qt[b], d_qt[b]
                mk.dma("sp", q_[:, :, 0:128], qT_d.rearrange("(c p) t -> p c t", p=128)[:, :, qb * 128:(qb + 1) * 128],
                       r=d_qd, w=[dq_])
                o_, do_ = ot[b], d_ot[b]
                if qb < NTL:
                    keys = [(kt_, m_) for (kt_, m_) in ((qb - 1, "p"), (qb, None), (qb + 1, "n")) if 0 <= kt_ < NTL]
                    keys += [(NTL, None), (NTL + 1, None)]
                else:
                    keys = [(NTL, None), (NTL + 1, None)]
                for g in range(4):
                    pso, dpso = mk.ps_rot("o", O_BANKS)
                    for ki, (kt_, m_) in enumerate(keys):
                        e_, de_ = et[ei % 4], d_et[ei % 4]
                        ei += 1
                        for ph in range(2):
                            pss, dpss = mk.ps_rot("s", S_BANKS)
                            for c2 in range(2):
                                mk.mm(pss[:, c2 * 128:(c2 + 1) * 128],
                                      kT[ph * 64:(ph + 1) * 64, g, kt_ * 128:(kt_ + 1) * 128],
                                      q_[ph * 64:(ph + 1) * 64, 2 * g + c2, 0:128], True, True,
                                      r=[d_kT, dq_], w=[dpss])
                            mk.act(e_[:, ph * 256:(ph + 1) * 256], pss[:, 0:256], AF.Exp, scale=SCALE, r=[dpss], w=[de_])
                        if m_ is not None:
                            mk.tt("pool", e_, e_, mprev if m_ == "p" else mnext, ALU.mult, r=[d_msk], w=[de_])
                        for hh in range(4):
                            col = (hh % 2) * 256 + (hh // 2) * 128
                            mk.mm(pso[:, hh * va:(hh + 1) * va], e_[:, col:col + 128], vv[:, kt_, g, :],
                                  ki == 0 and hh == 0, ki == len(keys) - 1, r=[de_, d_vv], w=[dpso], sgc=True)
                    s_, ds_ = sm[si % 4], d_sm[si % 4]
                    si += 1
                    for hh in range(4):
                        hq = 4 * g + hh
                        mk.ts("dve", s_[:, hh:hh + 1], pso[:, hh * va + vd:hh * va + va], esink[:, hq:hq + 1], None, ALU.add,
                              r=[dpso, d_esink], w=[ds_])
                    mk.op("dve", lambda E, s_=s_: E.reciprocal(s_[:, 4:8], s_[:, 0:4]), r=[ds_], w=[ds_])
                    for hh in range(4):
                        hq = 4 * g + hh
                        mk.ts("dve", o_[:, hq * 64:(hq + 1) * 64], pso[:, hh * va:hh * va + vd], s_[:, 4 + hh:5 + hh], None,
                              ALU.mult, r=[dpso, ds_], w=[do_])
                mk.dma("sp", o_d[qb * 128:(qb + 1) * 128, :], o_, r=[do_], w=[d_od[qb]])
        else:
            lam_init = 0.8 - 0.6 * math.exp(-0.3 * i)
            lp = A.alloc((256,), F32)
            d_lp = Dep()
            mk.dma("sp", lp, W["diff_lambda"][0].rearrange("a b -> (a b)").partition_broadcast(128), w=[d_lp])
            lam = A.alloc((8,), F32)
            mk.tt("dve", lp[:, 0:64], lp[:, 0:64], lp[:, 64:128], ALU.mult, r=[d_lp], w=[d_lp])
            mk.tt("dve", lp[:, 128:192], lp[:, 128:192], lp[:, 192:256], ALU.mult, r=[d_lp], w=[d_lp])
            mk.op("dve", lambda E: E.reduce_sum(lam[:, 0:1], lp[:, 0:64], mybir.AxisListType.X), r=[d_lp], w=[d_lp])
            mk.op("dve", lambda E: E.reduce_sum(lam[:, 1:2], lp[:, 128:192], mybir.AxisListType.X), r=[d_lp], w=[d_lp])
            mk.act(lam[:, 0:2], lam[:, 0:2], AF.Exp, r=[d_lp], w=[d_lp])
            mk.tt("dve", lam[:, 2:3], lam[:, 1:2], lam[:, 0:1], ALU.subtract, r=[d_lp], w=[d_lp])
            mk.ts("dve", lam[:, 3:4], lam[:, 2:3], -lam_init, None, ALU.add, r=[d_lp], w=[d_lp])
            gsub = A.alloc((128,), F32)
            d_gsub = Dep()
            mk.dma("sp", gsub, W["diff_subln_g"][0].partition_broadcast(128), w=[d_gsub])
            mk.ts("dve", gsub, gsub, 1.0 - lam_init, None, ALU.mult, r=[d_gsub], w=[d_gsub])
            kT = A.alloc((4, T), BF16)
            d_kT = Dep()
            vv = A.alloc((NT, 4, va), BF16)
            d_vv = Dep()
            o0 = [A.alloc((4, 128), F32) for _ in range(2)]
            d_o0 = [Dep() for _ in range(2)]
            junk = A.alloc((128,), F32)
            otd = [A.alloc((4, 512), BF16) for _ in range(2)]
            qtl = [(q * 4, 4) for q in range(8)] + ([(NTL, 2)] if ctx_out else [])
            oi = 0
            for hg in range(2):
                for c in range(4):
                    mk.dma("sp", kT[:, c, :], kT_d[(hg * 4 + c) * 128:(hg * 4 + c + 1) * 128, :], r=[d_kd[hg * 4 + c]], w=[d_kT])
                for tt in range(NT):
                    mk.dma("sp", vv[:, tt], v_d[tt * 128:(tt + 1) * 128, hg * 4 * va:(hg + 1) * 4 * va].rearrange(
                        "p (h d) -> p h d", d=va), r=[d_vd[tt]], w=[d_vv])
                for qi_, (qa, nq4) in enumerate(qtl):
                    nq = nq4 * 128
                    b = qi_ % 2
                    q_, dq_ = qt[b], d_qt[b]
                    mk.dma("sp", q_[:, 0:4, 0:nq],
                           qT_d.rearrange("(c p) t -> p c t", p=128)[:, hg * 4:hg * 4 + 4, qa * 128:qa * 128 + nq],
                           r=d_qd, w=[dq_])
                    o_, do_ = otd[b], d_ot[b]
                    keys = list(range(NT)) if qa < NTL else [NTL, NTL + 1]
                    for pr in range(4):
                        ob_, dob_ = o0[oi % 2], d_o0[oi % 2]
                        oi += 1
                        for ii in range(2):
                            psoA, dpsoA = mk.ps_rot("o", O_BANKS)
                            psoB, dpsoB = mk.ps_rot("o", O_BANKS)
                            for ki, kt_ in enumerate(keys):
                                pss, dpss = mk.ps_rot("s", S_BANKS)
                                mk.mm(pss[:, 0:nq], kT[ii * 64:(ii + 1) * 64, pr, kt_ * 128:(kt_ + 1) * 128],
                                      q_[ii * 64:(ii + 1) * 64, pr, 0:nq], True, True, r=[d_kT, dq_], w=[dpss])
                                e_, de_ = et[ei % 4], d_et[ei % 4]
                                ei += 1
                                mk.act(e_[:, 0:nq], pss[:, 0:nq], AF.Exp, scale=SCALE, r=[dpss], w=[de_])
                                for s4 in range(nq4):
                                    pb, dpb = (psoA, dpsoA) if s4 < 2 else (psoB, dpsoB)
                                    co = (s4 % 2) * 256
                                    mk.mm(pb[:, co:co + va], e_[:, s4 * 128:(s4 + 1) * 128], vv[:, kt_, pr, :],
                                          ki == 0 and s4 % 2 == 0, ki == len(keys) - 1, r=[de_, d_vv], w=[dpb], sgc=True)
                            for s4 in range(nq4):
                                pb, dpb = (psoA, dpsoA) if s4 < 2 else (psoB, dpsoB)
                                co = (s4 % 2) * 256
                                s_, ds_ = sm[si % 4], d_sm[si % 4]
                                si += 1
                                mk.op("dve", lambda E, s_=s_, pb=pb, co=co: E.reciprocal(s_[:, 0:1], pb[:, co + vd:co + va]),
                                      r=[dpb], w=[ds_])
                                if ii == 0:
                                    mk.ts("dve", ob_[:, s4, :], pb[:, co:co + vd], s_[:, 0:1], None, ALU.mult,
                                          r=[dpb, ds_], w=[dob_])
                                else:
                                    mk.ts("dve", s_[:, 1:2], s_[:, 0:1], lam[:, 3:4], None, ALU.mult, r=[ds_, d_lp], w=[ds_])
                                    mk.stt("dve", ob_[:, s4, :], pb[:, co:co + vd], s_[:, 1:2], ob_[:, s4, :], ALU.mult, ALU.add,
                                           r=[dpb, ds_, dob_], w=[dob_])
                                    mk.act(junk, ob_[:, s4, :], AF.Square, r=[dob_], w=[ds_], accum_out=s_[:, 2:3])
                                    mk.ts("dve", s_[:, 3:4], s_[:, 2:3], 1.0 / 128.0, RMS_EPS, ALU.mult, ALU.add, r=[ds_], w=[ds_])
                                    mk.act(s_[:, 3:4], s_[:, 3:4], AF.Sqrt, r=[ds_], w=[ds_])
                                    mk.op("dve", lambda E, s_=s_: E.reciprocal(s_[:, 4:5], s_[:, 3:4]), r=[ds_], w=[ds_])
                                    mk.stt("dve", o_[:, s4, pr * 128:(pr + 1) * 128], ob_[:, s4, :], s_[:, 4:5], gsub,
                                           ALU.mult, ALU.mult, r=[dob_, ds_, d_gsub], w=[do_])
                    for s4 in range(nq4):
                        tq = qa + s4
                        mk.dma("sp", o_d[tq * 128:(tq + 1) * 128, hg * 512:(hg + 1) * 512], o_[:, s4, :],
                               r=[do_], w=[d_od[tq]])
        A.release(m0)
        if _os.environ.get("KSTOP") == "p2":
            return
        m0 = A.mark()
        wo = A.alloc((8, 1024), BF16)
        d_wo = Dep()
        self.load_w_bf16(wo, d_wo, wo_ap.rearrange("(k p) n -> p k n", p=128))
        ybias, d_yb = None, None
        if swa:
            ybias = A.alloc((1024,), F32)
            d_yb = Dep()
            mk.dma("sp", ybias, W["swa_out_b"][0].partition_broadcast(128), w=[d_yb])
        oin = [A.alloc((1024,), BF16) for _ in range(2)]
        d_oin = [Dep() for _ in range(2)]
        oT = [A.alloc((8, 128), BF16) for _ in range(2)]
        d_oT = [Dep() for _ in range(2)]
        for qi_, tt in enumerate(qtiles):
            b = qi_ % 2
            mk.dma("sp", oin[b], o_d[tt * 128:(tt + 1) * 128, :], r=[d_od[tt]], w=[d_oin[b]])
            ps, dps = mk.next_ps()
            psb = ps.bitcast(BF16)
            for k in range(8):
                mk.tr(psb[:, k * 128:(k + 1) * 128], oin[b][:, k * 128:(k + 1) * 128], self.identb,
                      r=[d_oin[b], self.d_identb], w=[dps])
            mk.copy("act", oT[b], psb[:, 0:1024].rearrange("p (k t) -> p k t", k=8), r=[dps], w=[d_oT[b]])
            ys, dys = [], []
            for hf in range(2):
                ps2, dps2 = mk.next_ps()
                for k in range(8):
                    mk.mm(ps2[:, :], oT[b][:, k, :], wo[:, k, hf * 512:(hf + 1) * 512], k == 0, k == 7,
                          r=[d_oT[b], d_wo], w=[dps2])
                ys.append(ps2[:, :])
                dys.append(dps2)
            self.post_tile(tt, ys, dys, 0, self.xres[tt * 128:(tt + 1) * 128, :], self.xdep[tt], ybias=ybias, d_ybias=d_yb)
        self.set_src_xres(qtiles)
        A.release(m0)

    class _Pool:
        def __init__(self, A, shape, dtype, n):
            self.t = [A.alloc(shape, dtype) for _ in range(n)]
            self.d = [Dep() for _ in range(n)]
            self.i = 0

        def get(self):
            k = self.i % len(self.t)
            self.i += 1
            return self.t[k], self.d[k]

    def mixer_delta(self, i):
        mk, A, W, C = self.mk, self.A, self.W, self.C
        nc = self.nc
        P = Prog._Pool
        def _dt(name, shape, dtype):
            if self.test_mode:
                self.dbg_names.append("dbg_" + name)
                return nc.dram_tensor("dbg_" + name, shape, dtype, kind="ExternalOutput").ap()
            return nc.dram_tensor(name, shape, dtype).ap()
        qT_d = _dt("dn_qT", [1024, T], BF16)
        kT_d = _dt("dn_kT", [1024, T], BF16)
        k_d = _dt("dn_k", [T, 1024], BF16)
        v_d = _dt("dn_v", [T, 2048], BF16)
        z_d = _dt("dn_z", [SEQ, 2048], BF16)
        of_d = _dt("dn_of", [SEQ, 2048], F32)
        d_qTd = [Dep() for _ in range(8)]
        d_kTd = [Dep() for _ in range(8)]
        d_kd = [Dep() for _ in range(8)]
        d_vd = [Dep() for _ in range(16)]
        d_zd = [Dep() for _ in range(NTL)]
        d_ofd = [Dep() for _ in range(NTL)]
        gb = A.alloc((NT, 64), F32)
        d_gb = [Dep() for _ in range(NT)]
        wsrc = W["delta_qkvz_w"][0].rearrange("(k p) n -> p k n", p=128)
        m0 = A.mark()
        hT = A.alloc((8, T), BF16)
        d_hT = [Dep() for _ in range(NT)]
        m1 = A.mark()
        h32 = A.alloc((8, 128), F32)
        d_h32 = Dep()
        wba = A.alloc((8, 64), F32)
        d_wba = Dep()
        mk.dma("sp", wba, W["delta_ba_w"][0].rearrange("(k p) n -> p k n", p=128), w=[d_wba])
        dtb = A.alloc((32,), F32)
        nea = A.alloc((32,), F32)
        d_cst = Dep()
        mk.dma("sp", dtb, W["delta_dt_bias"][0].rearrange("d h -> (d h)").partition_broadcast(128), w=[d_cst])
        mk.dma("sp", nea, W["delta_a_log"][0].rearrange("d h -> (d h)").partition_broadcast(128), w=[d_cst])
        mk.act(nea, nea, AF.Exp, r=[d_cst], w=[d_cst])
        mk.ts("dve", nea, nea, -1.0, None, ALU.mult, r=[d_cst], w=[d_cst])
        v3 = lambda ap: ap.rearrange("p (d h) -> p d h", d=2)
        for tt in range(NT):
            self.prep_tile(tt, 0, hT, d_hT[tt], tt * 128, h32=h32, d_h32=d_h32)
            ps, dps = mk.next_ps()
            for kk in range(8):
                mk.mm(ps[:, 0:64], h32[:, kk, :], wba[:, kk, :], kk == 0, kk == 7, r=[d_h32, d_wba], w=[dps])
            bav = ps[:, 0:64].rearrange("p (d s h) -> p d s h", d=2, s=2)
            gv = v3(gb[:, tt, 0:32])
            mk.tt("dve", gv, bav[:, :, 1, :], v3(dtb), ALU.add, r=[dps, d_cst], w=[d_gb[tt]])
            mk.act(gb[:, tt, 0:32], gb[:, tt, 0:32], AF.Exp, r=[d_gb[tt]], w=[d_gb[tt]])
            mk.act(gb[:, tt, 0:32], gb[:, tt, 0:32], AF.Ln, bias=self.ones[:, 0:1], r=[d_gb[tt], self.d_ones], w=[d_gb[tt]])
            mk.tt("dve", gb[:, tt, 0:32], gb[:, tt, 0:32], nea, ALU.mult, r=[d_cst], w=[d_gb[tt]])
            mk.act(v3(gb[:, tt, 32:64]), bav[:, :, 0, :], AF.Sigmoid, r=[dps], w=[d_gb[tt]])
        self.dbg("dn_gb", gb, d_gb)
        A.release(m1)
        m1 = A.mark()
        wc5 = A.alloc((32, 5), F32)
        d_wc5 = Dep()
        wtmp = A.alloc((32,), F32)
        d_wtmp = Dep()
        for k in range(5):
            self.load_featmajor(wtmp, d_wtmp, W["delta_conv_w"][0, k], 32)
            mk.copy("dve", wc5[:, :, k], wtmp, r=[d_wtmp], w=[d_wc5])
        onesb = A.alloc((128,), BF16)
        d_onesb = Dep()
        mk.memset("dve", onesb, 1.0, w=[d_onesb])
        LP = SEQ + 4 + CTX + 4
        pbuf = A.alloc((LP,), F32)
        d_pbuf = Dep()
        mk.memset("dve", pbuf, 0.0, w=[d_pbuf])
        acc = A.alloc((T,), F32)
        d_acc = Dep()
        sq = A.alloc((T,), BF16)
        d_sq = Dep()
        obp = P(A, (T,), BF16, 2)
        wcp = P(A, (8, 128), BF16, 2)
        rnp = P(A, (512,), F32, 2)
        stp = P(A, (8, 128), BF16, 2)
        tts = [(q * 512, 512, 2 + q * 512) for q in range(8)] + [(SEQ, CTX, SEQ + 6)]
        QS = 128.0 ** -0.5
        for c in range(32):
            wcb, dwc = wcp.get()
            self.load_w_bf16(wcb, dwc, wsrc[:, :, c * 128:(c + 1) * 128])
            for (t0, n, co) in tts:
                ps, dps = mk.next_ps()
                for kk in range(8):
                    mk.mm(ps[:, 0:n], wcb[:, kk, :], hT[:, kk, t0:t0 + n], kk == 0, kk == 7,
                          r=[dwc] + d_hT[t0 // 128:(t0 + n) // 128], w=[dps])
                mk.copy("act", pbuf[:, co:co + n], ps[:, 0:n], r=[dps], w=[d_pbuf])
            for (s0, L, base) in ((0, SEQ, 0), (SEQ, CTX, SEQ + 4)):
                for (a0, a1, eng) in ((0, L, "dve"),):
                    av = acc[:, s0 + a0:s0 + a1]
                    n_ = a1 - a0
                    mk.ts(eng, av, pbuf[:, base + a0:base + a0 + n_], wc5[:, c, 0:1], None, ALU.mult,
                          r=[d_pbuf, d_wc5], w=[d_acc])
                    for k in range(1, 5):
                        mk.stt(eng, av, pbuf[:, base + a0 + k:base + a0 + k + n_], wc5[:, c, k:k + 1], av, ALU.mult, ALU.add,
                               r=[d_pbuf, d_wc5], w=[d_acc])
            ob, dob = obp.get()
            if c < 16:
                mk.act(acc, acc, AF.Silu, r=[d_acc], w=[d_acc])
                mk.act(sq, acc, AF.Square, r=[d_acc], w=[d_sq])
                for (t0, n, co) in tts:
                    ps, dps = mk.next_ps()
                    mk.mm(ps[:, 0:n], onesb, sq[:, t0:t0 + n], True, True, r=[d_onesb, d_sq], w=[dps])
                    rn, drn = rnp.get()
                    mk.ts("dve", rn[:, 0:n], ps[:, 0:n], RMS_EPS, None, ALU.add, r=[dps], w=[drn])
                    mk.act(rn[:, 0:n], rn[:, 0:n], AF.Sqrt, r=[drn], w=[drn])
                    mk.op("dve", lambda E, rn=rn, n=n: E.reciprocal(rn[:, 0:n], rn[:, 0:n]), r=[drn], w=[drn])
                    mk.stt("dve", ob[:, t0:t0 + n], acc[:, t0:t0 + n], QS if c < 8 else 1.0, rn[:, 0:n], ALU.mult, ALU.mult,
                           r=[d_acc, drn], w=[dob])
                if c < 8:
                    mk.dma("sp", qT_d[c * 128:(c + 1) * 128, :], ob, r=[dob], w=[d_qTd[c]])
                else:
                    mk.dma("sp", kT_d[(c - 8) * 128:(c - 7) * 128, :], ob, r=[dob], w=[d_kTd[c - 8]])
            else:
                mk.act(ob, acc, AF.Silu, r=[d_acc], w=[dob])
            if c >= 8:
                dst, ddst, cc = (k_d, d_kd[c - 8], c - 8) if c < 16 else (v_d, d_vd[c - 16], c - 16)
                dview = dst.rearrange("(t p) f -> p t f", p=128)
                for t8 in range(0, NT, 8):
                    nt8 = min(8, NT - t8)
                    ps, dps = mk.next_ps()
                    psb = ps.bitcast(BF16)
                    for q in range(nt8):
                        mk.tr(psb[:, q * 128:(q + 1) * 128], ob[:, (t8 + q) * 128:(t8 + q + 1) * 128], self.identb,
                              r=[dob, self.d_identb], w=[dps])
                    st, dst_ = stp.get()
                    mk.copy("act", st[:, 0:nt8, :], psb[:, 0:nt8 * 128].rearrange("p (t f) -> p t f", f=128), r=[dps], w=[dst_])
                    mk.dma("sp", dview[:, t8:t8 + nt8, cc * 128:(cc + 1) * 128], st[:, 0:nt8, :], r=[dst_], w=[ddst])
        A.release(m1)
        m1 = A.mark()
        wz = A.alloc((8, 2048), BF16)
        d_wz = Dep()
        self.load_w_bf16(wz, d_wz, wsrc[:, :, 4096:6144])
        ngb = A.alloc((4, 128), F32)
        d_ngb = Dep()
        for q in range(4):
            mk.dma("sp", ngb[:, q, :], W["delta_norm_g"][0].partition_broadcast(128), w=[d_ngb])
        zsp = P(A, (512,), F32, 2)
        ztp = P(A, (2048,), BF16, 2)
        for tt in range(NTL):
            zt, dzt = ztp.get()
            for cg in range(4):
                ps, dps = mk.next_ps()
                for kk in range(8):
                    mk.mm(ps[:, :], hT[:, kk, tt * 128:(tt + 1) * 128], wz[:, kk, cg * 512:(cg + 1) * 512], kk == 0, kk == 7,
                          r=[d_wz, d_hT[tt]], w=[dps])
                zs, dzs = zsp.get()
                mk.act(zs, ps[:, :], AF.Silu, r=[dps], w=[dzs])
                mk.tt("dve", zt[:, cg * 512:(cg + 1) * 512], zs, ngb.rearrange("p a b -> p (a b)"), ALU.mult,
                      r=[dzs, d_ngb], w=[dzt])
            mk.dma("sp", z_d[tt * 128:(tt + 1) * 128, :], zt, r=[dzt], w=[d_zd[tt]])
        A.release(m0)
        m0 = A.mark()
        cst = {}
        d_c2 = Dep()
        for nm in ("triF", "triB", "m2F", "m2B", "selF", "selB"):
            cst[nm] = A.alloc((128,), F32)
            mk.dma("sp", cst[nm], C[nm][:, :], w=[d_c2])
        S = A.alloc((16, 128), F32)
        Sb = A.alloc((16, 128), BF16)
        d_S = [Dep() for _ in range(16)]
        wo = A.alloc((16, 1024), BF16)
        d_wo = Dep()
        self.load_w_bf16(wo, d_wo, W["delta_out_w"][0].rearrange("(k p) n -> p k n", p=128))
        ldq = P(A, (8, 128), BF16, 2)
        ldkT = P(A, (8, 128), BF16, 2)
        ldk = P(A, (8, 128), BF16, 2)
        ldv = P(A, (16, 128), BF16, 2)
        smp = P(A, (8, 16), F32, 2)
        kkp = P(A, (128,), F32, 2)
        qkp = P(A, (128,), F32, 2)
        bmp = P(A, (128,), F32, 2)
        dmp = P(A, (128,), F32, 2)
        dtp = P(A, (128,), F32, 2)
        abp = P(A, (128,), F32, 8)
        ttp = P(A, (128,), F32, 3)
        tbp = P(A, (128,), BF16, 2)
        vbp = P(A, (128,), BF16, 2)
        kbp = P(A, (128,), BF16, 2)
        usp = P(A, (128,), F32, 2)
        wtp = P(A, (128,), BF16, 2)
        vnp = P(A, (128,), BF16, 2)
        itp = P(A, (128,), BF16, 2)
        o1p = P(A, (128,), F32, 2)
        kdp = P(A, (128,), BF16, 2)
        otp = P(A, (16, 128), F32, 1)
        ofp = P(A, (16, 128), F32, 1)
        zgp = P(A, (16, 128), BF16, 1)
        onp = P(A, (16, 128), BF16, 1)
        oTp = P(A, (16, 128), BF16, 1)
        s16p = P(A, (4, 16), F32, 2)
        sqt = A.alloc((16, 128), F32)
        d_sqt = Dep()
        kTv = kT_d.rearrange("(h p) t -> p h t", p=128)
        qTv = qT_d.rearrange("(h p) t -> p h t", p=128)
        evi = [0]

        def evac(out, ps, dps, dout):
            evi[0] += 1
            mk.copy("act" if evi[0] % 2 == 0 else "dve", out, ps, r=[dps], w=[dout])

        for d in range(2):
            order = ([NTL, NTL + 1] + list(range(NTL))) if d == 0 else ([NTL + 1, NTL] + list(range(NTL - 1, -1, -1)))
            tri, m2, sel = (cst["triF"], cst["m2F"], cst["selF"]) if d == 0 else (cst["triB"], cst["m2B"], cst["selB"])
            mk.memset("dve", S, 0.0, w=d_S)
            mk.memset("dve", Sb, 0.0, w=d_S)
            for tt in order:
                lat = tt < NTL
                tok = slice(tt * 128, (tt + 1) * 128)
                kTt, dkTt = ldkT.get()
                mk.dma("sp", kTt, kTv[:, :, tok], r=d_kTd, w=[dkTt])
                kt, dkt = ldk.get()
                mk.dma("sp", kt, k_d[tok, :].rearrange("p (h f) -> p h f", f=128), r=d_kd, w=[dkt])
                vt, dvt = ldv.get()
                mk.dma("sp", vt, v_d[tok, :].rearrange("p (h f) -> p h f", f=128), r=d_vd, w=[dvt])
                if lat:
                    qTt, dqTt = ldq.get()
                    mk.dma("sp", qTt, qTv[:, :, tok], r=d_qTd, w=[dqTt])
                g_d = gb[:, tt, d * 16:(d + 1) * 16]
                b_d = gb[:, tt, 32 + d * 16:32 + (d + 1) * 16]
                sm_, dsm = smp.get()
                gc, gl, eg, egl, kds, negb, beg = [sm_[:, q, :] for q in range(7)]
                ps, dps = mk.next_ps()
                mk.mm(ps[:, 0:16], tri, g_d, True, True, r=[d_c2, d_gb[tt]], w=[dps])
                mk.copy("dve", gc, ps[:, 0:16], r=[dps], w=[dsm])
                ps, dps = mk.next_ps()
                mk.mm(ps[:, 0:16], sel, gc, True, True, r=[d_c2, dsm], w=[dps])
                mk.copy("dve", gl, ps[:, 0:16], r=[dps], w=[dsm])
                mk.act(eg, gc, AF.Exp, r=[dsm], w=[dsm])
                mk.act(egl, gl, AF.Exp, r=[dsm], w=[dsm])
                mk.tt("dve", kds, gl, gc, ALU.subtract, r=[dsm], w=[dsm])
                mk.act(kds, kds, AF.Exp, r=[dsm], w=[dsm])
                mk.ts("dve", negb, b_d, -1.0, None, ALU.mult, r=[d_gb[tt]], w=[dsm])
                mk.tt("dve", beg, b_d, eg, ALU.mult, r=[d_gb[tt], dsm], w=[dsm])
                if lat:
                    ot, dot = otp.get()
                    if d == 1:
                        oft, doft = ofp.get()
                        mk.dma("sp", oft, of_d[tok, :].rearrange("p (h f) -> p h f", f=128), r=[d_ofd[tt]], w=[doft])
                for hq in range(8):
                    ps, dps = mk.next_ps()
                    mk.mm(ps[:, 0:128], kTt[:, hq, :], kTt[:, hq, :], True, True, r=[dkTt], w=[dps])
                    kkm, dkkm = kkp.get()
                    mk.tt("dve", kkm, ps[:, 0:128], m2, ALU.mult, r=[dps, d_c2], w=[dkkm])
                    if lat:
                        ps, dps = mk.next_ps()
                        mk.mm(ps[:, 0:128], kTt[:, hq, :], qTt[:, hq, :], True, True, r=[dkTt, dqTt], w=[dps])
                        qkm, dqkm = qkp.get()
                        mk.tt("dve", qkm, ps[:, 0:128], tri, ALU.mult, r=[dps, d_c2], w=[dqkm])
                    for hv in (2 * hq, 2 * hq + 1):
                        bm, dbm = bmp.get()
                        mk.ts("pool", bm, m2, g_d[:, hv:hv + 1], None, ALU.mult, r=[d_c2, d_gb[tt]], w=[dbm])
                        ps, dps = mk.next_ps()
                        mk.mm(ps[:, 0:128], tri, bm, True, True, r=[d_c2, dbm], w=[dps])
                        dm, ddm = dmp.get()
                        mk.act(dm, ps[:, 0:128], AF.Exp, r=[dps], w=[ddm])
                        a_k, da_k = abp.get()
                        mk.stt("dve", a_k, kkm, negb[:, hv:hv + 1], dm, ALU.mult, ALU.mult, r=[dkkm, dsm, ddm], w=[da_k])
                        ps, dps = mk.next_ps()
                        mk.tr(ps[:, 0:128], a_k, self.ident, r=[da_k, self.d_ident], w=[dps])
                        b_k, db_k = abp.get()
                        evac(b_k, ps[:, 0:128], dps, db_k)
                        tT, dtT = ttp.get()
                        mk.tt("pool", tT, b_k, self.ident, ALU.add, r=[db_k, self.d_ident], w=[dtT])
                        for lvl in range(1, 7):
                            ps, dps = mk.next_ps()
                            mk.mm(ps[:, 0:128], b_k, a_k, True, True, r=[db_k, da_k], w=[dps])
                            a_n, da_n = abp.get()
                            evac(a_n, ps[:, 0:128], dps, da_n)
                            if lvl < 6:
                                ps, dps = mk.next_ps()
                                mk.mm(ps[:, 0:128], a_k, b_k, True, True, r=[db_k, da_k], w=[dps])
                                b_n, db_n = abp.get()
                                evac(b_n, ps[:, 0:128], dps, db_n)
                            ps, dps = mk.next_ps()
                            mk.mm(ps[:, 0:128], a_n, tT, True, True, r=[da_n, dtT], w=[dps])
                            tN, dtN = ttp.get()
                            mk.tt("dve", tN, ps[:, 0:128], tT, ALU.add, r=[dps, dtT], w=[dtN])
                            tT, dtT = tN, dtN
                            a_k, da_k = a_n, da_n
                            if lvl < 6:
                                b_k, db_k = b_n, db_n
                        tTf, dtTf = tT, dtT
                        tT, dtT = tbp.get()
                        mk.copy("act", tT, tTf, r=[dtTf], w=[dtT])
                        vb, dvb = vbp.get()
                        mk.ts("pool", vb, vt[:, hv, :], b_d[:, hv:hv + 1], None, ALU.mult, r=[dvt, d_gb[tt]], w=[dvb])
                        kb, dkb = kbp.get()
                        mk.ts("pool", kb, kt[:, hq, :], beg[:, hv:hv + 1], None, ALU.mult, r=[dkt, dsm], w=[dkb])
                        ps, dps = mk.next_ps()
                        mk.mm(ps[:, 0:128], tT, vb, True, True, r=[dtT, dvb], w=[dps])
                        us, dus = usp.get()
                        evac(us, ps[:, 0:128], dps, dus)
                        ps, dps = mk.next_ps()
                        mk.mm(ps[:, 0:128], kb, tT, True, True, r=[dkb, dtT], w=[dps])
                        wT, dwT = wtp.get()
                        evac(wT, ps[:, 0:128], dps, dwT)
                        ps, dps = mk.next_ps()
                        mk.mm(ps[:, 0:128], wT, Sb[:, hv, :], True, True, r=[dwT, d_S[hv]], w=[dps])
                        vn, dvn = vnp.get()
                        mk.tt("dve", vn, us, ps[:, 0:128], ALU.subtract, r=[dus, dps], w=[dvn])
                        if lat:
                            ps, dps = mk.next_ps()
                            mk.mm(ps[:, 0:128], bm, tri, True, True, r=[d_c2, dbm], w=[dps])
                            dT, ddT = dtp.get()
                            mk.act(dT, ps[:, 0:128], AF.Exp, r=[dps], w=[ddT])
                            it, dit = itp.get()
                            mk.tt("pool", it, qkm, dT, ALU.mult, r=[dqkm, ddT], w=[dit])
                            ps, dps = mk.next_ps()
                            mk.mm(ps[:, 0:128], qTt[:, hq, :], Sb[:, hv, :], True, True, r=[dqTt, d_S[hv]], w=[dps])
                            o1, do1 = o1p.get()
                            mk.ts("dve", o1, ps[:, 0:128], eg[:, hv:hv + 1], None, ALU.mult, r=[dps, dsm], w=[do1])
                            ps, dps = mk.next_ps()
                            mk.mm(ps[:, 0:128], it, vn, True, True, r=[dit, dvn], w=[dps])
                            if d == 0:
                                mk.tt("dve", ot[:, hv, :], ps[:, 0:128], o1, ALU.add, r=[dps, do1], w=[dot])
                            else:
                                mk.tt("dve", o1, ps[:, 0:128], o1, ALU.add, r=[dps], w=[do1])
                                mk.tt("pool", ot[:, hv, :], o1, oft[:, hv, :], ALU.add, r=[do1, doft], w=[dot])
                        kd, dkd = kdp.get()
                        mk.ts("pool", kd, kt[:, hq, :], kds[:, hv:hv + 1], None, ALU.mult, r=[dkt, dsm], w=[dkd])
                        ps, dps = mk.next_ps()
                        mk.mm(ps[:, 0:128], kd, vn, True, True, r=[dkd, dvn], w=[dps])
                        mk.stt("dve", S[:, hv, :], S[:, hv, :], egl[:, hv:hv + 1], ps[:, 0:128], ALU.mult, ALU.add,
                               r=[dps, dsm], w=[d_S[hv]])
                        mk.copy("act", Sb[:, hv, :], S[:, hv, :], r=[], w=[d_S[hv]])
                if lat and d == 0:
                    mk.dma("sp", of_d[tok, :].rearrange("p (h f) -> p h f", f=128), ot, r=[dot], w=[d_ofd[tt]])
                if lat and d == 1:
                    zg, dzg = zgp.get()
                    mk.dma("sp", zg, z_d[tok, :].rearrange("p (h f) -> p h f", f=128), r=[d_zd[tt]], w=[dzg])
                    s16, ds16 = s16p.get()
                    mk.tt("pool", sqt, ot, ot, ALU.mult, r=[dot], w=[d_sqt])
                    mk.op("dve", lambda E, s16=s16: E.reduce_sum(s16[:, 0, :], sqt, mybir.AxisListType.X), r=[d_sqt], w=[ds16])
                    mk.ts("dve", s16[:, 1, :], s16[:, 0, :], 1.0 / 128.0, RMS_EPS, ALU.mult, ALU.add, r=[ds16], w=[ds16])
                    mk.act(s16[:, 1, :], s16[:, 1, :], AF.Sqrt, r=[ds16], w=[ds16])
                    mk.op("dve", lambda E, s16=s16: E.reciprocal(s16[:, 2, :], s16[:, 1, :]), r=[ds16], w=[ds16])
                    on, don = onp.get()
                    for hv in range(16):
                        mk.stt("dve", on[:, hv, :], ot[:, hv, :], s16[:, 2, hv:hv + 1], zg[:, hv, :],
                               ALU.mult, ALU.mult, r=[dot, ds16, dzg], w=[don])
                    oT, doT = oTp.get()
                    for hf in range(2):
                        ps, dps = mk.next_ps()
                        psb = ps.bitcast(BF16)
                        for q in range(8):
                            mk.tr(psb[:, q * 128:(q + 1) * 128], on[:, hf * 8 + q, :], self.identb,
                                  r=[don, self.d_identb], w=[dps])
                        mk.copy("act", oT[:, hf * 8:(hf + 1) * 8, :], psb[:, 0:1024].rearrange("p (k t) -> p k t", k=8),
                                r=[dps], w=[doT])
                    ys, dys = [], []
                    for hf in range(2):
                        ps2, dps2 = mk.next_ps()
                        for k in range(16):
                            mk.mm(ps2[:, :], oT[:, k, :], wo[:, k, hf * 512:(hf + 1) * 512], k == 0, k == 15,
                                  r=[doT, d_wo], w=[dps2])
                        ys.append(ps2[:, :])
                        dys.append(dps2)
                    self.post_tile(tt, ys, dys, 0, self.xres[tt * 128:(tt + 1) * 128, :], self.xdep[tt])
        self.set_src_xres(range(NTL))
        A.release(m0)

    def moe(self, i, last, final):
        mk, A, W = self.mk, self.A, self.W
        ntile = NTL if last else NT
        GT = 9
        groups = [(0, 9), (9, 18), (18, 26), (26, 34)] if not last else [(0, 8), (8, 16), (16, 24), (24, 32)]
        m0 = A.mark()
        wr = A.alloc((8, 32), F32)
        d_wr = Dep()
        mk.dma("sp", wr, W["router_w"][i].rearrange("(k p) n -> p k n", p=128), w=[d_wr])
        brbc = A.alloc((32,), F32)
        d_br = Dep()
        mk.dma("sp", brbc, W["router_b"][i].partition_broadcast(128), w=[d_br])
        b2 = A.alloc((1024,), F32, parts=32)
        d_b2 = Dep()
        mk.dma("sp", b2, W["exp_b2"][i], w=[d_b2])
        b1F = A.alloc((NE, 16), F32)
        d_b1F = Dep()
        for e in range(NE):
            self.load_featmajor(b1F[:, e, :], d_b1F, W["exp_b1"][i, e], 16)
        w1b = A.alloc((8, 2048), BF16)
        d_w1 = Dep()
        w2b = A.alloc((8, 1024), BF16)
        d_w2 = Dep()
        hT = A.alloc((8, GT * 128), BF16)
        acc = A.alloc((GT, 1024), F32)
        gw = A.alloc((GT, 32), F32)
        actT = A.alloc((8, GT * 128), BF16)
        h32 = A.alloc((8, 128), F32)
        d_h32 = Dep()
        lg = A.alloc((32,), F32)
        top8 = A.alloc((8,), F32)
        sm = A.alloc((4,), F32)
        ex = A.alloc((32,), F32)
        d_lg = Dep()
        gwT = A.alloc((128,), F32, parts=32)
        d_gwT = Dep()
        tg = [A.alloc((512,), F32) for _ in range(2)]
        tsg = [A.alloc((512,), F32) for _ in range(2)]
        tu = [A.alloc((512,), F32) for _ in range(2)]
        d_tmp = [Dep() for _ in range(2)]
        w1src = W["exp_w1"][i]
        w2src = W["exp_w2"][i]
        ti = 0
        for (ga, gb) in groups:
            ng = gb - ga
            d_hT = [Dep() for _ in range(ng)]
            d_acc = [Dep() for _ in range(ng)]
            d_gw = [Dep() for _ in range(ng)]
            d_actT = [Dep() for _ in range(ng)]
            for lt in range(ng):
                tt = ga + lt
                self.prep_tile(tt, 3, hT, d_hT[lt], lt * 128, h32=h32, d_h32=d_h32)
                ps, dps = mk.next_ps()
                for kk in range(8):
                    mk.mm(ps[:, 0:32], h32[:, kk, :], wr[:, kk, :], kk == 0, kk == 7, r=[d_h32, d_wr], w=[dps])
                mk.tt("dve", lg, ps[:, 0:32], brbc, ALU.add, r=[dps, d_br], w=[d_lg])
                mk.op("dve", lambda E: E.max(top8, lg), r=[], w=[d_lg])
                mk.ts("dve", sm[:, 0:1], top8[:, 0:1], -1.0, None, ALU.mult, r=[], w=[d_lg])
                mk.act(ex, lg, AF.Exp, bias=sm[:, 0:1], scale=1.0, r=[d_lg], w=[d_lg])
                mk.stt("dve", ex, lg, top8[:, 3:4], ex, ALU.is_ge, ALU.mult, r=[d_lg], w=[d_lg])
                mk.op("dve", lambda E: E.reduce_sum(sm[:, 1:2], ex, mybir.AxisListType.X), r=[], w=[d_lg])
                mk.op("dve", lambda E: E.reciprocal(sm[:, 2:3], sm[:, 1:2]), r=[], w=[d_lg])
                g_ap = gw[:, lt, :]
                mk.ts("dve", g_ap, ex, sm[:, 2:3], None, ALU.mult, r=[d_lg], w=[d_gw[lt]])
                ps, dps = mk.next_ps()
                mk.tr(ps[0:32, 0:128], g_ap, self.ident, r=[d_gw[lt], self.d_ident], w=[dps])
                mk.copy("dve", gwT, ps[0:32, 0:128], r=[dps], w=[d_gwT])
                for hf in range(2):
                    ps, dps = mk.next_ps()
                    mk.mm(ps[:, :], gwT, b2[:, hf * 512:(hf + 1) * 512], True, True, r=[d_gwT, d_b2], w=[dps])
                    mk.copy("act", acc[:, lt, hf * 512:(hf + 1) * 512], ps[:, :], r=[dps], w=[d_acc[lt]])
            t5 = [(a, min(4, ng - a)) for a in range(0, ng, 4)]
            for e in range(NE):
                self.load_w_bf16(w1b, d_w1, w1src[e].rearrange("(k p) n -> p k n", p=128))
                self.load_w_bf16(w2b, d_w2, w2src[e].rearrange("(k p) n -> p k n", p=128))
                for (a, nt4) in t5:
                    n = nt4 * 128
                    c0 = a * 128
                    rd = d_hT[a:a + nt4]
                    for j in range(8):
                        psg, dpsg = mk.next_ps()
                        for kk in range(8):
                            mk.mm(psg[:, 0:n], w1b[:, kk, j * 128:(j + 1) * 128], hT[:, kk, c0:c0 + n], kk == 0, kk == 7,
                                  r=[d_w1] + rd, w=[dpsg])
                        psu, dpsu = mk.next_ps()
                        for kk in range(8):
                            mk.mm(psu[:, 0:n], w1b[:, kk, 1024 + j * 128:1024 + (j + 1) * 128], hT[:, kk, c0:c0 + n],
                                  kk == 0, kk == 7, r=[d_w1] + rd, w=[dpsu])
                        tb = ti % 2
                        ti += 1
                        g_, s_, u_, dt_ = tg[tb], tsg[tb], tu[tb], d_tmp[tb]
                        mk.ts("dve", g_[:, 0:n], psg[:, 0:n], b1F[:, e, j:j + 1], 7.0, ALU.add, ALU.min,
                              r=[dpsg, d_b1F], w=[dt_])
                        mk.act(s_[:, 0:n], g_[:, 0:n], AF.Sigmoid, scale=1.702, r=[dt_], w=[dt_])
                        mk.ts("dve", u_[:, 0:n], psu[:, 0:n], b1F[:, e, 8 + j:9 + j], 7.0, ALU.add, ALU.min,
                              r=[dpsu, d_b1F], w=[dt_])
                        mk.ts("pool", u_[:, 0:n], u_[:, 0:n], -7.0, 1.0, ALU.max, ALU.add, r=[dt_], w=[dt_])
                        mk.tt("pool", g_[:, 0:n], g_[:, 0:n], s_[:, 0:n], ALU.mult, r=[dt_], w=[dt_])
                        mk.tt("dve", actT[:, j, c0:c0 + n], g_[:, 0:n], u_[:, 0:n], ALU.mult, r=[dt_],
                              w=d_actT[a:a + nt4])
                for lt in range(ng):
                    for hf in range(2):
                        ps, dps = mk.next_ps()
                        for j in range(8):
                            mk.mm(ps[:, :], actT[:, j, lt * 128:(lt + 1) * 128], w2b[:, j, hf * 512:(hf + 1) * 512],
                                  j == 0, j == 7, r=[d_actT[lt], d_w2], w=[dps])
                        av = acc[:, lt, hf * 512:(hf + 1) * 512]
                        mk.stt("dve", av, ps[:, :], gw[:, lt, e:e + 1], av, ALU.mult, ALU.add,
                               r=[dps, d_gw[lt]], w=[d_acc[lt]])
            for lt in range(ng):
                tt = ga + lt
                if final:
                    dst, ddst = self.out_d[tt * 128:(tt + 1) * 128, :], self.outdep[tt]
                else:
                    dst, ddst = self.xres[tt * 128:(tt + 1) * 128, :], self.xdep[tt]
                self.post_tile(tt, [acc[:, lt, 0:512], acc[:, lt, 512:1024]], [d_acc[lt]], 1, dst, ddst)
            if not final:
                self.set_src_xres(range(ga, gb))
        A.release(m0)

    def build(self):
        for li, i in enumerate(self.layers):
            last = (i == DEPTH - 1)
            final = (li == len(self.layers) - 1)
            self.phase_mod(i)
            kind = i % 4
            if kind == 0:
                self.mixer_conv(i, not last)
            elif kind in (1, 2):
                self.mixer_attn(i, kind, not last)
            else:
                self.mixer_delta(i)
            if self.do_moe:
                self.moe(i, last, final)
            else:
                self.copy_xres_to_out()
        self.mk.finish()

    def copy_xres_to_out(self):
        for tt in range(NT):
            b = self.xt_i % 2
            self.xt_i += 1
            xt, dxt = self.xt[b], self.d_xt[b]
            self.mk.dma("sp", xt, self.src[tt], r=[self.xdep[tt]], w=[dxt])
            self.mk.dma("sp", self.out_d[tt * 128:(tt + 1) * 128, :], xt, r=[dxt], w=[self.outdep[tt]])


def build_program(layers=(0, 1, 2, 3), test_mode=False, do_moe=True):
    nc = bass.Bass("TRN2", target_bir_lowering=False)
    p = Prog(nc, layers, test_mode=test_mode, do_moe=do_moe)
    p.build()
    return nc, p


def kernel(**inputs):
    nc, p = build_program()
    consts = host_constants()
    in_maps = []
    for b in range(8):
        m = {"x": np.ascontiguousarray(inputs["x"][b]), "ctx": np.ascontiguousarray(inputs["ctx"][b]),
             "c": np.ascontiguousarray(inputs["c"][b]), "c_ctx": np.ascontiguousarray(inputs["c_ctx"])}
        for name, _ in W_SPECS:
            m[name] = np.ascontiguousarray(inputs[name])
        m.update(consts)
        in_maps.append(m)
    res = run_bass_kernel_spmd(nc, in_maps, core_ids=list(range(8)))
    return np.stack([np.asarray(r["out"]) for r in res.results], axis=0).astype(np.float32)
```

```python
import math
import numpy as np
from contextlib import ExitStack
import concourse.bass as bass
import concourse.mybir as mybir
from concourse.bass_utils import run_bass_kernel_spmd

F32 = mybir.dt.float32
BF16 = mybir.dt.bfloat16
AF = mybir.ActivationFunctionType
ALU = mybir.AluOpType

D = 1024
SEQ = 4096
CTX = 256
T = SEQ + CTX
NT = T // 128
NTL = SEQ // 128
DEPTH = 4
ALPHA = (2 * DEPTH) ** 0.25
LN_EPS = 1e-5
RMS_EPS = 1e-6
NE = 32
ENGS = ("pe", "act", "dve", "pool", "sp")
N_DMA_SEMS = 12


class Dep:
    __slots__ = ("w", "r")

    def __init__(self):
        self.w = None
        self.r = {}


class MK:
    def __init__(self, nc):
        self.nc = nc
        self.es = ExitStack()
        self.prog = {e: [] for e in ENGS}
        self.cnt = {e: 0 for e in ENGS}
        self.sems = {e: self.es.enter_context(nc.semaphore("sem_" + e)) for e in ENGS}
        self.dsem, self.dsem_uses, self.dq_count = {}, {}, {}
        for q in ("sp", "pool", "act"):
            self.dsem[q] = [self.es.enter_context(nc.semaphore(f"dma_{q}_{i}")) for i in range(N_DMA_SEMS)]
            self.dsem_uses[q] = [0] * N_DMA_SEMS
            self.dq_count[q] = 0
        self.waited = {e: {} for e in ENGS}
        self.semobj = {("e", e): self.sems[e] for e in ENGS}
        for q in self.dsem:
            for i, s in enumerate(self.dsem[q]):
                self.semobj[("d", q, i)] = s
        self.n_inst = 0
        self.n_wait = 0
        self.ps_banks = None
        self.ps_deps = None
        self.ps_i = 0

    def sbuf(self, name, shape, dtype):
        return self.es.enter_context(self.nc.sbuf_tensor(name, list(shape), dtype))

    def init_psum(self):
        self.ps_banks = [self.es.enter_context(self.nc.psum_tensor(f"psb{i}", [128, 512], F32)) for i in range(8)]
        self.ps_deps = [Dep() for _ in range(8)]

    def next_ps(self):
        i = self.ps_i % 8
        self.ps_i += 1
        return self.ps_banks[i], self.ps_deps[i]

    def ps_rot(self, key, banks):
        if not hasattr(self, "_rot"):
            self._rot = {}
        k = self._rot.get(key, 0)
        self._rot[key] = k + 1
        i = banks[k % len(banks)]
        return self.ps_banks[i], self.ps_deps[i]

    def _waits(self, eng, deps, force_same=False):
        hard, soft = deps
        out = []
        for sk in set(hard) | set(soft):
            hv, sv = hard.get(sk, 0), soft.get(sk, 0)
            if sk == ("e", eng) and not force_same:
                if eng == "pe":
                    continue
                val = hv
            else:
                val = max(hv, sv)
            if val <= 0 or self.waited[eng].get(sk, 0) >= val:
                continue
            self.waited[eng][sk] = val
            out.append((self.semobj[sk], val))
        return out

    def _collect(self, r, w):
        hard, soft = {}, {}
        for b in r:
            if b.w is not None and hard.get(b.w[0], 0) < b.w[1]:
                hard[b.w[0]] = b.w[1]
        for b in w:
            if b.w is not None and hard.get(b.w[0], 0) < b.w[1]:
                hard[b.w[0]] = b.w[1]
            for sk, val in b.r.items():
                if soft.get(sk, 0) < val:
                    soft[sk] = val
        return hard, soft

    def _commit(self, ev, r, w):
        sk, val = ev
        for b in r:
            if b.r.get(sk, 0) < val:
                b.r[sk] = val
        for b in w:
            b.w = ev
            b.r = {}

    def op(self, eng, fn, r=(), w=()):
        waits = self._waits(eng, self._collect(r, w))
        self.cnt[eng] += 1
        ev = (("e", eng), self.cnt[eng])
        sem = self.sems[eng]
        self.n_wait += len(waits)
        self.n_inst += 1

        def emit(E, waits=waits, fn=fn, sem=sem):
            for s, v in waits:
                E.wait_ge(s, v)
            fn(E).then_inc(sem, 1)
        self.prog[eng].append(emit)
        self._commit(ev, r, w)
        return ev

    def dma(self, q, out, in_, r=(), w=(), **kw):
        k = self.dq_count[q]
        self.dq_count[q] += 1
        i = k % N_DMA_SEMS
        sk = ("d", q, i)
        deps = self._collect(r, w)
        prev = self.dsem_uses[q][i]
        if prev > 0:
            deps[0][sk] = max(deps[0].get(sk, 0), 16 * prev)
        waits = self._waits(q, deps, force_same=True)
        self.dsem_uses[q][i] += 1
        ev = (sk, 16 * self.dsem_uses[q][i])
        sem = self.dsem[q][i]
        self.n_wait += len(waits)
        self.n_inst += 1

        def emit(E, waits=waits, sem=sem, out=out, in_=in_, kw=kw):
            for s, v in waits:
                E.wait_ge(s, v)
            E.dma_start(out=out, in_=in_, **kw).then_inc(sem, 16)
        self.prog[q].append(emit)
        self._commit(ev, r, w)
        return ev

    def barrier(self):
        evs = []
        for e in ENGS:
            if self.cnt[e] > 0:
                evs.append((("e", e), self.cnt[e]))
        for q in self.dsem:
            for i in range(N_DMA_SEMS):
                if self.dsem_uses[q][i] > 0:
                    evs.append((("d", q, i), 16 * self.dsem_uses[q][i]))
        for e in ENGS:
            waits = []
            for sk, val in evs:
                if sk == ("e", e):
                    continue
                if self.waited[e].get(sk, 0) >= val:
                    continue
                self.waited[e][sk] = val
                waits.append((self.semobj[sk], val))
            if waits:
                def emit(E, waits=waits):
                    for s, v in waits:
                        E.wait_ge(s, v)
                self.prog[e].append(emit)

    def finish(self):
        self.barrier()
        prog = self.prog
        with self.nc.Block() as block:
            @block.tensor
            def _(E):
                for f in prog["pe"]:
                    f(E)

            @block.scalar
            def _(E):
                for f in prog["act"]:
                    f(E)

            @block.vector
            def _(E):
                for f in prog["dve"]:
                    f(E)

            @block.gpsimd
            def _(E):
                for f in prog["pool"]:
                    f(E)

            @block.sync
            def _(E):
                for f in prog["sp"]:
                    f(E)
        self.es.close()

    def mm(self, out, lhsT, rhs, start, stop, r=(), w=(), sgc=False):
        if sgc:
            return self.op("pe", lambda E: E.matmul(out, lhsT, rhs, start=start, stop=stop, skip_group_check=True), r, w)
        return self.op("pe", lambda E: E.matmul(out, lhsT, rhs, start=start, stop=stop), r, w)

    def tr(self, out, in_, ident, r=(), w=()):
        return self.op("pe", lambda E: E.transpose(out, in_, ident), r, w)

    def ts(self, eng, out, in0, s1, s2, op0, op1=None, r=(), w=(), accum_out=None):
        if op1 is None:
            if accum_out is None:
                return self.op(eng, lambda E: E.tensor_scalar(out, in0, s1, None, op0), r, w)
        if accum_out is not None:
            return self.op(eng, lambda E: E.tensor_scalar(out, in0, s1, s2, op0, op1, accum_out=accum_out), r, w)
        return self.op(eng, lambda E: E.tensor_scalar(out, in0, s1, s2, op0, op1), r, w)

    def tt(self, eng, out, in0, in1, op, r=(), w=()):
        return self.op(eng, lambda E: E.tensor_tensor(out, in0, in1, op), r, w)

    def stt(self, eng, out, in0, scalar, in1, op0, op1, r=(), w=()):
        return self.op(eng, lambda E: E.scalar_tensor_tensor(out, in0, scalar, in1, op0, op1), r, w)

    def act(self, out, in_, func, bias=None, scale=1.0, r=(), w=(), accum_out=None):
        kw = {}
        if bias is not None:
            kw["bias"] = bias
        if accum_out is not None:
            kw["accum_out"] = accum_out
        return self.op("act", lambda E: E.activation(out, in_, func, scale=scale, **kw), r, w)

    def copy(self, eng, out, in_, r=(), w=()):
        if eng == "act":
            return self.op("act", lambda E: E.copy(out, in_), r, w)
        return self.op(eng, lambda E: E.tensor_copy(out, in_), r, w)

    def memset(self, eng, ap, val, r=(), w=()):
        return self.op(eng, lambda E: E.memset(ap, val), r, w)


class Arena:
    def __init__(self, mk, words):
        self.mk = mk
        self.t = mk.sbuf("arena", [128, words], F32)
        self.cap = words
        self.off = 0

    def mark(self):
        return self.off

    def release(self, m):
        self.mk.barrier()
        self.off = m

    def alloc(self, shape, dtype=F32, parts=128):
        n = 1
        for s in shape:
            n *= s
        size = 4 if dtype == F32 else 2
        words = (n * size + 3) // 4
        words = (words + 7) // 8 * 8
        if self.off + words > self.cap:
            raise RuntimeError(f"arena overflow: need {words} at {self.off} cap {self.cap}")
        ap = self.t[0:parts, self.off:self.off + words]
        self.off += words
        self.peak = max(getattr(self, "peak", 0), self.off)
        if dtype != F32:
            ap = ap.bitcast(dtype)
        ap = ap[:, 0:n]
        if len(shape) == 2:
            ap = ap.rearrange("p (a b) -> p a b", a=shape[0])
        elif len(shape) == 3:
            ap = ap.rearrange("p (a b c) -> p a b c", a=shape[0], b=shape[1])
        elif len(shape) == 4:
            ap = ap.rearrange("p (a b c d) -> p a b c d", a=shape[0], b=shape[1], c=shape[2])
        return ap


W_SPECS = [
    ("mod_w", [4, 1024, 6144]), ("mod_b", [4, 6144]), ("ln1_g", [4, 1024]), ("ln1_b", [4, 1024]),
    ("ln2_g", [4, 1024]), ("ln2_b", [4, 1024]), ("router_w", [4, 1024, 32]), ("router_b", [4, 32]),
    ("exp_w1", [4, 32, 1024, 2048]), ("exp_b1", [4, 32, 2048]), ("exp_w2", [4, 32, 1024, 1024]),
    ("exp_b2", [4, 32, 1024]), ("conv_in_w", [1, 1024, 3072]), ("conv_w", [1, 3, 1024]),
    ("conv_out_w", [1, 1024, 1024]), ("swa_qkv_w", [1, 1024, 1536]), ("swa_qkv_b", [1, 1536]),
    ("swa_sink", [1, 16]), ("swa_out_w", [1, 1024, 1024]), ("swa_out_b", [1, 1024]),
    ("diff_qkv_w", [1, 1024, 3072]), ("diff_lambda", [1, 4, 64]), ("diff_subln_g", [1, 128]),
    ("diff_out_w", [1, 1024, 1024]), ("delta_qkvz_w", [1, 1024, 6144]), ("delta_ba_w", [1, 1024, 64]),
    ("delta_a_log", [1, 2, 16]), ("delta_dt_bias", [1, 2, 16]), ("delta_conv_w", [1, 5, 4096]),
    ("delta_norm_g", [1, 128]), ("delta_out_w", [1, 2048, 1024]),
]


def host_constants():
    c = {}
    c["ident"] = np.eye(128, dtype=np.float32)
    rows = SEQ // 64
    row = np.repeat(np.arange(rows, dtype=np.float32), 64)
    col = np.tile(np.arange(64, dtype=np.float32), rows)
    half = 32
    inv = (10000.0 ** (-np.arange(0, half, 2, dtype=np.float32) / half)).astype(np.float32)
    ar = row[:, None] * inv
    ac = col[:, None] * inv
    ang = np.concatenate([ar, ar, ac, ac], axis=-1).astype(np.float32)
    cos = np.cos(ang).astype(np.float32).T
    sin = np.sin(ang).astype(np.float32).T
    sign = np.ones((64, 1), np.float32)
    sign[0:16] = -1.0
    sign[32:48] = -1.0
    c["cosT"] = np.ascontiguousarray(np.concatenate([cos, cos], axis=0))
    c["sinT"] = np.ascontiguousarray(np.concatenate([sin, sin], axis=0))
    Rm = np.zeros((128, 128), np.float32)
    for hh in range(2):
        for p in range(64):
            blk = p // 16
            if blk % 2 == 0:
                Rm[hh * 64 + p + 16, hh * 64 + p] = -1.0
            else:
                Rm[hh * 64 + p - 16, hh * 64 + p] = 1.0
    c["rotm"] = Rm
    kj = np.arange(128)[:, None]
    qi = np.arange(128)[None, :]
    c["mprev"] = np.tile((kj >= qi).astype(np.float32), (1, 4))
    c["mnext"] = np.tile((kj <= qi).astype(np.float32), (1, 4))
    k_ = np.arange(128)[:, None]
    m_ = np.arange(128)[None, :]
    c["triF"] = (k_ <= m_).astype(np.float32)
    c["triB"] = (k_ >= m_).astype(np.float32)
    c["m2F"] = (k_ > m_).astype(np.float32)
    c["m2B"] = (k_ < m_).astype(np.float32)
    c["selF"] = np.repeat((np.arange(128) == 127).astype(np.float32)[:, None], 128, axis=1)
    c["selB"] = np.repeat((np.arange(128) == 0).astype(np.float32)[:, None], 128, axis=1)
    return c


class Prog:
    def __init__(self, nc, layers, test_mode=False, do_moe=True):
        self.nc = nc
        self.do_moe = do_moe
        self.dbg_names = []
        self.layers = list(layers)
        self.test_mode = test_mode
        self.mk = MK(nc)
        mk = self.mk
        mk.init_psum()
        self.A = Arena(mk, 51 * 1024)
        dt = nc.dram_tensor
        self.x_d = dt("x", [SEQ, D], F32, kind="ExternalInput").ap()
        self.ctx_d = dt("ctx", [CTX, D], F32, kind="ExternalInput").ap()
        self.c_d = dt("c", [D], F32, kind="ExternalInput").ap()
        self.cctx_d = dt("c_ctx", [D], F32, kind="ExternalInput").ap()
        wshapes = dict(W_SPECS)

        class _LazyW(dict):
            def __missing__(d, name):
                ap = dt(name, wshapes[name], F32, kind="ExternalInput").ap()
                d[name] = ap
                return ap
        self.W = _LazyW()
        if not test_mode:
            for name, shape in W_SPECS:
                self.W[name]
        self.C = {}
        for name, arr in host_constants().items():
            self.C[name] = dt(name, list(arr.shape), F32, kind="ExternalInput").ap()
        out_rows = T if test_mode else SEQ
        self.out_d = dt("out", [out_rows, D], F32, kind="ExternalOutput").ap()
        self.xres = dt("xres", [T, D], F32).ap()
        self.xdep = [Dep() for _ in range(NT)]
        self.outdep = [Dep() for _ in range(NT)]
        self.src = [self.x_d[t * 128:(t + 1) * 128, :] for t in range(NTL)] + \
                   [self.ctx_d[t * 128:(t + 1) * 128, :] for t in range(NT - NTL)]
        self.setup_persistent()

    def setup_persistent(self):
        mk, A = self.mk, self.A
        self.ident = A.alloc((128,), F32)
        self.d_ident = Dep()
        mk.dma("sp", self.ident, self.C["ident"][:, :], w=[self.d_ident])
        self.identb = A.alloc((128,), BF16)
        self.d_identb = Dep()
        mk.copy("dve", self.identb, self.ident, r=[self.d_ident], w=[self.d_identb])
        self.ones = A.alloc((128,), F32)
        self.d_ones = Dep()
        mk.memset("dve", self.ones, 1.0, w=[self.d_ones])
        self.modF = A.alloc((2, 6, 8), F32)
        self.d_modF = Dep()
        self.modbc = A.alloc((2, 2, 1024), F32)
        self.d_modbc = Dep()
        self.lnbc = A.alloc((4, 1024), F32)
        self.d_lnbc = Dep()
        self.xt = [A.alloc((1024,), F32) for _ in range(2)]
        self.d_xt = [Dep() for _ in range(2)]
        self.zt = [A.alloc((1024,), F32) for _ in range(2)]
        self.d_zt = [Dep() for _ in range(2)]
        self.st = [A.alloc((2, 6), F32) for _ in range(2)]
        self.mv = [A.alloc((4,), F32) for _ in range(2)]
        self.xt_i = 0
        self.zt_i = 0
        self.fm_tmp = A.alloc((128,), F32, parts=64)
        self.d_fm_tmp = Dep()

    def load_featmajor(self, dst, d_dst, vec_ap, n):
        mk = self.mk
        tmp, d_tmp = self.fm_tmp[0:n, :], self.d_fm_tmp
        mk.dma("sp", tmp, vec_ap.rearrange("(j p) -> j p", p=128), w=[d_tmp])
        ps, dps = mk.next_ps()
        mk.tr(ps[:, 0:n], tmp, self.ident[0:n, 0:n], r=[d_tmp, self.d_ident], w=[dps])
        mk.copy("dve", dst, ps[:, 0:n], r=[dps], w=[d_dst])

    def phase_mod(self, i):
        mk, A, W = self.mk, self.A, self.W
        m0 = A.mark()
        cT = A.alloc((8, 2), F32)
        d_cT = Dep()
        ctmp = A.alloc((8,), F32)
        d_ctmp = Dep()
        self.load_featmajor(ctmp, d_ctmp, self.c_d, 8)
        mk.act(cT[:, :, 0], ctmp, AF.Silu, r=[d_ctmp], w=[d_cT])
        self.load_featmajor(ctmp, d_ctmp, self.cctx_d, 8)
        mk.act(cT[:, :, 1], ctmp, AF.Silu, r=[d_ctmp], w=[d_cT])
        sbc = A.alloc((2, 8, 128), F32)
        d_sbc = Dep()
        for lc in range(2):
            for k in range(8):
                mk.ts("dve", sbc[:, lc, k, :], self.ones, cT[:, k, lc:lc + 1], None, ALU.mult,
                      r=[self.d_ones, d_cT], w=[d_sbc])
        mbF = A.alloc((48,), F32)
        d_mbF = Dep()
        self.load_featmajor(mbF, d_mbF, W["mod_b"][i], 48)
        mbbc = A.alloc((1024,), F32)
        d_mbbc = Dep()
        wsl = [A.alloc((8, 1024), F32) for _ in range(2)]
        d_wsl = [Dep() for _ in range(2)]
        mw = W["mod_w"][i].rearrange("(k p) n -> p k n", p=128)
        for m in range(6):
            ws, dws = wsl[m % 2], d_wsl[m % 2]
            mk.dma("sp", ws, mw[:, :, m * 1024:(m + 1) * 1024], w=[dws])
            for kc in range(8):
                ps, dps = mk.next_ps()
                for kk in range(8):
                    mk.mm(ps[:, 0:2], ws[:, kk, kc * 128:(kc + 1) * 128], cT[:, kk, 0:2], kk == 0, kk == 7,
                          r=[dws, d_cT], w=[dps])
                mk.ts("dve", self.modF[:, :, m, kc], ps[:, 0:2], mbF[:, m * 8 + kc:m * 8 + kc + 1], None, ALU.add,
                      r=[dps, d_mbF], w=[self.d_modF])
            if m in (2, 5):
                j = 0 if m == 2 else 1
                mk.dma("sp", mbbc, W["mod_b"][i][m * 1024:(m + 1) * 1024].partition_broadcast(128), w=[d_mbbc])
                for lc in range(2):
                    for hf in range(2):
                        ps, dps = mk.next_ps()
                        for kk in range(8):
                            mk.mm(ps[:, :], sbc[:, lc, kk, :], ws[:, kk, hf * 512:(hf + 1) * 512], kk == 0, kk == 7,
                                  r=[dws, d_sbc], w=[dps])
                        mk.tt("dve", self.modbc[:, lc, j, hf * 512:(hf + 1) * 512], ps[:, :],
                              mbbc[:, hf * 512:(hf + 1) * 512], ALU.add, r=[dps, d_mbbc], w=[self.d_modbc])
        for m in (1, 4):
            mk.ts("dve", self.modF[:, :, m, :], self.modF[:, :, m, :], 1.0, None, ALU.add, r=[], w=[self.d_modF])
        for q, nm in enumerate(("ln1_g", "ln1_b", "ln2_g", "ln2_b")):
            mk.dma("sp", self.lnbc[:, q, :], W[nm][i].partition_broadcast(128), w=[self.d_lnbc])
        A.release(m0)

    def prep_tile(self, tt, mi, hT, d_hT, col0, h32=None, d_h32=None):
        mk = self.mk
        lc = 0 if tt < NTL else 1
        b = self.xt_i % 2
        self.xt_i += 1
        xt, dxt = self.xt[b], self.d_xt[b]
        mk.dma("sp", xt, self.src[tt], r=[self.xdep[tt]], w=[dxt])
        for hf in range(2):
            ps, dps = mk.next_ps()
            for q in range(4):
                k = hf * 4 + q
                mk.tr(ps[:, q * 128:(q + 1) * 128], xt[:, k * 128:(k + 1) * 128], self.ident,
                      r=[dxt, self.d_ident], w=[dps])
            for q in range(4):
                k = hf * 4 + q
                o = hT[:, k, col0:col0 + 128]
                sc = self.modF[:, lc, mi + 1, k:k + 1]
                sh = self.modF[:, lc, mi, k:k + 1]
                if h32 is not None:
                    o32 = h32[:, k, :]
                    mk.ts("dve", o32, ps[:, q * 128:(q + 1) * 128], sc, sh, ALU.mult, ALU.add,
                          r=[dps, self.d_modF], w=[d_h32])
                    mk.copy("act", o, o32, r=[d_h32], w=[d_hT])
                elif q % 2 == 0:
                    mk.ts("dve", o, ps[:, q * 128:(q + 1) * 128], sc, sh, ALU.mult, ALU.add,
                          r=[dps, self.d_modF], w=[d_hT])
                else:
                    mk.act(o, ps[:, q * 128:(q + 1) * 128], AF.Identity, bias=sh, scale=sc,
                           r=[dps, self.d_modF], w=[d_hT])

    def post_tile(self, tt, y, d_y, j, dst, d_dst, ybias=None, d_ybias=None):
        mk = self.mk
        lc = 0 if tt < NTL else 1
        b = self.xt_i % 2
        self.xt_i += 1
        xt, dxt = self.xt[b], self.d_xt[b]
        mk.dma("sp", xt, self.src[tt], r=[self.xdep[tt]], w=[dxt])
        zb = self.zt_i % 2
        self.zt_i += 1
        z, dz = self.zt[zb], self.d_zt[zb]
        st, mv = self.st[zb], self.mv[zb]
        for hf in range(2):
            sl = slice(hf * 512, (hf + 1) * 512)
            if ybias is not None:
                mk.tt("dve", z[:, sl], y[hf], ybias[:, sl], ALU.add, r=list(d_y) + [d_ybias], w=[dz])
                mk.tt("dve", z[:, sl], z[:, sl], self.modbc[:, lc, j, sl], ALU.mult, r=[self.d_modbc], w=[dz])
            else:
                mk.tt("dve", z[:, sl], y[hf], self.modbc[:, lc, j, sl], ALU.mult, r=list(d_y) + [self.d_modbc], w=[dz])
        mk.stt("dve", z, xt, ALPHA, z, ALU.mult, ALU.add, r=[dxt], w=[dz])
        mk.op("dve", lambda E: E.bn_stats(st[:, 0, :], z[:, 0:512]), r=[], w=[dz])
        mk.op("dve", lambda E: E.bn_stats(st[:, 1, :], z[:, 512:1024]), r=[], w=[dz])
        mk.op("dve", lambda E: E.bn_aggr(mv[:, 0:2], st), r=[], w=[dz])
        mk.ts("dve", mv[:, 2:3], mv[:, 1:2], LN_EPS, None, ALU.add, r=[], w=[dz])
        mk.act(mv[:, 2:3], mv[:, 2:3], AF.Sqrt, r=[dz], w=[dz])
        mk.op("dve", lambda E: E.reciprocal(mv[:, 2:3], mv[:, 2:3]), r=[dz], w=[dz])
        mk.ts("dve", z, z, mv[:, 0:1], mv[:, 2:3], ALU.subtract, ALU.mult, r=[], w=[dz])
        mk.tt("pool", z, z, self.lnbc[:, 2 * j, :], ALU.mult, r=[self.d_lnbc], w=[dz])
        mk.tt("pool", z, z, self.lnbc[:, 2 * j + 1, :], ALU.add, r=[self.d_lnbc], w=[dz])
        mk.dma("sp", dst, z, r=[dz], w=[d_dst])

    def dbg(self, name, ap, deps, dtype=F32):
        if not self.test_mode:
            return
        shape = list(ap.shape)
        t = self.nc.dram_tensor("dbg_" + name, shape, dtype, kind="ExternalOutput").ap()
        self.mk.dma("sp", t, ap, r=list(deps), w=[Dep()])
        self.dbg_names.append("dbg_" + name)

    def set_src_xres(self, tiles):
        for tt in tiles:
            self.src[tt] = self.xres[tt * 128:(tt + 1) * 128, :]

    def load_w_bf16(self, dst, d_dst, src_ap):
        self.mk.dma("pool", dst, src_ap, w=[d_dst])

    def mixer_conv(self, i, ctx_out):
        mk, A, W = self.mk, self.A, self.W
        ntiles = NT
        m0 = A.mark()
        hT = A.alloc((8, T), BF16)
        d_hT = [Dep() for _ in range(NT)]
        for tt in range(NT):
            self.prep_tile(tt, 0, hT, d_hT[tt], tt * 128)
        self.dbg("hT", hT, d_hT, BF16)
        self.dbg("modF", self.modF, [self.d_modF])
        self.dbg("modbc", self.modbc, [self.d_modbc])
        gT_d = self.nc.dram_tensor("conv_gT", [D, T], BF16).ap()
        d_gTd = [Dep() for _ in range(8)]
        wc = A.alloc((8, 3), F32)
        d_wc = Dep()
        wtmp = A.alloc((8,), F32)
        d_wtmp = Dep()
        for k in range(3):
            self.load_featmajor(wtmp, d_wtmp, W["conv_w"][0, k], 8)
            mk.copy("dve", wc[:, :, k], wtmp, r=[d_wtmp], w=[d_wc])
        wj = [A.alloc((3, 8, 128), BF16) for _ in range(2)]
        d_wj = [Dep() for _ in range(2)]
        LP = SEQ + 2 + CTX + 2
        cu = A.alloc((LP,), F32)
        d_cu = Dep()
        bsb = A.alloc((T,), F32)
        d_bsb = Dep()
        t1 = A.alloc((SEQ,), F32)
        d_t1 = Dep()
        gj = [A.alloc((T,), BF16)]
        d_gj = [Dep()]
        csb = [A.alloc((512,), F32) for _ in range(2)]
        d_csb = [Dep() for _ in range(2)]
        mk.memset("dve", cu, 0.0, w=[d_cu])
        win = W["conv_in_w"][0].rearrange("(k p) n -> p k n", p=128)
        tts = [(q * 512, 512, 1 + q * 512) for q in range(8)] + [(SEQ, CTX, SEQ + 3)]
        ci = 0
        for j in range(8):
            wjb, dwj = wj[j % 2], d_wj[j % 2]
            for s in range(3):
                self.load_w_bf16(wjb[:, s], dwj, win[:, :, s * 1024 + j * 128:s * 1024 + (j + 1) * 128])
            for (t0, n, co) in tts:
                pss = []
                for s in range(3):
                    ps, dps = mk.next_ps()
                    for kk in range(8):
                        mk.mm(ps[:, 0:n], wjb[:, s, kk, :], hT[:, kk, t0:t0 + n], kk == 0, kk == 7,
                              r=[dwj] + d_hT[t0 // 128:(t0 + n) // 128], w=[dps])
                    pss.append((ps, dps))
                cb, dcb = csb[ci % 2], d_csb[ci % 2]
                ci += 1
                mk.copy("act", bsb[:, t0:t0 + n], pss[0][0][:, 0:n], r=[pss[0][1]], w=[d_bsb])
                mk.copy("act", cb[:, 0:n], pss[1][0][:, 0:n], r=[pss[1][1]], w=[dcb])
                mk.tt("dve", cu[:, co:co + n], cb[:, 0:n], pss[2][0][:, 0:n], ALU.mult, r=[dcb, pss[2][1]], w=[d_cu])
            g, dg = gj[0], d_gj[0]
            for (s0, L, c0) in ((0, SEQ, 0), (SEQ, CTX, SEQ + 2)):
                tv = t1[:, 0:L]
                mk.ts("dve", tv, cu[:, c0 + 1:c0 + 1 + L], wc[:, j, 1:2], None, ALU.mult, r=[d_cu, d_wc], w=[d_t1])
                mk.stt("dve", tv, cu[:, c0:c0 + L], wc[:, j, 0:1], tv, ALU.mult, ALU.add, r=[d_cu], w=[d_t1])
                mk.stt("dve", tv, cu[:, c0 + 2:c0 + 2 + L], wc[:, j, 2:3], tv, ALU.mult, ALU.add, r=[d_cu], w=[d_t1])
                mk.tt("dve", g[:, s0:s0 + L], tv, bsb[:, s0:s0 + L], ALU.mult, r=[d_bsb], w=[dg])
            mk.dma("sp", gT_d[j * 128:(j + 1) * 128, :], g, r=[dg], w=[d_gTd[j]])
        A.release(m0)
        m0 = A.mark()
        gT = A.alloc((8, T), BF16)
        d_gT = Dep()
        for j in range(8):
            mk.dma("sp", gT[:, j, :], gT_d[j * 128:(j + 1) * 128, :], r=[d_gTd[j]], w=[d_gT])
        wo = A.alloc((8, 1024), BF16)
        d_wo = Dep()
        self.load_w_bf16(wo, d_wo, W["conv_out_w"][0].rearrange("(k p) n -> p k n", p=128))
        self.dbg("gT", gT, [d_gT], BF16)
        for tt in range(NT):
            ys, dys = [], []
            for hf in range(2):
                ps, dps = mk.next_ps()
                for j in range(8):
                    mk.mm(ps[:, :], gT[:, j, tt * 128:(tt + 1) * 128], wo[:, j, hf * 512:(hf + 1) * 512], j == 0, j == 7,
                          r=[d_gT, d_wo], w=[dps])
                ys.append(ps[:, :])
                dys.append(dps)
            self.post_tile(tt, ys, dys, 0, self.xres[tt * 128:(tt + 1) * 128, :], self.xdep[tt])
        self.set_src_xres(range(NT))
        A.release(m0)


    def mixer_attn(self, i, kind, ctx_out):
        mk, A, W, C = self.mk, self.A, self.W, self.C
        nc = self.nc
        swa = (kind == 1)
        qtiles = list(range(NT)) if ctx_out else list(range(NTL))
        if swa:
            wqkv, bqkv = W["swa_qkv_w"][0], W["swa_qkv_b"][0]
            nkc, nvh, vd = 4, 4, 64
            wo_ap, kcol0, vcol0 = W["swa_out_w"][0], 1024, 1280
        else:
            wqkv, bqkv = W["diff_qkv_w"][0], None
            nkc, nvh, vd = 8, 8, 128
            wo_ap, kcol0, vcol0 = W["diff_out_w"][0], 1024, 2048
        va = vd + 1
        qT_d = nc.dram_tensor(f"att{i}_qT", [1024, T], BF16).ap()
        kT_d = nc.dram_tensor(f"att{i}_kT", [nkc * 128, T], BF16).ap()
        v_d = nc.dram_tensor(f"att{i}_v", [T, nvh * va], BF16).ap()
        if self.test_mode:
            o_d = nc.dram_tensor(f"dbg_att{i}_o", [T, 1024], BF16, kind="ExternalOutput").ap()
            self.dbg_names.append(f"dbg_att{i}_o")
            qT_dbg = nc.dram_tensor(f"dbg_att{i}_qT", [1024, T], BF16, kind="ExternalOutput").ap()
            self.dbg_names.append(f"dbg_att{i}_qT")
        else:
            o_d = nc.dram_tensor(f"att{i}_o", [T, 1024], BF16).ap()
        d_qd = [Dep() for _ in range(8)]
        d_kd = [Dep() for _ in range(nkc)]
        d_vd = [Dep() for _ in range(NT)]
        d_od = [Dep() for _ in range(NT)]
        wv = wqkv.rearrange("(k p) n -> p k n", p=128)
        m0 = A.mark()
        hT = A.alloc((8, T), BF16)
        d_hT = [Dep() for _ in range(NT)]
        for tt in range(NT):
            self.prep_tile(tt, 0, hT, d_hT[tt], tt * 128)
        cosT = A.alloc((SEQ,), F32)
        sinT = A.alloc((SEQ,), F32)
        rotm = A.alloc((128,), F32)
        d_tab = Dep()
        mk.dma("sp", cosT, C["cosT"][:, :], w=[d_tab])
        mk.dma("sp", sinT, C["sinT"][:, :], w=[d_tab])
        mk.dma("sp", rotm, C["rotm"][:, :], w=[d_tab])
        bF = A.alloc((16,), F32)
        d_bF = Dep()
        if swa:
            self.load_featmajor(bF[:, 0:12], d_bF, bqkv, 12)
            bkd = A.alloc((4,), F32)
            d_bkd = Dep()
            for g in range(4):
                for hh in range(2):
                    mk.dma("sp", bkd[hh * 64:(hh + 1) * 64, g:g + 1],
                           bqkv[1024 + g * 64:1024 + (g + 1) * 64].rearrange("(p o) -> p o", o=1), w=[d_bkd])
        else:
            mk.memset("dve", bF, 0.0, w=[d_bF])
        wc = [A.alloc((8, 128), BF16) for _ in range(2)]
        d_wc = [Dep() for _ in range(2)]
        qsb = [A.alloc((512,), F32) for _ in range(2)]
        d_qsb = [Dep() for _ in range(2)]
        t1 = [A.alloc((512,), F32) for _ in range(2)]
        d_t1 = [Dep() for _ in range(2)]
        och = [A.alloc((T,), BF16) for _ in range(2)]
        d_och = [Dep() for _ in range(2)]
        tts = [(q * 512, 512) for q in range(8)] + [(SEQ, CTX)]
        chunks = []
        for c in range(8):
            chunks.append(("q", c, qT_d[c * 128:(c + 1) * 128, :], d_qd[c], [(c * 128, 128, 0)], bF[:, c:c + 1]))
        if swa:
            for g in range(4):
                chunks.append(("k", g, kT_d[g * 128:(g + 1) * 128, :], d_kd[g],
                               [(kcol0 + g * 64, 64, 0), (kcol0 + g * 64, 64, 64)], bkd[:, g:g + 1]))
        else:
            for c in range(8):
                chunks.append(("k", c, kT_d[c * 128:(c + 1) * 128, :], d_kd[c], [(kcol0 + c * 128, 128, 0)], bF[:, 8:9]))
        ri = 0
        for ci, (nm, c, dst, d_dst, wcols, bias) in enumerate(chunks):
            wcb, dwc = wc[ci % 2], d_wc[ci % 2]
            for (c0, ncol, o0) in wcols:
                self.load_w_bf16(wcb[:, :, o0:o0 + ncol], dwc, wv[:, :, c0:c0 + ncol])
            ob, dob = och[ci % 2], d_och[ci % 2]
            for (t0, n) in tts:
                ps, dps = mk.next_ps()
                for kk in range(8):
                    mk.mm(ps[:, 0:n], wcb[:, kk, :], hT[:, kk, t0:t0 + n], kk == 0, kk == 7,
                          r=[dwc] + d_hT[t0 // 128:(t0 + n) // 128], w=[dps])
                if t0 >= SEQ:
                    mk.act(ob[:, t0:t0 + n], ps[:, 0:n], AF.Identity, bias=bias, scale=1.0,
                           r=[dps, d_bF] + ([d_bkd] if swa else []), w=[dob])
                    continue
                rb = ri % 2
                ri += 1
                q_, dq_, t_, dt_ = qsb[rb], d_qsb[rb], t1[rb], d_t1[rb]
                mk.act(q_[:, 0:n], ps[:, 0:n], AF.Identity, bias=bias, scale=1.0,
                       r=[dps, d_bF] + ([d_bkd] if swa else []), w=[dq_])
                ps2, dps2 = mk.next_ps()
                mk.mm(ps2[:, 0:n], rotm, q_[:, 0:n], True, True, r=[d_tab, dq_], w=[dps2])
                mk.tt("dve", t_[:, 0:n], ps2[:, 0:n], sinT[:, t0:t0 + n], ALU.mult, r=[dps2, d_tab], w=[dt_])
                mk.tt("pool", q_[:, 0:n], q_[:, 0:n], cosT[:, t0:t0 + n], ALU.mult, r=[d_tab], w=[dq_])
                mk.tt("dve", ob[:, t0:t0 + n], q_[:, 0:n], t_[:, 0:n], ALU.add, r=[dq_, dt_], w=[dob])
            mk.dma("sp", dst, ob, r=[dob], w=[d_dst])
            if self.test_mode and nm == "q":
                mk.dma("sp", qT_dbg[c * 128:(c + 1) * 128, :], ob, r=[dob], w=[Dep()])
        nvc = nvh * vd
        wvb = A.alloc((8, nvc), BF16)
        d_wvb = Dep()
        self.load_w_bf16(wvb, d_wvb, wv[:, :, vcol0:vcol0 + nvc])
        vbias = None
        if swa:
            vbias = A.alloc((nvc,), F32)
            d_vbias = Dep()
            mk.dma("sp", vbias, bqkv[vcol0:vcol0 + nvc].partition_broadcast(128), w=[d_vbias])
        vt = [A.alloc((nvh, va), BF16) for _ in range(2)]
        d_vt = [Dep() for _ in range(2)]
        for b in range(2):
            mk.memset("dve", vt[b], 1.0, w=[d_vt[b]])
        for tt in range(NT):
            vb, dvb = vt[tt % 2], d_vt[tt % 2]
            for c0 in range(0, nvc, 512):
                ncol = min(512, nvc - c0)
                ps, dps = mk.next_ps()
                for kk in range(8):
                    mk.mm(ps[:, 0:ncol], hT[:, kk, tt * 128:(tt + 1) * 128], wvb[:, kk, c0:c0 + ncol], kk == 0, kk == 7,
                          r=[d_wvb, d_hT[tt]], w=[dps])
                h0, nh = c0 // vd, ncol // vd
                src = ps[:, 0:ncol].rearrange("p (h d) -> p h d", d=vd)
                if swa:
                    mk.tt("dve", vb[:, h0:h0 + nh, 0:vd], src,
                          vbias[:, c0:c0 + ncol].rearrange("p (h d) -> p h d", d=vd), ALU.add,
                          r=[dps, d_vbias], w=[dvb])
                else:
                    mk.copy("act", vb[:, h0:h0 + nh, 0:vd], src, r=[dps], w=[dvb])
            mk.dma("sp", v_d[tt * 128:(tt + 1) * 128, :].rearrange("p (h d) -> p h d", d=va), vb, r=[dvb], w=[d_vd[tt]])
        A.release(m0)
        import os as _os
        if _os.environ.get("KSTOP") == "p1":
            return
        m0 = A.mark()
        SCALE = 0.125
        S_BANKS, O_BANKS = [0, 1, 2, 3], [4, 5, 6, 7]
        qt = [A.alloc((8, 512), BF16) for _ in range(2)]
        d_qt = [Dep() for _ in range(2)]
        et = [A.alloc((512,), BF16) for _ in range(4)]
        d_et = [Dep() for _ in range(4)]
        ot = [A.alloc((1024,), BF16) for _ in range(2)]
        d_ot = [Dep() for _ in range(2)]
        sm = [A.alloc((8,), F32) for _ in range(4)]
        d_sm = [Dep() for _ in range(4)]
        ei = 0
        si = 0
        if swa:
            kT = A.alloc((4, T), BF16)
            d_kT = Dep()
            for g in range(4):
                mk.dma("sp", kT[:, g, :], kT_d[g * 128:(g + 1) * 128, :], r=[d_kd[g]], w=[d_kT])
            vv = A.alloc((NT, nvh, va), BF16)
            d_vv = Dep()
            for tt in range(NT):
                mk.dma("sp", vv[:, tt], v_d[tt * 128:(tt + 1) * 128, :].rearrange("p (h d) -> p h d", d=va),
                       r=[d_vd[tt]], w=[d_vv])
            mprev = A.alloc((512,), BF16)
            mnext = A.alloc((512,), BF16)
            d_msk = Dep()
            mk.dma("pool", mprev, C["mprev"][:, :], w=[d_msk])
            mk.dma("pool", mnext, C["mnext"][:, :], w=[d_msk])
            esink = A.alloc((16,), F32)
            d_esink = Dep()
            mk.dma("sp", esink, W["swa_sink"][0].partition_broadcast(128), w=[d_esink])
            mk.act(esink, esink, AF.Exp, r=[d_esink], w=[d_esink])
            for qi_, qb in enumerate(qtiles):
                b = qi_ % 2
                q_, dq_ = qt[b], d_qt[b]
                mk.dma("sp", q_[:, :, 0:128], qT_d.rearrange("(c p) t -> p c t", p=128)[:, :, qb * 128:(qb + 1) * 128],
                       r=d_qd, w=[dq_])
                o_, do_ = ot[b], d_ot[b]
                if qb < NTL:
                    keys = [(kt_, m_) for (kt_, m_) in ((qb - 1, "p"), (qb, None), (qb + 1, "n")) if 0 <= kt_ < NTL]
                    keys += [(NTL, None), (NTL + 1, None)]
                else:
                    keys = [(NTL, None), (NTL + 1, None)]
                for g in range(4):
                    pso, dpso = mk.ps_rot("o", O_BANKS)
                    for ki, (kt_, m_) in enumerate(keys):
                        e_, de_ = et[ei % 4], d_et[ei % 4]
                        ei += 1
                        for ph in range(2):
                            pss, dpss = mk.ps_rot("s", S_BANKS)
                            for c2 in range(2):
                                mk.mm(pss[:, c2 * 128:(c2 + 1) * 128],
                                      kT[ph * 64:(ph + 1) * 64, g, kt_ * 128:(kt_ + 1) * 128],
                                      q_[ph * 64:(ph + 1) * 64, 2 * g + c2, 0:128], True, True,
                                      r=[d_kT, dq_], w=[dpss])
                            mk.act(e_[:, ph * 256:(ph + 1) * 256], pss[:, 0:256], AF.Exp, scale=SCALE, r=[dpss], w=[de_])
                        if m_ is not None:
                            mk.tt("pool", e_, e_, mprev if m_ == "p" else mnext, ALU.mult, r=[d_msk], w=[de_])
                        for hh in range(4):
                            col = (hh % 2) * 256 + (hh // 2) * 128
                            mk.mm(pso[:, hh * va:(hh + 1) * va], e_[:, col:col + 128], vv[:, kt_, g, :],
                                  ki == 0 and hh == 0, ki == len(keys) - 1, r=[de_, d_vv], w=[dpso], sgc=True)
                    s_, ds_ = sm[si % 4], d_sm[si % 4]
                    si += 1
                    for hh in range(4):
                        hq = 4 * g + hh
                        mk.ts("dve", s_[:, hh:hh + 1], pso[:, hh * va + vd:hh * va + va], esink[:, hq:hq + 1], None, ALU.add,
                              r=[dpso, d_esink], w=[ds_])
                    mk.op("dve", lambda E, s_=s_: E.reciprocal(s_[:, 4:8], s_[:, 0:4]), r=[ds_], w=[ds_])
                    for hh in range(4):
                        hq = 4 * g + hh
                        mk.ts("dve", o_[:, hq * 64:(hq + 1) * 64], pso[:, hh * va:hh * va + vd], s_[:, 4 + hh:5 + hh], None,
                              ALU.mult, r=[dpso, ds_], w=[do_])
                mk.dma("sp", o_d[qb * 128:(qb + 1) * 128, :], o_, r=[do_], w=[d_od[qb]])
        else:
            lam_init = 0.8 - 0.6 * math.exp(-0.3 * i)
            lp = A.alloc((256,), F32)
            d_lp = Dep()
            mk.dma("sp", lp, W["diff_lambda"][0].rearrange("a b -> (a b)").partition_broadcast(128), w=[d_lp])
            lam = A.alloc((8,), F32)
            mk.tt("dve", lp[:, 0:64], lp[:, 0:64], lp[:, 64:128], ALU.mult, r=[d_lp], w=[d_lp])
            mk.tt("dve", lp[:, 128:192], lp[:, 128:192], lp[:, 192:256], ALU.mult, r=[d_lp], w=[d_lp])
            mk.op("dve", lambda E: E.reduce_sum(lam[:, 0:1], lp[:, 0:64], mybir.AxisListType.X), r=[d_lp], w=[d_lp])
            mk.op("dve", lambda E: E.reduce_sum(lam[:, 1:2], lp[:, 128:192], mybir.AxisListType.X), r=[d_lp], w=[d_lp])
            mk.act(lam[:, 0:2], lam[:, 0:2], AF.Exp, r=[d_lp], w=[d_lp])
            mk.tt("dve", lam[:, 2:3], lam[:, 1:2], lam[:, 0:1], ALU.subtract, r=[d_lp], w=[d_lp])
            mk.ts("dve", lam[:, 3:4], lam[:, 2:3], -lam_init, None, ALU.add, r=[d_lp], w=[d_lp])
            gsub = A.alloc((128,), F32)
            d_gsub = Dep()
            mk.dma("sp", gsub, W["diff_subln_g"][0].partition_broadcast(128), w=[d_gsub])
            mk.ts("dve", gsub, gsub, 1.0 - lam_init, None, ALU.mult, r=[d_gsub], w=[d_gsub])
            kT = A.alloc((4, T), BF16)
            d_kT = Dep()
            vv = A.alloc((NT, 4, va), BF16)
            d_vv = Dep()
            o0 = [A.alloc((4, 128), F32) for _ in range(2)]
            d_o0 = [Dep() for _ in range(2)]
            junk = A.alloc((128,), F32)
            otd = [A.alloc((4, 512), BF16) for _ in range(2)]
            qtl = [(q * 4, 4) for q in range(8)] + ([(NTL, 2)] if ctx_out else [])
            oi = 0
            for hg in range(2):
                for c in range(4):
                    mk.dma("sp", kT[:, c, :], kT_d[(hg * 4 + c) * 128:(hg * 4 + c + 1) * 128, :], r=[d_kd[hg * 4 + c]], w=[d_kT])
                for tt in range(NT):
                    mk.dma("sp", vv[:, tt], v_d[tt * 128:(tt + 1) * 128, hg * 4 * va:(hg + 1) * 4 * va].rearrange(
                        "p (h d) -> p h d", d=va), r=[d_vd[tt]], w=[d_vv])
                for qi_, (qa, nq4) in enumerate(qtl):
                    nq = nq4 * 128
                    b = qi_ % 2
                    q_, dq_ = qt[b], d_qt[b]
                    mk.dma("sp", q_[:, 0:4, 0:nq],
                           qT_d.rearrange("(c p) t -> p c t", p=128)[:, hg * 4:hg * 4 + 4, qa * 128:qa * 128 + nq],
                           r=d_qd, w=[dq_])
                    o_, do_ = otd[b], d_ot[b]
                    keys = list(range(NT)) if qa < NTL else [NTL, NTL + 1]
                    for pr in range(4):
                        ob_, dob_ = o0[oi % 2], d_o0[oi % 2]
                        oi += 1
                        for ii in range(2):
                            psoA, dpsoA = mk.ps_rot("o", O_BANKS)
                            psoB, dpsoB = mk.ps_rot("o", O_BANKS)
                            pend = []

                            def emit_pv(ki, kt_, e_, de_):
                                for s4 in range(nq4):
                                    pb, dpb = (psoA, dpsoA) if s4 < 2 else (psoB, dpsoB)
                                    co = (s4 % 2) * 256
                                    mk.mm(pb[:, co:co + va], e_[:, s4 * 128:(s4 + 1) * 128], vv[:, kt_, pr, :],
                                          ki == 0 and s4 % 2 == 0, ki == len(keys) - 1, r=[de_, d_vv], w=[dpb], sgc=True)

                            for ki, kt_ in enumerate(keys):
                                pss, dpss = mk.ps_rot("s", S_BANKS)
                                mk.mm(pss[:, 0:nq], kT[ii * 64:(ii + 1) * 64, pr, kt_ * 128:(kt_ + 1) * 128],
                                      q_[ii * 64:(ii + 1) * 64, pr, 0:nq], True, True, r=[d_kT, dq_], w=[dpss])
                                e_, de_ = et[ei % 4], d_et[ei % 4]
                                ei += 1
                                mk.act(e_[:, 0:nq], pss[:, 0:nq], AF.Exp, scale=SCALE, r=[dpss], w=[de_])
                                pend.append((ki, kt_, e_, de_))
                                if len(pend) > 2:
                                    emit_pv(*pend.pop(0))
                            while pend:
                                emit_pv(*pend.pop(0))
                            for s4 in range(nq4):
                                pb, dpb = (psoA, dpsoA) if s4 < 2 else (psoB, dpsoB)
                                co = (s4 % 2) * 256
                                s_, ds_ = sm[si % 4], d_sm[si % 4]
                                si += 1
                                mk.op("dve", lambda E, s_=s_, pb=pb, co=co: E.reciprocal(s_[:, 0:1], pb[:, co + vd:co + va]),
                                      r=[dpb], w=[ds_])
                                if ii == 0:
                                    mk.ts("dve", ob_[:, s4, :], pb[:, co:co + vd], s_[:, 0:1], None, ALU.mult,
                                          r=[dpb, ds_], w=[dob_])
                                else:
                                    mk.ts("dve", s_[:, 1:2], s_[:, 0:1], lam[:, 3:4], None, ALU.mult, r=[ds_, d_lp], w=[ds_])
                                    mk.stt("dve", ob_[:, s4, :], pb[:, co:co + vd], s_[:, 1:2], ob_[:, s4, :], ALU.mult, ALU.add,
                                           r=[dpb, ds_, dob_], w=[dob_])
                                    mk.act(junk, ob_[:, s4, :], AF.Square, r=[dob_], w=[ds_], accum_out=s_[:, 2:3])
                                    mk.ts("dve", s_[:, 3:4], s_[:, 2:3], 1.0 / 128.0, RMS_EPS, ALU.mult, ALU.add, r=[ds_], w=[ds_])
                                    mk.act(s_[:, 3:4], s_[:, 3:4], AF.Sqrt, r=[ds_], w=[ds_])
                                    mk.op("dve", lambda E, s_=s_: E.reciprocal(s_[:, 4:5], s_[:, 3:4]), r=[ds_], w=[ds_])
                                    mk.stt("dve", o_[:, s4, pr * 128:(pr + 1) * 128], ob_[:, s4, :], s_[:, 4:5], gsub,
                                           ALU.mult, ALU.mult, r=[dob_, ds_, d_gsub], w=[do_])
                    for s4 in range(nq4):
                        tq = qa + s4
                        mk.dma("sp", o_d[tq * 128:(tq + 1) * 128, hg * 512:(hg + 1) * 512], o_[:, s4, :],
                               r=[do_], w=[d_od[tq]])
        A.release(m0)
        if _os.environ.get("KSTOP") == "p2":
            return
        m0 = A.mark()
        wo = A.alloc((8, 1024), BF16)
        d_wo = Dep()
        self.load_w_bf16(wo, d_wo, wo_ap.rearrange("(k p) n -> p k n", p=128))
        ybias, d_yb = None, None
        if swa:
            ybias = A.alloc((1024,), F32)
            d_yb = Dep()
            mk.dma("sp", ybias, W["swa_out_b"][0].partition_broadcast(128), w=[d_yb])
        oin = [A.alloc((1024,), BF16) for _ in range(2)]
        d_oin = [Dep() for _ in range(2)]
        oT = [A.alloc((8, 128), BF16) for _ in range(2)]
        d_oT = [Dep() for _ in range(2)]
        for qi_, tt in enumerate(qtiles):
            b = qi_ % 2
            mk.dma("sp", oin[b], o_d[tt * 128:(tt + 1) * 128, :], r=[d_od[tt]], w=[d_oin[b]])
            ps, dps = mk.next_ps()
            psb = ps.bitcast(BF16)
            for k in range(8):
                mk.tr(psb[:, k * 128:(k + 1) * 128], oin[b][:, k * 128:(k + 1) * 128], self.identb,
                      r=[d_oin[b], self.d_identb], w=[dps])
            mk.copy("act", oT[b], psb[:, 0:1024].rearrange("p (k t) -> p k t", k=8), r=[dps], w=[d_oT[b]])
            ys, dys = [], []
            for hf in range(2):
                ps2, dps2 = mk.next_ps()
                for k in range(8):
                    mk.mm(ps2[:, :], oT[b][:, k, :], wo[:, k, hf * 512:(hf + 1) * 512], k == 0, k == 7,
                          r=[d_oT[b], d_wo], w=[dps2])
                ys.append(ps2[:, :])
                dys.append(dps2)
            self.post_tile(tt, ys, dys, 0, self.xres[tt * 128:(tt + 1) * 128, :], self.xdep[tt], ybias=ybias, d_ybias=d_yb)
        self.set_src_xres(qtiles)
        A.release(m0)

    class _Pool:
        def __init__(self, A, shape, dtype, n):
            self.t = [A.alloc(shape, dtype) for _ in range(n)]
            self.d = [Dep() for _ in range(n)]
            self.i = 0

        def get(self):
            k = self.i % len(self.t)
            self.i += 1
            return self.t[k], self.d[k]

    def mixer_delta(self, i):
        mk, A, W, C = self.mk, self.A, self.W, self.C
        nc = self.nc
        P = Prog._Pool
        def _dt(name, shape, dtype):
            if self.test_mode:
                self.dbg_names.append("dbg_" + name)
                return nc.dram_tensor("dbg_" + name, shape, dtype, kind="ExternalOutput").ap()
            return nc.dram_tensor(name, shape, dtype).ap()
        qT_d = _dt("dn_qT", [1024, T], BF16)
        kT_d = _dt("dn_kT", [1024, T], BF16)
        k_d = _dt("dn_k", [T, 1024], BF16)
        v_d = _dt("dn_v", [T, 2048], BF16)
        z_d = _dt("dn_z", [SEQ, 2048], BF16)
        of_d = _dt("dn_of", [SEQ, 2048], F32)
        d_qTd = [Dep() for _ in range(8)]
        d_kTd = [Dep() for _ in range(8)]
        d_kd = [Dep() for _ in range(8)]
        d_vd = [Dep() for _ in range(16)]
        d_zd = [Dep() for _ in range(NTL)]
        d_ofd = [Dep() for _ in range(NTL)]
        mg = A.mark()
        gb = A.alloc((NT, 64), F32)
        d_gb = [Dep() for _ in range(NT)]
        wsrc = W["delta_qkvz_w"][0].rearrange("(k p) n -> p k n", p=128)
        m0 = A.mark()
        hT = A.alloc((8, T), BF16)
        d_hT = [Dep() for _ in range(NT)]
        m1 = A.mark()
        h32 = A.alloc((8, 128), F32)
        d_h32 = Dep()
        wba = A.alloc((8, 64), F32)
        d_wba = Dep()
        mk.dma("sp", wba, W["delta_ba_w"][0].rearrange("(k p) n -> p k n", p=128), w=[d_wba])
        dtb = A.alloc((32,), F32)
        nea = A.alloc((32,), F32)
        d_cst = Dep()
        mk.dma("sp", dtb, W["delta_dt_bias"][0].rearrange("d h -> (d h)").partition_broadcast(128), w=[d_cst])
        mk.dma("sp", nea, W["delta_a_log"][0].rearrange("d h -> (d h)").partition_broadcast(128), w=[d_cst])
        mk.act(nea, nea, AF.Exp, r=[d_cst], w=[d_cst])
        mk.ts("dve", nea, nea, -1.0, None, ALU.mult, r=[d_cst], w=[d_cst])
        v3 = lambda ap: ap.rearrange("p (d h) -> p d h", d=2)
        for tt in range(NT):
            self.prep_tile(tt, 0, hT, d_hT[tt], tt * 128, h32=h32, d_h32=d_h32)
            ps, dps = mk.next_ps()
            for kk in range(8):
                mk.mm(ps[:, 0:64], h32[:, kk, :], wba[:, kk, :], kk == 0, kk == 7, r=[d_h32, d_wba], w=[dps])
            bav = ps[:, 0:64].rearrange("p (d s h) -> p d s h", d=2, s=2)
            gv = v3(gb[:, tt, 0:32])
            mk.tt("dve", gv, bav[:, :, 1, :], v3(dtb), ALU.add, r=[dps, d_cst], w=[d_gb[tt]])
            mk.act(gb[:, tt, 0:32], gb[:, tt, 0:32], AF.Exp, r=[d_gb[tt]], w=[d_gb[tt]])
            mk.act(gb[:, tt, 0:32], gb[:, tt, 0:32], AF.Ln, bias=self.ones[:, 0:1], r=[d_gb[tt], self.d_ones], w=[d_gb[tt]])
            mk.tt("dve", gb[:, tt, 0:32], gb[:, tt, 0:32], nea, ALU.mult, r=[d_cst], w=[d_gb[tt]])
            mk.act(v3(gb[:, tt, 32:64]), bav[:, :, 0, :], AF.Sigmoid, r=[dps], w=[d_gb[tt]])
        self.dbg("dn_gb", gb, d_gb)
        A.release(m1)
        m1 = A.mark()
        wc5 = A.alloc((32, 5), F32)
        d_wc5 = Dep()
        wtmp = A.alloc((32,), F32)
        d_wtmp = Dep()
        for k in range(5):
            self.load_featmajor(wtmp, d_wtmp, W["delta_conv_w"][0, k], 32)
            mk.copy("dve", wc5[:, :, k], wtmp, r=[d_wtmp], w=[d_wc5])
        onesb = A.alloc((128,), BF16)
        d_onesb = Dep()
        mk.memset("dve", onesb, 1.0, w=[d_onesb])
        LP = SEQ + 4 + CTX + 4
        pbuf = A.alloc((LP,), F32)
        d_pbuf = Dep()
        mk.memset("dve", pbuf, 0.0, w=[d_pbuf])
        acc = A.alloc((T,), F32)
        d_acc = Dep()
        sq = A.alloc((T,), BF16)
        d_sq = Dep()
        obp = P(A, (T,), BF16, 2)
        wcp = P(A, (8, 128), BF16, 2)
        rnp = P(A, (512,), F32, 2)
        stp = P(A, (8, 128), BF16, 2)
        tts = [(q * 512, 512, 2 + q * 512) for q in range(8)] + [(SEQ, CTX, SEQ + 6)]
        QS = 128.0 ** -0.5
        for c in range(32):
            wcb, dwc = wcp.get()
            self.load_w_bf16(wcb, dwc, wsrc[:, :, c * 128:(c + 1) * 128])
            for (t0, n, co) in tts:
                ps, dps = mk.next_ps()
                for kk in range(8):
                    mk.mm(ps[:, 0:n], wcb[:, kk, :], hT[:, kk, t0:t0 + n], kk == 0, kk == 7,
                          r=[dwc] + d_hT[t0 // 128:(t0 + n) // 128], w=[dps])
                mk.copy("act", pbuf[:, co:co + n], ps[:, 0:n], r=[dps], w=[d_pbuf])
            for (s0, L, base) in ((0, SEQ, 0), (SEQ, CTX, SEQ + 4)):
                for (a0, a1, eng) in ((0, L, "dve"),):
                    av = acc[:, s0 + a0:s0 + a1]
                    n_ = a1 - a0
                    mk.ts(eng, av, pbuf[:, base + a0:base + a0 + n_], wc5[:, c, 0:1], None, ALU.mult,
                          r=[d_pbuf, d_wc5], w=[d_acc])
                    for k in range(1, 5):
                        mk.stt(eng, av, pbuf[:, base + a0 + k:base + a0 + k + n_], wc5[:, c, k:k + 1], av, ALU.mult, ALU.add,
                               r=[d_pbuf, d_wc5], w=[d_acc])
            ob, dob = obp.get()
            if c < 16:
                mk.act(acc, acc, AF.Silu, r=[d_acc], w=[d_acc])
                mk.act(sq, acc, AF.Square, r=[d_acc], w=[d_sq])
                for (t0, n, co) in tts:
                    ps, dps = mk.next_ps()
                    mk.mm(ps[:, 0:n], onesb, sq[:, t0:t0 + n], True, True, r=[d_onesb, d_sq], w=[dps])
                    rn, drn = rnp.get()
                    mk.ts("dve", rn[:, 0:n], ps[:, 0:n], RMS_EPS, None, ALU.add, r=[dps], w=[drn])
                    mk.act(rn[:, 0:n], rn[:, 0:n], AF.Sqrt, r=[drn], w=[drn])
                    mk.op("dve", lambda E, rn=rn, n=n: E.reciprocal(rn[:, 0:n], rn[:, 0:n]), r=[drn], w=[drn])
                    mk.stt("dve", ob[:, t0:t0 + n], acc[:, t0:t0 + n], QS if c < 8 else 1.0, rn[:, 0:n], ALU.mult, ALU.mult,
                           r=[d_acc, drn], w=[dob])
                if c < 8:
                    mk.dma("sp", qT_d[c * 128:(c + 1) * 128, :], ob, r=[dob], w=[d_qTd[c]])
                else:
                    mk.dma("sp", kT_d[(c - 8) * 128:(c - 7) * 128, :], ob, r=[dob], w=[d_kTd[c - 8]])
            else:
                mk.act(ob, acc, AF.Silu, r=[d_acc], w=[dob])
            if c >= 8:
                dst, ddst, cc = (k_d, d_kd[c - 8], c - 8) if c < 16 else (v_d, d_vd[c - 16], c - 16)
                dview = dst.rearrange("(t p) f -> p t f", p=128)
                for t8 in range(0, NT, 8):
                    nt8 = min(8, NT - t8)
                    ps, dps = mk.next_ps()
                    psb = ps.bitcast(BF16)
                    for q in range(nt8):
                        mk.tr(psb[:, q * 128:(q + 1) * 128], ob[:, (t8 + q) * 128:(t8 + q + 1) * 128], self.identb,
                              r=[dob, self.d_identb], w=[dps])
                    st, dst_ = stp.get()
                    mk.copy("act", st[:, 0:nt8, :], psb[:, 0:nt8 * 128].rearrange("p (t f) -> p t f", f=128), r=[dps], w=[dst_])
                    mk.dma("sp", dview[:, t8:t8 + nt8, cc * 128:(cc + 1) * 128], st[:, 0:nt8, :], r=[dst_], w=[ddst])
        A.release(m1)
        m1 = A.mark()
        wz = A.alloc((8, 2048), BF16)
        d_wz = Dep()
        self.load_w_bf16(wz, d_wz, wsrc[:, :, 4096:6144])
        ngb = A.alloc((4, 128), F32)
        d_ngb = Dep()
        for q in range(4):
            mk.dma("sp", ngb[:, q, :], W["delta_norm_g"][0].partition_broadcast(128), w=[d_ngb])
        zsp = P(A, (512,), F32, 2)
        ztp = P(A, (2048,), BF16, 2)
        for tt in range(NTL):
            zt, dzt = ztp.get()
            for cg in range(4):
                ps, dps = mk.next_ps()
                for kk in range(8):
                    mk.mm(ps[:, :], hT[:, kk, tt * 128:(tt + 1) * 128], wz[:, kk, cg * 512:(cg + 1) * 512], kk == 0, kk == 7,
                          r=[d_wz, d_hT[tt]], w=[dps])
                zs, dzs = zsp.get()
                mk.act(zs, ps[:, :], AF.Silu, r=[dps], w=[dzs])
                mk.tt("dve", zt[:, cg * 512:(cg + 1) * 512], zs, ngb.rearrange("p a b -> p (a b)"), ALU.mult,
                      r=[dzs, d_ngb], w=[dzt])
            mk.dma("sp", z_d[tt * 128:(tt + 1) * 128, :], zt, r=[dzt], w=[d_zd[tt]])
        A.release(m0)
        m0 = A.mark()
        cst = {}
        d_c2 = Dep()
        for nm in ("triF", "triB", "m2F", "m2B", "selF", "selB"):
            cst[nm] = A.alloc((128,), F32)
            mk.dma("sp", cst[nm], C[nm][:, :], w=[d_c2])
        S = A.alloc((16, 128), F32)
        Sb = A.alloc((16, 128), BF16)
        d_S = [Dep() for _ in range(16)]
        wo = A.alloc((16, 1024), BF16)
        d_wo = Dep()
        self.load_w_bf16(wo, d_wo, W["delta_out_w"][0].rearrange("(k p) n -> p k n", p=128))
        ldq = P(A, (8, 128), BF16, 2)
        ldkT = P(A, (8, 128), BF16, 2)
        ldk = P(A, (8, 128), BF16, 2)
        ldv = P(A, (16, 128), BF16, 2)
        smp = P(A, (8, 16), F32, 2)
        kkp = P(A, (128,), F32, 2)
        qkp = P(A, (128,), F32, 2)
        bmp = P(A, (128,), F32, 2)
        dmp = P(A, (128,), F32, 2)
        dtp = P(A, (128,), F32, 2)
        abp = P(A, (128,), F32, 8)
        ttp = P(A, (128,), F32, 3)
        tbp = P(A, (128,), BF16, 2)
        vbp = P(A, (128,), BF16, 2)
        kbp = P(A, (128,), BF16, 2)
        usp = P(A, (128,), F32, 2)
        wtp = P(A, (128,), BF16, 2)
        vnp = P(A, (128,), BF16, 2)
        itp = P(A, (128,), BF16, 2)
        o1p = P(A, (128,), F32, 2)
        kdp = P(A, (128,), BF16, 2)
        otp = P(A, (16, 128), F32, 1)
        ofp = P(A, (16, 128), F32, 1)
        zgp = P(A, (16, 128), BF16, 1)
        onp = P(A, (16, 128), BF16, 1)
        oTp = P(A, (16, 128), BF16, 1)
        s16p = P(A, (4, 16), F32, 2)
        sqt = A.alloc((16, 128), F32)
        d_sqt = Dep()
        kTv = kT_d.rearrange("(h p) t -> p h t", p=128)
        qTv = qT_d.rearrange("(h p) t -> p h t", p=128)
        evi = [0]

        def evac(out, ps, dps, dout):
            evi[0] += 1
            mk.copy("act" if evi[0] % 2 == 0 else "dve", out, ps, r=[dps], w=[dout])

        for d in range(2):
            order = ([NTL, NTL + 1] + list(range(NTL))) if d == 0 else ([NTL + 1, NTL] + list(range(NTL - 1, -1, -1)))
            tri, m2, sel = (cst["triF"], cst["m2F"], cst["selF"]) if d == 0 else (cst["triB"], cst["m2B"], cst["selB"])
            mk.memset("dve", S, 0.0, w=d_S)
            mk.memset("dve", Sb, 0.0, w=d_S)
            for tt in order:
                lat = tt < NTL
                tok = slice(tt * 128, (tt + 1) * 128)
                kTt, dkTt = ldkT.get()
                mk.dma("sp", kTt, kTv[:, :, tok], r=d_kTd, w=[dkTt])
                kt, dkt = ldk.get()
                mk.dma("sp", kt, k_d[tok, :].rearrange("p (h f) -> p h f", f=128), r=d_kd, w=[dkt])
                vt, dvt = ldv.get()
                mk.dma("sp", vt, v_d[tok, :].rearrange("p (h f) -> p h f", f=128), r=d_vd, w=[dvt])
                if lat:
                    qTt, dqTt = ldq.get()
                    mk.dma("sp", qTt, qTv[:, :, tok], r=d_qTd, w=[dqTt])
                g_d = gb[:, tt, d * 16:(d + 1) * 16]
                b_d = gb[:, tt, 32 + d * 16:32 + (d + 1) * 16]
                sm_, dsm = smp.get()
                gc, gl, eg, egl, kds, negb, beg = [sm_[:, q, :] for q in range(7)]
                ps, dps = mk.next_ps()
                mk.mm(ps[:, 0:16], tri, g_d, True, True, r=[d_c2, d_gb[tt]], w=[dps])
                mk.copy("dve", gc, ps[:, 0:16], r=[dps], w=[dsm])
                ps, dps = mk.next_ps()
                mk.mm(ps[:, 0:16], sel, gc, True, True, r=[d_c2, dsm], w=[dps])
                mk.copy("dve", gl, ps[:, 0:16], r=[dps], w=[dsm])
                mk.act(eg, gc, AF.Exp, r=[dsm], w=[dsm])
                mk.act(egl, gl, AF.Exp, r=[dsm], w=[dsm])
                mk.tt("dve", kds, gl, gc, ALU.subtract, r=[dsm], w=[dsm])
                mk.act(kds, kds, AF.Exp, r=[dsm], w=[dsm])
                mk.ts("dve", negb, b_d, -1.0, None, ALU.mult, r=[d_gb[tt]], w=[dsm])
                mk.tt("dve", beg, b_d, eg, ALU.mult, r=[d_gb[tt], dsm], w=[dsm])
                if lat:
                    ot, dot = otp.get()
                    if d == 1:
                        oft, doft = ofp.get()
                        mk.dma("sp", oft, of_d[tok, :].rearrange("p (h f) -> p h f", f=128), r=[d_ofd[tt]], w=[doft])
                for hq in range(8):
                    ps, dps = mk.next_ps()
                    mk.mm(ps[:, 0:128], kTt[:, hq, :], kTt[:, hq, :], True, True, r=[dkTt], w=[dps])
                    kkm, dkkm = kkp.get()
                    mk.tt("dve", kkm, ps[:, 0:128], m2, ALU.mult, r=[dps, d_c2], w=[dkkm])
                    if lat:
                        ps, dps = mk.next_ps()
                        mk.mm(ps[:, 0:128], kTt[:, hq, :], qTt[:, hq, :], True, True, r=[dkTt, dqTt], w=[dps])
                        qkm, dqkm = qkp.get()
                        mk.tt("dve", qkm, ps[:, 0:128], tri, ALU.mult, r=[dps, d_c2], w=[dqkm])
                    for hv in (2 * hq, 2 * hq + 1):
                        bm, dbm = bmp.get()
                        mk.ts("pool", bm, m2, g_d[:, hv:hv + 1], None, ALU.mult, r=[d_c2, d_gb[tt]], w=[dbm])
                        ps, dps = mk.next_ps()
                        mk.mm(ps[:, 0:128], tri, bm, True, True, r=[d_c2, dbm], w=[dps])
                        dm, ddm = dmp.get()
                        mk.act(dm, ps[:, 0:128], AF.Exp, r=[dps], w=[ddm])
                        a_k, da_k = abp.get()
                        mk.stt("dve", a_k, kkm, negb[:, hv:hv + 1], dm, ALU.mult, ALU.mult, r=[dkkm, dsm, ddm], w=[da_k])
                        ps, dps = mk.next_ps()
                        mk.tr(ps[:, 0:128], a_k, self.ident, r=[da_k, self.d_ident], w=[dps])
                        b_k, db_k = abp.get()
                        evac(b_k, ps[:, 0:128], dps, db_k)
                        tT, dtT = ttp.get()
                        mk.tt("pool", tT, b_k, self.ident, ALU.add, r=[db_k, self.d_ident], w=[dtT])
                        for lvl in range(1, 7):
                            ps, dps = mk.next_ps()
                            mk.mm(ps[:, 0:128], b_k, a_k, True, True, r=[db_k, da_k], w=[dps])
                            a_n, da_n = abp.get()
                            evac(a_n, ps[:, 0:128], dps, da_n)
                            if lvl < 6:
                                ps, dps = mk.next_ps()
                                mk.mm(ps[:, 0:128], a_k, b_k, True, True, r=[db_k, da_k], w=[dps])
                                b_n, db_n = abp.get()
                                evac(b_n, ps[:, 0:128], dps, db_n)
                            ps, dps = mk.next_ps()
                            mk.mm(ps[:, 0:128], a_n, tT, True, True, r=[da_n, dtT], w=[dps])
                            tN, dtN = ttp.get()
                            mk.tt("dve", tN, ps[:, 0:128], tT, ALU.add, r=[dps, dtT], w=[dtN])
                            tT, dtT = tN, dtN
                            a_k, da_k = a_n, da_n
                            if lvl < 6:
                                b_k, db_k = b_n, db_n
                        tTf, dtTf = tT, dtT
                        tT, dtT = tbp.get()
                        mk.copy("act", tT, tTf, r=[dtTf], w=[dtT])
                        vb, dvb = vbp.get()
                        mk.ts("pool", vb, vt[:, hv, :], b_d[:, hv:hv + 1], None, ALU.mult, r=[dvt, d_gb[tt]], w=[dvb])
                        kb, dkb = kbp.get()
                        mk.ts("pool", kb, kt[:, hq, :], beg[:, hv:hv + 1], None, ALU.mult, r=[dkt, dsm], w=[dkb])
                        ps, dps = mk.next_ps()
                        mk.mm(ps[:, 0:128], tT, vb, True, True, r=[dtT, dvb], w=[dps])
                        us, dus = usp.get()
                        evac(us, ps[:, 0:128], dps, dus)
                        ps, dps = mk.next_ps()
                        mk.mm(ps[:, 0:128], kb, tT, True, True, r=[dkb, dtT], w=[dps])
                        wT, dwT = wtp.get()
                        evac(wT, ps[:, 0:128], dps, dwT)
                        ps, dps = mk.next_ps()
                        mk.mm(ps[:, 0:128], wT, Sb[:, hv, :], True, True, r=[dwT, d_S[hv]], w=[dps])
                        vn, dvn = vnp.get()
                        mk.tt("dve", vn, us, ps[:, 0:128], ALU.subtract, r=[dus, dps], w=[dvn])
                        if lat:
                            ps, dps = mk.next_ps()
                            mk.mm(ps[:, 0:128], bm, tri, True, True, r=[d_c2, dbm], w=[dps])
                            dT, ddT = dtp.get()
                            mk.act(dT, ps[:, 0:128], AF.Exp, r=[dps], w=[ddT])
                            it, dit = itp.get()
                            mk.tt("pool", it, qkm, dT, ALU.mult, r=[dqkm, ddT], w=[dit])
                            ps, dps = mk.next_ps()
                            mk.mm(ps[:, 0:128], qTt[:, hq, :], Sb[:, hv, :], True, True, r=[dqTt, d_S[hv]], w=[dps])
                            o1, do1 = o1p.get()
                            mk.ts("dve", o1, ps[:, 0:128], eg[:, hv:hv + 1], None, ALU.mult, r=[dps, dsm], w=[do1])
                            ps, dps = mk.next_ps()
                            mk.mm(ps[:, 0:128], it, vn, True, True, r=[dit, dvn], w=[dps])
                            if d == 0:
                                mk.tt("dve", ot[:, hv, :], ps[:, 0:128], o1, ALU.add, r=[dps, do1], w=[dot])
                            else:
                                mk.tt("dve", o1, ps[:, 0:128], o1, ALU.add, r=[dps], w=[do1])
                                mk.tt("pool", ot[:, hv, :], o1, oft[:, hv, :], ALU.add, r=[do1, doft], w=[dot])
                        kd, dkd = kdp.get()
                        mk.ts("pool", kd, kt[:, hq, :], kds[:, hv:hv + 1], None, ALU.mult, r=[dkt, dsm], w=[dkd])
                        ps, dps = mk.next_ps()
                        mk.mm(ps[:, 0:128], kd, vn, True, True, r=[dkd, dvn], w=[dps])
                        mk.stt("dve", S[:, hv, :], S[:, hv, :], egl[:, hv:hv + 1], ps[:, 0:128], ALU.mult, ALU.add,
                               r=[dps, dsm], w=[d_S[hv]])
                        mk.copy("act", Sb[:, hv, :], S[:, hv, :], r=[], w=[d_S[hv]])
                if lat and d == 0:
                    mk.dma("sp", of_d[tok, :].rearrange("p (h f) -> p h f", f=128), ot, r=[dot], w=[d_ofd[tt]])
                if lat and d == 1:
                    zg, dzg = zgp.get()
                    mk.dma("sp", zg, z_d[tok, :].rearrange("p (h f) -> p h f", f=128), r=[d_zd[tt]], w=[dzg])
                    s16, ds16 = s16p.get()
                    mk.tt("pool", sqt, ot, ot, ALU.mult, r=[dot], w=[d_sqt])
                    mk.op("dve", lambda E, s16=s16: E.reduce_sum(s16[:, 0, :], sqt, mybir.AxisListType.X), r=[d_sqt], w=[ds16])
                    mk.ts("dve", s16[:, 1, :], s16[:, 0, :], 1.0 / 128.0, RMS_EPS, ALU.mult, ALU.add, r=[ds16], w=[ds16])
                    mk.act(s16[:, 1, :], s16[:, 1, :], AF.Sqrt, r=[ds16], w=[ds16])
                    mk.op("dve", lambda E, s16=s16: E.reciprocal(s16[:, 2, :], s16[:, 1, :]), r=[ds16], w=[ds16])
                    on, don = onp.get()
                    for hv in range(16):
                        mk.stt("dve", on[:, hv, :], ot[:, hv, :], s16[:, 2, hv:hv + 1], zg[:, hv, :],
                               ALU.mult, ALU.mult, r=[dot, ds16, dzg], w=[don])
                    oT, doT = oTp.get()
                    for hf in range(2):
                        ps, dps = mk.next_ps()
                        psb = ps.bitcast(BF16)
                        for q in range(8):
                            mk.tr(psb[:, q * 128:(q + 1) * 128], on[:, hf * 8 + q, :], self.identb,
                                  r=[don, self.d_identb], w=[dps])
                        mk.copy("act", oT[:, hf * 8:(hf + 1) * 8, :], psb[:, 0:1024].rearrange("p (k t) -> p k t", k=8),
                                r=[dps], w=[doT])
                    ys, dys = [], []
                    for hf in range(2):
                        ps2, dps2 = mk.next_ps()
                        for k in range(16):
                            mk.mm(ps2[:, :], oT[:, k, :], wo[:, k, hf * 512:(hf + 1) * 512], k == 0, k == 15,
                                  r=[doT, d_wo], w=[dps2])
                        ys.append(ps2[:, :])
                        dys.append(dps2)
                    self.post_tile(tt, ys, dys, 0, self.xres[tt * 128:(tt + 1) * 128, :], self.xdep[tt])
        self.set_src_xres(range(NTL))
        A.release(mg)

    def moe(self, i, last, final):
        mk, A, W = self.mk, self.A, self.W
        ntile = NTL if last else NT
        GT = 9
        groups = [(0, 9), (9, 18), (18, 26), (26, 34)] if not last else [(0, 8), (8, 16), (16, 24), (24, 32)]
        m0 = A.mark()
        wr = A.alloc((8, 32), F32)
        d_wr = Dep()
        mk.dma("sp", wr, W["router_w"][i].rearrange("(k p) n -> p k n", p=128), w=[d_wr])
        brbc = A.alloc((32,), F32)
        d_br = Dep()
        mk.dma("sp", brbc, W["router_b"][i].partition_broadcast(128), w=[d_br])
        b2 = A.alloc((1024,), F32, parts=32)
        d_b2 = Dep()
        mk.dma("sp", b2, W["exp_b2"][i], w=[d_b2])
        b1F = A.alloc((NE, 16), F32)
        d_b1F = Dep()
        for e in range(NE):
            self.load_featmajor(b1F[:, e, :], d_b1F, W["exp_b1"][i, e], 16)
        w1b = A.alloc((8, 2048), BF16)
        d_w1 = Dep()
        w2b = A.alloc((8, 1024), BF16)
        d_w2 = Dep()
        hT = A.alloc((8, GT * 128), BF16)
        acc = A.alloc((GT, 1024), F32)
        gw = A.alloc((GT, 32), F32)
        actT = A.alloc((8, GT * 128), BF16)
        h32 = A.alloc((8, 128), F32)
        d_h32 = Dep()
        lg = A.alloc((32,), F32)
        top8 = A.alloc((8,), F32)
        sm = A.alloc((4,), F32)
        ex = A.alloc((32,), F32)
        d_lg = Dep()
        gwT = A.alloc((128,), F32, parts=32)
        d_gwT = Dep()
        NTB = 3
        tg = [A.alloc((512,), F32) for _ in range(NTB)]
        tsg = [A.alloc((512,), F32) for _ in range(NTB)]
        tu = [A.alloc((512,), F32) for _ in range(NTB)]
        d_tg = [Dep() for _ in range(NTB)]
        d_tsg = [Dep() for _ in range(NTB)]
        d_tu = [Dep() for _ in range(NTB)]
        mk.ts("dve", b1F[:, :, 8:16], b1F[:, :, 8:16], 1.0, None, ALU.add, r=[d_b1F], w=[d_b1F])
        w1src = W["exp_w1"][i]
        w2src = W["exp_w2"][i]
        ti = 0
        for (ga, gb) in groups:
            ng = gb - ga
            d_hT = [Dep() for _ in range(ng)]
            d_acc = [Dep() for _ in range(ng)]
            d_gw = [Dep() for _ in range(ng)]
            d_actT = [Dep() for _ in range(ng)]
            for lt in range(ng):
                tt = ga + lt
                self.prep_tile(tt, 3, hT, d_hT[lt], lt * 128, h32=h32, d_h32=d_h32)
                ps, dps = mk.next_ps()
                for kk in range(8):
                    mk.mm(ps[:, 0:32], h32[:, kk, :], wr[:, kk, :], kk == 0, kk == 7, r=[d_h32, d_wr], w=[dps])
                mk.tt("dve", lg, ps[:, 0:32], brbc, ALU.add, r=[dps, d_br], w=[d_lg])
                mk.op("dve", lambda E: E.max(top8, lg), r=[], w=[d_lg])
                mk.ts("dve", sm[:, 0:1], top8[:, 0:1], -1.0, None, ALU.mult, r=[], w=[d_lg])
                mk.act(ex, lg, AF.Exp, bias=sm[:, 0:1], scale=1.0, r=[d_lg], w=[d_lg])
                mk.stt("dve", ex, lg, top8[:, 3:4], ex, ALU.is_ge, ALU.mult, r=[d_lg], w=[d_lg])
                mk.op("dve", lambda E: E.reduce_sum(sm[:, 1:2], ex, mybir.AxisListType.X), r=[], w=[d_lg])
                mk.op("dve", lambda E: E.reciprocal(sm[:, 2:3], sm[:, 1:2]), r=[], w=[d_lg])
                g_ap = gw[:, lt, :]
                mk.ts("dve", g_ap, ex, sm[:, 2:3], None, ALU.mult, r=[d_lg], w=[d_gw[lt]])
                ps, dps = mk.next_ps()
                mk.tr(ps[0:32, 0:128], g_ap, self.ident, r=[d_gw[lt], self.d_ident], w=[dps])
                mk.copy("dve", gwT, ps[0:32, 0:128], r=[dps], w=[d_gwT])
                for hf in range(2):
                    ps, dps = mk.next_ps()
                    mk.mm(ps[:, :], gwT, b2[:, hf * 512:(hf + 1) * 512], True, True, r=[d_gwT, d_b2], w=[dps])
                    mk.copy("act", acc[:, lt, hf * 512:(hf + 1) * 512], ps[:, :], r=[dps], w=[d_acc[lt]])
            t5 = [(a, min(4, ng - a)) for a in range(0, ng, 4)]
            for e in range(NE):
                self.load_w_bf16(w1b, d_w1, w1src[e].rearrange("(k p) n -> p k n", p=128))
                self.load_w_bf16(w2b, d_w2, w2src[e].rearrange("(k p) n -> p k n", p=128))
                for (a, nt4) in t5:
                    n = nt4 * 128
                    c0 = a * 128
                    rd = d_hT[a:a + nt4]
                    for j in range(8):
                        psg, dpsg = mk.next_ps()
                        for kk in range(8):
                            mk.mm(psg[:, 0:n], w1b[:, kk, j * 128:(j + 1) * 128], hT[:, kk, c0:c0 + n], kk == 0, kk == 7,
                                  r=[d_w1] + rd, w=[dpsg])
                        psu, dpsu = mk.next_ps()
                        for kk in range(8):
                            mk.mm(psu[:, 0:n], w1b[:, kk, 1024 + j * 128:1024 + (j + 1) * 128], hT[:, kk, c0:c0 + n],
                                  kk == 0, kk == 7, r=[d_w1] + rd, w=[dpsu])
                        tb = ti % NTB
                        ti += 1
                        g_, s_, u_ = tg[tb], tsg[tb], tu[tb]
                        dg_, ds_, du_ = d_tg[tb], d_tsg[tb], d_tu[tb]
                        mk.ts("dve", g_[:, 0:n], psg[:, 0:n], b1F[:, e, j:j + 1], 7.0, ALU.add, ALU.min,
                              r=[dpsg, d_b1F], w=[dg_])
                        mk.act(s_[:, 0:n], g_[:, 0:n], AF.Sigmoid, scale=1.702, r=[dg_], w=[ds_])
                        mk.ts("dve", u_[:, 0:n], psu[:, 0:n], b1F[:, e, 8 + j:9 + j], -6.0, ALU.add, ALU.max,
                              r=[dpsu, d_b1F], w=[du_])
                        mk.tt("pool", s_[:, 0:n], g_[:, 0:n], s_[:, 0:n], ALU.mult, r=[dg_, ds_], w=[ds_])
                        mk.stt("dve", actT[:, j, c0:c0 + n], u_[:, 0:n], 8.0, s_[:, 0:n], ALU.min, ALU.mult,
                               r=[du_, ds_], w=d_actT[a:a + nt4])
                for lt in range(ng):
                    for hf in range(2):
                        ps, dps = mk.next_ps()
                        for j in range(8):
                            mk.mm(ps[:, :], actT[:, j, lt * 128:(lt + 1) * 128], w2b[:, j, hf * 512:(hf + 1) * 512],
                                  j == 0, j == 7, r=[d_actT[lt], d_w2], w=[dps])
                        av = acc[:, lt, hf * 512:(hf + 1) * 512]
                        mk.stt("dve", av, ps[:, :], gw[:, lt, e:e + 1], av, ALU.mult, ALU.add,
                               r=[dps, d_gw[lt]], w=[d_acc[lt]])
            for lt in range(ng):
                tt = ga + lt
                if final:
                    dst, ddst = self.out_d[tt * 128:(tt + 1) * 128, :], self.outdep[tt]
                else:
                    dst, ddst = self.xres[tt * 128:(tt + 1) * 128, :], self.xdep[tt]
                self.post_tile(tt, [acc[:, lt, 0:512], acc[:, lt, 512:1024]], [d_acc[lt]], 1, dst, ddst)
            if not final:
                self.set_src_xres(range(ga, gb))
        A.release(m0)

    def build(self):
        for li, i in enumerate(self.layers):
            last = (i == DEPTH - 1)
            final = (li == len(self.layers) - 1)
            self.phase_mod(i)
            kind = i % 4
            if kind == 0:
                self.mixer_conv(i, not last)
            elif kind in (1, 2):
                self.mixer_attn(i, kind, not last)
            else:
                self.mixer_delta(i)
            if self.do_moe:
                self.moe(i, last, final)
            else:
                self.copy_xres_to_out()
        self.mk.finish()

    def copy_xres_to_out(self):
        for tt in range(NT):
            b = self.xt_i % 2
            self.xt_i += 1
            xt, dxt = self.xt[b], self.d_xt[b]
            self.mk.dma("sp", xt, self.src[tt], r=[self.xdep[tt]], w=[dxt])
            self.mk.dma("sp", self.out_d[tt * 128:(tt + 1) * 128, :], xt, r=[dxt], w=[self.outdep[tt]])


def build_program(layers=(0, 1, 2, 3), test_mode=False, do_moe=True):
    nc = bass.Bass("TRN2", target_bir_lowering=False)
    p = Prog(nc, layers, test_mode=test_mode, do_moe=do_moe)
    p.build()
    return nc, p


def kernel(**inputs):
    nc, p = build_program()
    consts = host_constants()
    in_maps = []
    for b in range(8):
        m = {"x": np.ascontiguousarray(inputs["x"][b]), "ctx": np.ascontiguousarray(inputs["ctx"][b]),
             "c": np.ascontiguousarray(inputs["c"][b]), "c_ctx": np.ascontiguousarray(inputs["c_ctx"])}
        for name, _ in W_SPECS:
            m[name] = np.ascontiguousarray(inputs[name])
        m.update(consts)
        in_maps.append(m)
    res = run_bass_kernel_spmd(nc, in_maps, core_ids=list(range(8)))
    return np.stack([np.asarray(r["out"]) for r in res.results], axis=0).astype(np.float32)
```

```python
import math
import numpy as np
from contextlib import ExitStack
import concourse.bass as bass
import concourse.mybir as mybir
from concourse.bass_utils import run_bass_kernel_spmd

F32 = mybir.dt.float32
BF16 = mybir.dt.bfloat16
AF = mybir.ActivationFunctionType
ALU = mybir.AluOpType

D = 1024
SEQ = 4096
CTX = 256
T = SEQ + CTX
NT = T // 128
NTL = SEQ // 128
DEPTH = 4
ALPHA = (2 * DEPTH) ** 0.25
LN_EPS = 1e-5
RMS_EPS = 1e-6
NE = 32
ENGS = ("pe", "act", "dve", "pool", "sp")
N_DMA_SEMS = 12


class Dep:
    __slots__ = ("w", "r")

    def __init__(self):
        self.w = None
        self.r = {}


class MK:
    def __init__(self, nc):
        self.nc = nc
        self.es = ExitStack()
        self.prog = {e: [] for e in ENGS}
        self.cnt = {e: 0 for e in ENGS}
        self.sems = {e: self.es.enter_context(nc.semaphore("sem_" + e)) for e in ENGS}
        self.dsem, self.dsem_uses, self.dq_count = {}, {}, {}
        for q in ("sp", "pool", "act"):
            self.dsem[q] = [self.es.enter_context(nc.semaphore(f"dma_{q}_{i}")) for i in range(N_DMA_SEMS)]
            self.dsem_uses[q] = [0] * N_DMA_SEMS
            self.dq_count[q] = 0
        self.waited = {e: {} for e in ENGS}
        self.semobj = {("e", e): self.sems[e] for e in ENGS}
        for q in self.dsem:
            for i, s in enumerate(self.dsem[q]):
                self.semobj[("d", q, i)] = s
        self.n_inst = 0
        self.n_wait = 0
        self.ps_banks = None
        self.ps_deps = None
        self.ps_i = 0

    def sbuf(self, name, shape, dtype):
        return self.es.enter_context(self.nc.sbuf_tensor(name, list(shape), dtype))

    def init_psum(self):
        self.ps_banks = [self.es.enter_context(self.nc.psum_tensor(f"psb{i}", [128, 512], F32)) for i in range(8)]
        self.ps_deps = [Dep() for _ in range(8)]

    def next_ps(self):
        i = self.ps_i % 8
        self.ps_i += 1
        return self.ps_banks[i], self.ps_deps[i]

    def ps_rot(self, key, banks):
        if not hasattr(self, "_rot"):
            self._rot = {}
        k = self._rot.get(key, 0)
        self._rot[key] = k + 1
        i = banks[k % len(banks)]
        return self.ps_banks[i], self.ps_deps[i]

    def _waits(self, eng, deps, force_same=False):
        hard, soft = deps
        out = []
        for sk in set(hard) | set(soft):
            hv, sv = hard.get(sk, 0), soft.get(sk, 0)
            if sk == ("e", eng) and not force_same:
                if eng == "pe":
                    continue
                val = hv
            else:
                val = max(hv, sv)
            if val <= 0 or self.waited[eng].get(sk, 0) >= val:
                continue
            self.waited[eng][sk] = val
            out.append((self.semobj[sk], val))
        return out

    def _collect(self, r, w):
        hard, soft = {}, {}
        for b in r:
            if b.w is not None and hard.get(b.w[0], 0) < b.w[1]:
                hard[b.w[0]] = b.w[1]
        for b in w:
            if b.w is not None and hard.get(b.w[0], 0) < b.w[1]:
                hard[b.w[0]] = b.w[1]
            for sk, val in b.r.items():
                if soft.get(sk, 0) < val:
                    soft[sk] = val
        return hard, soft

    def _commit(self, ev, r, w):
        sk, val = ev
        for b in r:
            if b.r.get(sk, 0) < val:
                b.r[sk] = val
        for b in w:
            b.w = ev
            b.r = {}

    def op(self, eng, fn, r=(), w=()):
        waits = self._waits(eng, self._collect(r, w))
        self.cnt[eng] += 1
        ev = (("e", eng), self.cnt[eng])
        sem = self.sems[eng]
        self.n_wait += len(waits)
        self.n_inst += 1

        def emit(E, waits=waits, fn=fn, sem=sem):
            for s, v in waits:
                E.wait_ge(s, v)
            fn(E).then_inc(sem, 1)
        self.prog[eng].append(emit)
        self._commit(ev, r, w)
        return ev

    def dma(self, q, out, in_, r=(), w=(), **kw):
        k = self.dq_count[q]
        self.dq_count[q] += 1
        i = k % N_DMA_SEMS
        sk = ("d", q, i)
        deps = self._collect(r, w)
        prev = self.dsem_uses[q][i]
        if prev > 0:
            deps[0][sk] = max(deps[0].get(sk, 0), 16 * prev)
        waits = self._waits(q, deps, force_same=True)
        self.dsem_uses[q][i] += 1
        ev = (sk, 16 * self.dsem_uses[q][i])
        sem = self.dsem[q][i]
        self.n_wait += len(waits)
        self.n_inst += 1

        def emit(E, waits=waits, sem=sem, out=out, in_=in_, kw=kw):
            for s, v in waits:
                E.wait_ge(s, v)
            E.dma_start(out=out, in_=in_, **kw).then_inc(sem, 16)
        self.prog[q].append(emit)
        self._commit(ev, r, w)
        return ev

    def barrier(self):
        evs = []
        for e in ENGS:
            if self.cnt[e] > 0:
                evs.append((("e", e), self.cnt[e]))
        for q in self.dsem:
            for i in range(N_DMA_SEMS):
                if self.dsem_uses[q][i] > 0:
                    evs.append((("d", q, i), 16 * self.dsem_uses[q][i]))
        for e in ENGS:
            waits = []
            for sk, val in evs:
                if sk == ("e", e):
                    continue
                if self.waited[e].get(sk, 0) >= val:
                    continue
                self.waited[e][sk] = val
                waits.append((self.semobj[sk], val))
            if waits:
                def emit(E, waits=waits):
                    for s, v in waits:
                        E.wait_ge(s, v)
                self.prog[e].append(emit)

    def finish(self):
        self.barrier()
        prog = self.prog
        with self.nc.Block() as block:
            @block.tensor
            def _(E):
                for f in prog["pe"]:
                    f(E)

            @block.scalar
            def _(E):
                for f in prog["act"]:
                    f(E)

            @block.vector
            def _(E):
                for f in prog["dve"]:
                    f(E)

            @block.gpsimd
            def _(E):
                for f in prog["pool"]:
                    f(E)

            @block.sync
            def _(E):
                for f in prog["sp"]:
                    f(E)
        self.es.close()

    def mm(self, out, lhsT, rhs, start, stop, r=(), w=(), sgc=False):
        if sgc:
            return self.op("pe", lambda E: E.matmul(out, lhsT, rhs, start=start, stop=stop, skip_group_check=True), r, w)
        return self.op("pe", lambda E: E.matmul(out, lhsT, rhs, start=start, stop=stop), r, w)

    def tr(self, out, in_, ident, r=(), w=()):
        return self.op("pe", lambda E: E.transpose(out, in_, ident), r, w)

    def ts(self, eng, out, in0, s1, s2, op0, op1=None, r=(), w=(), accum_out=None):
        if op1 is None:
            if accum_out is None:
                return self.op(eng, lambda E: E.tensor_scalar(out, in0, s1, None, op0), r, w)
        if accum_out is not None:
            return self.op(eng, lambda E: E.tensor_scalar(out, in0, s1, s2, op0, op1, accum_out=accum_out), r, w)
        return self.op(eng, lambda E: E.tensor_scalar(out, in0, s1, s2, op0, op1), r, w)

    def tt(self, eng, out, in0, in1, op, r=(), w=()):
        return self.op(eng, lambda E: E.tensor_tensor(out, in0, in1, op), r, w)

    def stt(self, eng, out, in0, scalar, in1, op0, op1, r=(), w=()):
        return self.op(eng, lambda E: E.scalar_tensor_tensor(out, in0, scalar, in1, op0, op1), r, w)

    def act(self, out, in_, func, bias=None, scale=1.0, r=(), w=(), accum_out=None):
        kw = {}
        if bias is not None:
            kw["bias"] = bias
        if accum_out is not None:
            kw["accum_out"] = accum_out
        return self.op("act", lambda E: E.activation(out, in_, func, scale=scale, **kw), r, w)

    def copy(self, eng, out, in_, r=(), w=()):
        if eng == "act":
            return self.op("act", lambda E: E.copy(out, in_), r, w)
        return self.op(eng, lambda E: E.tensor_copy(out, in_), r, w)

    def memset(self, eng, ap, val, r=(), w=()):
        return self.op(eng, lambda E: E.memset(ap, val), r, w)


class Arena:
    def __init__(self, mk, words):
        self.mk = mk
        self.t = mk.sbuf("arena", [128, words], F32)
        self.cap = words
        self.off = 0

    def mark(self):
        return self.off

    def release(self, m):
        self.mk.barrier()
        self.off = m

    def alloc(self, shape, dtype=F32, parts=128):
        n = 1
        for s in shape:
            n *= s
        size = 4 if dtype == F32 else 2
        words = (n * size + 3) // 4
        words = (words + 7) // 8 * 8
        if self.off + words > self.cap:
            raise RuntimeError(f"arena overflow: need {words} at {self.off} cap {self.cap}")
        ap = self.t[0:parts, self.off:self.off + words]
        self.off += words
        self.peak = max(getattr(self, "peak", 0), self.off)
        if dtype != F32:
            ap = ap.bitcast(dtype)
        ap = ap[:, 0:n]
        if len(shape) == 2:
            ap = ap.rearrange("p (a b) -> p a b", a=shape[0])
        elif len(shape) == 3:
            ap = ap.rearrange("p (a b c) -> p a b c", a=shape[0], b=shape[1])
        elif len(shape) == 4:
            ap = ap.rearrange("p (a b c d) -> p a b c d", a=shape[0], b=shape[1], c=shape[2])
        return ap


W_SPECS = [
    ("mod_w", [4, 1024, 6144]), ("mod_b", [4, 6144]), ("ln1_g", [4, 1024]), ("ln1_b", [4, 1024]),
    ("ln2_g", [4, 1024]), ("ln2_b", [4, 1024]), ("router_w", [4, 1024, 32]), ("router_b", [4, 32]),
    ("exp_w1", [4, 32, 1024, 2048]), ("exp_b1", [4, 32, 2048]), ("exp_w2", [4, 32, 1024, 1024]),
    ("exp_b2", [4, 32, 1024]), ("conv_in_w", [1, 1024, 3072]), ("conv_w", [1, 3, 1024]),
    ("conv_out_w", [1, 1024, 1024]), ("swa_qkv_w", [1, 1024, 1536]), ("swa_qkv_b", [1, 1536]),
    ("swa_sink", [1, 16]), ("swa_out_w", [1, 1024, 1024]), ("swa_out_b", [1, 1024]),
    ("diff_qkv_w", [1, 1024, 3072]), ("diff_lambda", [1, 4, 64]), ("diff_subln_g", [1, 128]),
    ("diff_out_w", [1, 1024, 1024]), ("delta_qkvz_w", [1, 1024, 6144]), ("delta_ba_w", [1, 1024, 64]),
    ("delta_a_log", [1, 2, 16]), ("delta_dt_bias", [1, 2, 16]), ("delta_conv_w", [1, 5, 4096]),
    ("delta_norm_g", [1, 128]), ("delta_out_w", [1, 2048, 1024]),
]


def host_constants():
    c = {}
    c["ident"] = np.eye(128, dtype=np.float32)
    rows = SEQ // 64
    row = np.repeat(np.arange(rows, dtype=np.float32), 64)
    col = np.tile(np.arange(64, dtype=np.float32), rows)
    half = 32
    inv = (10000.0 ** (-np.arange(0, half, 2, dtype=np.float32) / half)).astype(np.float32)
    ar = row[:, None] * inv
    ac = col[:, None] * inv
    ang = np.concatenate([ar, ar, ac, ac], axis=-1).astype(np.float32)
    cos = np.cos(ang).astype(np.float32).T
    sin = np.sin(ang).astype(np.float32).T
    sign = np.ones((64, 1), np.float32)
    sign[0:16] = -1.0
    sign[32:48] = -1.0
    c["cosT"] = np.ascontiguousarray(np.concatenate([cos, cos], axis=0))
    c["sinT"] = np.ascontiguousarray(np.concatenate([sin, sin], axis=0))
    Rm = np.zeros((128, 128), np.float32)
    for hh in range(2):
        for p in range(64):
            blk = p // 16
            if blk % 2 == 0:
                Rm[hh * 64 + p + 16, hh * 64 + p] = -1.0
            else:
                Rm[hh * 64 + p - 16, hh * 64 + p] = 1.0
    c["rotm"] = Rm
    kj = np.arange(128)[:, None]
    qi = np.arange(128)[None, :]
    c["mprev"] = np.tile((kj >= qi).astype(np.float32), (1, 4))
    c["mnext"] = np.tile((kj <= qi).astype(np.float32), (1, 4))
    k_ = np.arange(128)[:, None]
    m_ = np.arange(128)[None, :]
    c["triF"] = (k_ <= m_).astype(np.float32)
    c["triB"] = (k_ >= m_).astype(np.float32)
    c["m2F"] = (k_ > m_).astype(np.float32)
    c["m2B"] = (k_ < m_).astype(np.float32)
    c["selF"] = np.repeat((np.arange(128) == 127).astype(np.float32)[:, None], 128, axis=1)
    c["selB"] = np.repeat((np.arange(128) == 0).astype(np.float32)[:, None], 128, axis=1)
    return c


class Prog:
    def __init__(self, nc, layers, test_mode=False, do_moe=True):
        self.nc = nc
        self.do_moe = do_moe
        self.dbg_names = []
        self.layers = list(layers)
        self.test_mode = test_mode
        self.mk = MK(nc)
        mk = self.mk
        mk.init_psum()
        self.A = Arena(mk, 51 * 1024)
        dt = nc.dram_tensor
        self.x_d = dt("x", [SEQ, D], F32, kind="ExternalInput").ap()
        self.ctx_d = dt("ctx", [CTX, D], F32, kind="ExternalInput").ap()
        self.c_d = dt("c", [D], F32, kind="ExternalInput").ap()
        self.cctx_d = dt("c_ctx", [D], F32, kind="ExternalInput").ap()
        wshapes = dict(W_SPECS)

        class _LazyW(dict):
            def __missing__(d, name):
                ap = dt(name, wshapes[name], F32, kind="ExternalInput").ap()
                d[name] = ap
                return ap
        self.W = _LazyW()
        if not test_mode:
            for name, shape in W_SPECS:
                self.W[name]
        self.C = {}
        for name, arr in host_constants().items():
            self.C[name] = dt(name, list(arr.shape), F32, kind="ExternalInput").ap()
        out_rows = T if test_mode else SEQ
        self.out_d = dt("out", [out_rows, D], F32, kind="ExternalOutput").ap()
        self.xres = dt("xres", [T, D], F32).ap()
        self.xdep = [Dep() for _ in range(NT)]
        self.outdep = [Dep() for _ in range(NT)]
        self.src = [self.x_d[t * 128:(t + 1) * 128, :] for t in range(NTL)] + \
                   [self.ctx_d[t * 128:(t + 1) * 128, :] for t in range(NT - NTL)]
        self.setup_persistent()

    def setup_persistent(self):
        mk, A = self.mk, self.A
        self.ident = A.alloc((128,), F32)
        self.d_ident = Dep()
        mk.dma("sp", self.ident, self.C["ident"][:, :], w=[self.d_ident])
        self.identb = A.alloc((128,), BF16)
        self.d_identb = Dep()
        mk.copy("dve", self.identb, self.ident, r=[self.d_ident], w=[self.d_identb])
        self.ones = A.alloc((128,), F32)
        self.d_ones = Dep()
        mk.memset("dve", self.ones, 1.0, w=[self.d_ones])
        self.modF = A.alloc((2, 6, 8), F32)
        self.d_modF = Dep()
        self.modbc = A.alloc((2, 2, 1024), F32)
        self.d_modbc = Dep()
        self.lnbc = A.alloc((4, 1024), F32)
        self.d_lnbc = Dep()
        self.xt = [A.alloc((1024,), F32) for _ in range(2)]
        self.d_xt = [Dep() for _ in range(2)]
        self.zt = [A.alloc((1024,), F32) for _ in range(2)]
        self.d_zt = [Dep() for _ in range(2)]
        self.st = [A.alloc((2, 6), F32) for _ in range(2)]
        self.mv = [A.alloc((4,), F32) for _ in range(2)]
        self.xt_i = 0
        self.zt_i = 0
        self.fm_tmp = A.alloc((128,), F32, parts=64)
        self.d_fm_tmp = Dep()

    def load_featmajor(self, dst, d_dst, vec_ap, n):
        mk = self.mk
        tmp, d_tmp = self.fm_tmp[0:n, :], self.d_fm_tmp
        mk.dma("sp", tmp, vec_ap.rearrange("(j p) -> j p", p=128), w=[d_tmp])
        ps, dps = mk.next_ps()
        mk.tr(ps[:, 0:n], tmp, self.ident[0:n, 0:n], r=[d_tmp, self.d_ident], w=[dps])
        mk.copy("dve", dst, ps[:, 0:n], r=[dps], w=[d_dst])

    def phase_mod(self, i):
        mk, A, W = self.mk, self.A, self.W
        m0 = A.mark()
        cT = A.alloc((8, 2), F32)
        d_cT = Dep()
        ctmp = A.alloc((8,), F32)
        d_ctmp = Dep()
        self.load_featmajor(ctmp, d_ctmp, self.c_d, 8)
        mk.act(cT[:, :, 0], ctmp, AF.Silu, r=[d_ctmp], w=[d_cT])
        self.load_featmajor(ctmp, d_ctmp, self.cctx_d, 8)
        mk.act(cT[:, :, 1], ctmp, AF.Silu, r=[d_ctmp], w=[d_cT])
        sbc = A.alloc((2, 8, 128), F32)
        d_sbc = Dep()
        for lc in range(2):
            for k in range(8):
                mk.ts("dve", sbc[:, lc, k, :], self.ones, cT[:, k, lc:lc + 1], None, ALU.mult,
                      r=[self.d_ones, d_cT], w=[d_sbc])
        mbF = A.alloc((48,), F32)
        d_mbF = Dep()
        self.load_featmajor(mbF, d_mbF, W["mod_b"][i], 48)
        mbbc = A.alloc((1024,), F32)
        d_mbbc = Dep()
        wsl = [A.alloc((8, 1024), F32) for _ in range(2)]
        d_wsl = [Dep() for _ in range(2)]
        mw = W["mod_w"][i].rearrange("(k p) n -> p k n", p=128)
        for m in range(6):
            ws, dws = wsl[m % 2], d_wsl[m % 2]
            mk.dma("sp", ws, mw[:, :, m * 1024:(m + 1) * 1024], w=[dws])
            for kc in range(8):
                ps, dps = mk.next_ps()
                for kk in range(8):
                    mk.mm(ps[:, 0:2], ws[:, kk, kc * 128:(kc + 1) * 128], cT[:, kk, 0:2], kk == 0, kk == 7,
                          r=[dws, d_cT], w=[dps])
                mk.ts("dve", self.modF[:, :, m, kc], ps[:, 0:2], mbF[:, m * 8 + kc:m * 8 + kc + 1], None, ALU.add,
                      r=[dps, d_mbF], w=[self.d_modF])
            if m in (2, 5):
                j = 0 if m == 2 else 1
                mk.dma("sp", mbbc, W["mod_b"][i][m * 1024:(m + 1) * 1024].partition_broadcast(128), w=[d_mbbc])
                for lc in range(2):
                    for hf in range(2):
                        ps, dps = mk.next_ps()
                        for kk in range(8):
                            mk.mm(ps[:, :], sbc[:, lc, kk, :], ws[:, kk, hf * 512:(hf + 1) * 512], kk == 0, kk == 7,
                                  r=[dws, d_sbc], w=[dps])
                        mk.tt("dve", self.modbc[:, lc, j, hf * 512:(hf + 1) * 512], ps[:, :],
                              mbbc[:, hf * 512:(hf + 1) * 512], ALU.add, r=[dps, d_mbbc], w=[self.d_modbc])
        for m in (1, 4):
            mk.ts("dve", self.modF[:, :, m, :], self.modF[:, :, m, :], 1.0, None, ALU.add, r=[], w=[self.d_modF])
        for q, nm in enumerate(("ln1_g", "ln1_b", "ln2_g", "ln2_b")):
            mk.dma("sp", self.lnbc[:, q, :], W[nm][i].partition_broadcast(128), w=[self.d_lnbc])
        A.release(m0)

    def prep_tile(self, tt, mi, hT, d_hT, col0, h32=None, d_h32=None):
        mk = self.mk
        lc = 0 if tt < NTL else 1
        b = self.xt_i % 2
        self.xt_i += 1
        xt, dxt = self.xt[b], self.d_xt[b]
        mk.dma("sp", xt, self.src[tt], r=[self.xdep[tt]], w=[dxt])
        for hf in range(2):
            ps, dps = mk.next_ps()
            for q in range(4):
                k = hf * 4 + q
                mk.tr(ps[:, q * 128:(q + 1) * 128], xt[:, k * 128:(k + 1) * 128], self.ident,
                      r=[dxt, self.d_ident], w=[dps])
            for q in range(4):
                k = hf * 4 + q
                o = hT[:, k, col0:col0 + 128]
                sc = self.modF[:, lc, mi + 1, k:k + 1]
                sh = self.modF[:, lc, mi, k:k + 1]
                if h32 is not None:
                    o32 = h32[:, k, :]
                    mk.ts("dve", o32, ps[:, q * 128:(q + 1) * 128], sc, sh, ALU.mult, ALU.add,
                          r=[dps, self.d_modF], w=[d_h32])
                    mk.copy("act", o, o32, r=[d_h32], w=[d_hT])
                elif q % 2 == 0:
                    mk.ts("dve", o, ps[:, q * 128:(q + 1) * 128], sc, sh, ALU.mult, ALU.add,
                          r=[dps, self.d_modF], w=[d_hT])
                else:
                    mk.act(o, ps[:, q * 128:(q + 1) * 128], AF.Identity, bias=sh, scale=sc,
                           r=[dps, self.d_modF], w=[d_hT])

    def post_tile(self, tt, y, d_y, j, dst, d_dst, ybias=None, d_ybias=None):
        mk = self.mk
        lc = 0 if tt < NTL else 1
        b = self.xt_i % 2
        self.xt_i += 1
        xt, dxt = self.xt[b], self.d_xt[b]
        mk.dma("sp", xt, self.src[tt], r=[self.xdep[tt]], w=[dxt])
        zb = self.zt_i % 2
        self.zt_i += 1
        z, dz = self.zt[zb], self.d_zt[zb]
        st, mv = self.st[zb], self.mv[zb]
        for hf in range(2):
            sl = slice(hf * 512, (hf + 1) * 512)
            if ybias is not None:
                mk.tt("dve", z[:, sl], y[hf], ybias[:, sl], ALU.add, r=list(d_y) + [d_ybias], w=[dz])
                mk.tt("dve", z[:, sl], z[:, sl], self.modbc[:, lc, j, sl], ALU.mult, r=[self.d_modbc], w=[dz])
            else:
                mk.tt("dve", z[:, sl], y[hf], self.modbc[:, lc, j, sl], ALU.mult, r=list(d_y) + [self.d_modbc], w=[dz])
        mk.stt("dve", z, xt, ALPHA, z, ALU.mult, ALU.add, r=[dxt], w=[dz])
        mk.op("dve", lambda E: E.bn_stats(st[:, 0, :], z[:, 0:512]), r=[], w=[dz])
        mk.op("dve", lambda E: E.bn_stats(st[:, 1, :], z[:, 512:1024]), r=[], w=[dz])
        mk.op("dve", lambda E: E.bn_aggr(mv[:, 0:2], st), r=[], w=[dz])
        mk.ts("dve", mv[:, 2:3], mv[:, 1:2], LN_EPS, None, ALU.add, r=[], w=[dz])
        mk.act(mv[:, 2:3], mv[:, 2:3], AF.Sqrt, r=[dz], w=[dz])
        mk.op("dve", lambda E: E.reciprocal(mv[:, 2:3], mv[:, 2:3]), r=[dz], w=[dz])
        mk.ts("dve", z, z, mv[:, 0:1], mv[:, 2:3], ALU.subtract, ALU.mult, r=[], w=[dz])
        mk.tt("pool", z, z, self.lnbc[:, 2 * j, :], ALU.mult, r=[self.d_lnbc], w=[dz])
        mk.tt("pool", z, z, self.lnbc[:, 2 * j + 1, :], ALU.add, r=[self.d_lnbc], w=[dz])
        mk.dma("sp", dst, z, r=[dz], w=[d_dst])

    def dbg(self, name, ap, deps, dtype=F32):
        if not self.test_mode:
            return
        shape = list(ap.shape)
        t = self.nc.dram_tensor("dbg_" + name, shape, dtype, kind="ExternalOutput").ap()
        self.mk.dma("sp", t, ap, r=list(deps), w=[Dep()])
        self.dbg_names.append("dbg_" + name)

    def set_src_xres(self, tiles):
        for tt in tiles:
            self.src[tt] = self.xres[tt * 128:(tt + 1) * 128, :]

    def load_w_bf16(self, dst, d_dst, src_ap):
        self.mk.dma("pool", dst, src_ap, w=[d_dst])

    def mixer_conv(self, i, ctx_out):
        mk, A, W = self.mk, self.A, self.W
        ntiles = NT
        m0 = A.mark()
        hT = A.alloc((8, T), BF16)
        d_hT = [Dep() for _ in range(NT)]
        for tt in range(NT):
            self.prep_tile(tt, 0, hT, d_hT[tt], tt * 128)
        self.dbg("hT", hT, d_hT, BF16)
        self.dbg("modF", self.modF, [self.d_modF])
        self.dbg("modbc", self.modbc, [self.d_modbc])
        gT_d = self.nc.dram_tensor("conv_gT", [D, T], BF16).ap()
        d_gTd = [Dep() for _ in range(8)]
        wc = A.alloc((8, 3), F32)
        d_wc = Dep()
        wtmp = A.alloc((8,), F32)
        d_wtmp = Dep()
        for k in range(3):
            self.load_featmajor(wtmp, d_wtmp, W["conv_w"][0, k], 8)
            mk.copy("dve", wc[:, :, k], wtmp, r=[d_wtmp], w=[d_wc])
        wj = [A.alloc((3, 8, 128), BF16) for _ in range(2)]
        d_wj = [Dep() for _ in range(2)]
        LP = SEQ + 2 + CTX + 2
        cu = A.alloc((LP,), F32)
        d_cu = Dep()
        bsb = A.alloc((T,), F32)
        d_bsb = Dep()
        t1 = A.alloc((SEQ,), F32)
        d_t1 = Dep()
        gj = [A.alloc((T,), BF16)]
        d_gj = [Dep()]
        csb = [A.alloc((512,), F32) for _ in range(2)]
        d_csb = [Dep() for _ in range(2)]
        mk.memset("dve", cu, 0.0, w=[d_cu])
        win = W["conv_in_w"][0].rearrange("(k p) n -> p k n", p=128)
        tts = [(q * 512, 512, 1 + q * 512) for q in range(8)] + [(SEQ, CTX, SEQ + 3)]
        ci = 0
        for j in range(8):
            wjb, dwj = wj[j % 2], d_wj[j % 2]
            for s in range(3):
                self.load_w_bf16(wjb[:, s], dwj, win[:, :, s * 1024 + j * 128:s * 1024 + (j + 1) * 128])
            for (t0, n, co) in tts:
                pss = []
                for s in range(3):
                    ps, dps = mk.next_ps()
                    for kk in range(8):
                        mk.mm(ps[:, 0:n], wjb[:, s, kk, :], hT[:, kk, t0:t0 + n], kk == 0, kk == 7,
                              r=[dwj] + d_hT[t0 // 128:(t0 + n) // 128], w=[dps])
                    pss.append((ps, dps))
                cb, dcb = csb[ci % 2], d_csb[ci % 2]
                ci += 1
                mk.copy("act", bsb[:, t0:t0 + n], pss[0][0][:, 0:n], r=[pss[0][1]], w=[d_bsb])
                mk.copy("act", cb[:, 0:n], pss[1][0][:, 0:n], r=[pss[1][1]], w=[dcb])
                mk.tt("dve", cu[:, co:co + n], cb[:, 0:n], pss[2][0][:, 0:n], ALU.mult, r=[dcb, pss[2][1]], w=[d_cu])
            g, dg = gj[0], d_gj[0]
            for (s0, L, c0) in ((0, SEQ, 0), (SEQ, CTX, SEQ + 2)):
                tv = t1[:, 0:L]
                mk.ts("dve", tv, cu[:, c0 + 1:c0 + 1 + L], wc[:, j, 1:2], None, ALU.mult, r=[d_cu, d_wc], w=[d_t1])
                mk.stt("dve", tv, cu[:, c0:c0 + L], wc[:, j, 0:1], tv, ALU.mult, ALU.add, r=[d_cu], w=[d_t1])
                mk.stt("dve", tv, cu[:, c0 + 2:c0 + 2 + L], wc[:, j, 2:3], tv, ALU.mult, ALU.add, r=[d_cu], w=[d_t1])
                mk.tt("dve", g[:, s0:s0 + L], tv, bsb[:, s0:s0 + L], ALU.mult, r=[d_bsb], w=[dg])
            mk.dma("sp", gT_d[j * 128:(j + 1) * 128, :], g, r=[dg], w=[d_gTd[j]])
        A.release(m0)
        m0 = A.mark()
        gT = A.alloc((8, T), BF16)
        d_gT = Dep()
        for j in range(8):
            mk.dma("sp", gT[:, j, :], gT_d[j * 128:(j + 1) * 128, :], r=[d_gTd[j]], w=[d_gT])
        wo = A.alloc((8, 1024), BF16)
        d_wo = Dep()
        self.load_w_bf16(wo, d_wo, W["conv_out_w"][0].rearrange("(k p) n -> p k n", p=128))
        self.dbg("gT", gT, [d_gT], BF16)
        for tt in range(NT):
            ys, dys = [], []
            for hf in range(2):
                ps, dps = mk.next_ps()
                for j in range(8):
                    mk.mm(ps[:, :], gT[:, j, tt * 128:(tt + 1) * 128], wo[:, j, hf * 512:(hf + 1) * 512], j == 0, j == 7,
                          r=[d_gT, d_wo], w=[dps])
                ys.append(ps[:, :])
                dys.append(dps)
            self.post_tile(tt, ys, dys, 0, self.xres[tt * 128:(tt + 1) * 128, :], self.xdep[tt])
        self.set_src_xres(range(NT))
        A.release(m0)


    def mixer_attn(self, i, kind, ctx_out):
        mk, A, W, C = self.mk, self.A, self.W, self.C
        nc = self.nc
        swa = (kind == 1)
        qtiles = list(range(NT)) if ctx_out else list(range(NTL))
        if swa:
            wqkv, bqkv = W["swa_qkv_w"][0], W["swa_qkv_b"][0]
            nkc, nvh, vd = 4, 4, 64
            wo_ap, kcol0, vcol0 = W["swa_out_w"][0], 1024, 1280
        else:
            wqkv, bqkv = W["diff_qkv_w"][0], None
            nkc, nvh, vd = 8, 8, 128
            wo_ap, kcol0, vcol0 = W["diff_out_w"][0], 1024, 2048
        va = vd + 1
        qT_d = nc.dram_tensor(f"att{i}_qT", [1024, T], BF16).ap()
        kT_d = nc.dram_tensor(f"att{i}_kT", [nkc * 128, T], BF16).ap()
        v_d = nc.dram_tensor(f"att{i}_v", [T, nvh * va], BF16).ap()
        if self.test_mode:
            o_d = nc.dram_tensor(f"dbg_att{i}_o", [T, 1024], BF16, kind="ExternalOutput").ap()
            self.dbg_names.append(f"dbg_att{i}_o")
            qT_dbg = nc.dram_tensor(f"dbg_att{i}_qT", [1024, T], BF16, kind="ExternalOutput").ap()
            self.dbg_names.append(f"dbg_att{i}_qT")
        else:
            o_d = nc.dram_tensor(f"att{i}_o", [T, 1024], BF16).ap()
        d_qd = [Dep() for _ in range(8)]
        d_kd = [Dep() for _ in range(nkc)]
        d_vd = [Dep() for _ in range(NT)]
        d_od = [Dep() for _ in range(NT)]
        wv = wqkv.rearrange("(k p) n -> p k n", p=128)
        m0 = A.mark()
        hT = A.alloc((8, T), BF16)
        d_hT = [Dep() for _ in range(NT)]
        for tt in range(NT):
            self.prep_tile(tt, 0, hT, d_hT[tt], tt * 128)
        cosT = A.alloc((SEQ,), F32)
        sinT = A.alloc((SEQ,), F32)
        rotm = A.alloc((128,), F32)
        d_tab = Dep()
        mk.dma("sp", cosT, C["cosT"][:, :], w=[d_tab])
        mk.dma("sp", sinT, C["sinT"][:, :], w=[d_tab])
        mk.dma("sp", rotm, C["rotm"][:, :], w=[d_tab])
        bF = A.alloc((16,), F32)
        d_bF = Dep()
        if swa:
            self.load_featmajor(bF[:, 0:12], d_bF, bqkv, 12)
            bkd = A.alloc((4,), F32)
            d_bkd = Dep()
            for g in range(4):
                for hh in range(2):
                    mk.dma("sp", bkd[hh * 64:(hh + 1) * 64, g:g + 1],
                           bqkv[1024 + g * 64:1024 + (g + 1) * 64].rearrange("(p o) -> p o", o=1), w=[d_bkd])
        else:
            mk.memset("dve", bF, 0.0, w=[d_bF])
        wc = [A.alloc((8, 128), BF16) for _ in range(2)]
        d_wc = [Dep() for _ in range(2)]
        qsb = [A.alloc((512,), F32) for _ in range(2)]
        d_qsb = [Dep() for _ in range(2)]
        t1 = [A.alloc((512,), F32) for _ in range(2)]
        d_t1 = [Dep() for _ in range(2)]
        och = [A.alloc((T,), BF16) for _ in range(2)]
        d_och = [Dep() for _ in range(2)]
        tts = [(q * 512, 512) for q in range(8)] + [(SEQ, CTX)]
        chunks = []
        for c in range(8):
            chunks.append(("q", c, qT_d[c * 128:(c + 1) * 128, :], d_qd[c], [(c * 128, 128, 0)], bF[:, c:c + 1]))
        if swa:
            for g in range(4):
                chunks.append(("k", g, kT_d[g * 128:(g + 1) * 128, :], d_kd[g],
                               [(kcol0 + g * 64, 64, 0), (kcol0 + g * 64, 64, 64)], bkd[:, g:g + 1]))
        else:
            for c in range(8):
                chunks.append(("k", c, kT_d[c * 128:(c + 1) * 128, :], d_kd[c], [(kcol0 + c * 128, 128, 0)], bF[:, 8:9]))
        ri = 0
        for ci, (nm, c, dst, d_dst, wcols, bias) in enumerate(chunks):
            wcb, dwc = wc[ci % 2], d_wc[ci % 2]
            for (c0, ncol, o0) in wcols:
                self.load_w_bf16(wcb[:, :, o0:o0 + ncol], dwc, wv[:, :, c0:c0 + ncol])
            ob, dob = och[ci % 2], d_och[ci % 2]
            for (t0, n) in tts:
                ps, dps = mk.next_ps()
                for kk in range(8):
                    mk.mm(ps[:, 0:n], wcb[:, kk, :], hT[:, kk, t0:t0 + n], kk == 0, kk == 7,
                          r=[dwc] + d_hT[t0 // 128:(t0 + n) // 128], w=[dps])
                if t0 >= SEQ:
                    mk.act(ob[:, t0:t0 + n], ps[:, 0:n], AF.Identity, bias=bias, scale=1.0,
                           r=[dps, d_bF] + ([d_bkd] if swa else []), w=[dob])
                    continue
                rb = ri % 2
                ri += 1
                q_, dq_, t_, dt_ = qsb[rb], d_qsb[rb], t1[rb], d_t1[rb]
                mk.act(q_[:, 0:n], ps[:, 0:n], AF.Identity, bias=bias, scale=1.0,
                       r=[dps, d_bF] + ([d_bkd] if swa else []), w=[dq_])
                ps2, dps2 = mk.next_ps()
                mk.mm(ps2[:, 0:n], rotm, q_[:, 0:n], True, True, r=[d_tab, dq_], w=[dps2])
                mk.tt("dve", t_[:, 0:n], ps2[:, 0:n], sinT[:, t0:t0 + n], ALU.mult, r=[dps2, d_tab], w=[dt_])
                mk.tt("pool", q_[:, 0:n], q_[:, 0:n], cosT[:, t0:t0 + n], ALU.mult, r=[d_tab], w=[dq_])
                mk.tt("dve", ob[:, t0:t0 + n], q_[:, 0:n], t_[:, 0:n], ALU.add, r=[dq_, dt_], w=[dob])
            mk.dma("sp", dst, ob, r=[dob], w=[d_dst])
            if self.test_mode and nm == "q":
                mk.dma("sp", qT_dbg[c * 128:(c + 1) * 128, :], ob, r=[dob], w=[Dep()])
        nvc = nvh * vd
        wvb = A.alloc((8, nvc), BF16)
        d_wvb = Dep()
        self.load_w_bf16(wvb, d_wvb, wv[:, :, vcol0:vcol0 + nvc])
        vbias = None
        if swa:
            vbias = A.alloc((nvc,), F32)
            d_vbias = Dep()
            mk.dma("sp", vbias, bqkv[vcol0:vcol0 + nvc].partition_broadcast(128), w=[d_vbias])
        vt = [A.alloc((nvh, va), BF16) for _ in range(2)]
        d_vt = [Dep() for _ in range(2)]
        for b in range(2):
            mk.memset("dve", vt[b], 1.0, w=[d_vt[b]])
        for tt in range(NT):
            vb, dvb = vt[tt % 2], d_vt[tt % 2]
            for c0 in range(0, nvc, 512):
                ncol = min(512, nvc - c0)
                ps, dps = mk.next_ps()
                for kk in range(8):
                    mk.mm(ps[:, 0:ncol], hT[:, kk, tt * 128:(tt + 1) * 128], wvb[:, kk, c0:c0 + ncol], kk == 0, kk == 7,
                          r=[d_wvb, d_hT[tt]], w=[dps])
                h0, nh = c0 // vd, ncol // vd
                src = ps[:, 0:ncol].rearrange("p (h d) -> p h d", d=vd)
                if swa:
                    mk.tt("dve", vb[:, h0:h0 + nh, 0:vd], src,
                          vbias[:, c0:c0 + ncol].rearrange("p (h d) -> p h d", d=vd), ALU.add,
                          r=[dps, d_vbias], w=[dvb])
                else:
                    mk.copy("act", vb[:, h0:h0 + nh, 0:vd], src, r=[dps], w=[dvb])
            mk.dma("sp", v_d[tt * 128:(tt + 1) * 128, :].rearrange("p (h d) -> p h d", d=va), vb, r=[dvb], w=[d_vd[tt]])
        A.release(m0)
        import os as _os
        if _os.environ.get("KSTOP") == "p1":
            return
        m0 = A.mark()
        SCALE = 0.125
        S_BANKS, O_BANKS = [0, 1, 2, 3], [4, 5, 6, 7]
        qt = [A.alloc((8, 512), BF16) for _ in range(2)]
        d_qt = [Dep() for _ in range(2)]
        et = [A.alloc((512,), BF16) for _ in range(4)]
        d_et = [Dep() for _ in range(4)]
        ot = [A.alloc((1024,), BF16) for _ in range(2)]
        d_ot = [Dep() for _ in range(2)]
        sm = [A.alloc((8,), F32) for _ in range(4)]
        d_sm = [Dep() for _ in range(4)]
        ei = 0
        si = 0
        if swa:
            kT = A.alloc((4, T), BF16)
            d_kT = Dep()
            for g in range(4):
                mk.dma("sp", kT[:, g, :], kT_d[g * 128:(g + 1) * 128, :], r=[d_kd[g]], w=[d_kT])
            vv = A.alloc((NT, nvh, va), BF16)
            d_vv = Dep()
            for tt in range(NT):
                mk.dma("sp", vv[:, tt], v_d[tt * 128:(tt + 1) * 128, :].rearrange("p (h d) -> p h d", d=va),
                       r=[d_vd[tt]], w=[d_vv])
            mprev = A.alloc((512,), BF16)
            mnext = A.alloc((512,), BF16)
            d_msk = Dep()
            mk.dma("pool", mprev, C["mprev"][:, :], w=[d_msk])
            mk.dma("pool", mnext, C["mnext"][:, :], w=[d_msk])
            esink = A.alloc((16,), F32)
            d_esink = Dep()
            mk.dma("sp", esink, W["swa_sink"][0].partition_broadcast(128), w=[d_esink])
            mk.act(esink, esink, AF.Exp, r=[d_esink], w=[d_esink])
            for qi_, qb in enumerate(qtiles):
                b = qi_ % 2
                q_, dq_ = qt[b], d_qt[b]
                mk.dma("sp", q_[:, :, 0:128], qT_d.rearrange("(c p) t -> p c t", p=128)[:, :, qb * 128:(qb + 1) * 128],
                       r=d_qd, w=[dq_])
                o_, do_ = ot[b], d_ot[b]
                if qb < NTL:
                    keys = [(kt_, m_) for (kt_, m_) in ((qb - 1, "p"), (qb, None), (qb + 1, "n")) if 0 <= kt_ < NTL]
                    keys += [(NTL, None), (NTL + 1, None)]
                else:
                    keys = [(NTL, None), (NTL + 1, None)]
                for g in range(4):
                    pso, dpso = mk.ps_rot("o", O_BANKS)
                    for ki, (kt_, m_) in enumerate(keys):
                        e_, de_ = et[ei % 4], d_et[ei % 4]
                        ei += 1
                        for ph in range(2):
                            pss, dpss = mk.ps_rot("s", S_BANKS)
                            for c2 in range(2):
                                mk.mm(pss[:, c2 * 128:(c2 + 1) * 128],
                                      kT[ph * 64:(ph + 1) * 64, g, kt_ * 128:(kt_ + 1) * 128],
                                      q_[ph * 64:(ph + 1) * 64, 2 * g + c2, 0:128], True, True,
                                      r=[d_kT, dq_], w=[dpss])
                            mk.act(e_[:, ph * 256:(ph + 1) * 256], pss[:, 0:256], AF.Exp, scale=SCALE, r=[dpss], w=[de_])
                        if m_ is not None:
                            mk.tt("pool", e_, e_, mprev if m_ == "p" else mnext, ALU.mult, r=[d_msk], w=[de_])
                        for hh in range(4):
                            col = (hh % 2) * 256 + (hh // 2) * 128
                            mk.mm(pso[:, hh * va:(hh + 1) * va], e_[:, col:col + 128], vv[:, kt_, g, :],
                                  ki == 0 and hh == 0, ki == len(keys) - 1, r=[de_, d_vv], w=[dpso], sgc=True)
                    s_, ds_ = sm[si % 4], d_sm[si % 4]
                    si += 1
                    for hh in range(4):
                        hq = 4 * g + hh
                        mk.ts("dve", s_[:, hh:hh + 1], pso[:, hh * va + vd:hh * va + va], esink[:, hq:hq + 1], None, ALU.add,
                              r=[dpso, d_esink], w=[ds_])
                    mk.op("dve", lambda E, s_=s_: E.reciprocal(s_[:, 4:8], s_[:, 0:4]), r=[ds_], w=[ds_])
                    for hh in range(4):
                        hq = 4 * g + hh
                        mk.ts("dve", o_[:, hq * 64:(hq + 1) * 64], pso[:, hh * va:hh * va + vd], s_[:, 4 + hh:5 + hh], None,
                              ALU.mult, r=[dpso, ds_], w=[do_])
                mk.dma("sp", o_d[qb * 128:(qb + 1) * 128, :], o_, r=[do_], w=[d_od[qb]])
        else:
            lam_init = 0.8 - 0.6 * math.exp(-0.3 * i)
            lp = A.alloc((256,), F32)
            d_lp = Dep()
            mk.dma("sp", lp, W["diff_lambda"][0].rearrange("a b -> (a b)").partition_broadcast(128), w=[d_lp])
            lam = A.alloc((8,), F32)
            mk.tt("dve", lp[:, 0:64], lp[:, 0:64], lp[:, 64:128], ALU.mult, r=[d_lp], w=[d_lp])
            mk.tt("dve", lp[:, 128:192], lp[:, 128:192], lp[:, 192:256], ALU.mult, r=[d_lp], w=[d_lp])
            mk.op("dve", lambda E: E.reduce_sum(lam[:, 0:1], lp[:, 0:64], mybir.AxisListType.X), r=[d_lp], w=[d_lp])
            mk.op("dve", lambda E: E.reduce_sum(lam[:, 1:2], lp[:, 128:192], mybir.AxisListType.X), r=[d_lp], w=[d_lp])
            mk.act(lam[:, 0:2], lam[:, 0:2], AF.Exp, r=[d_lp], w=[d_lp])
            mk.tt("dve", lam[:, 2:3], lam[:, 1:2], lam[:, 0:1], ALU.subtract, r=[d_lp], w=[d_lp])
            mk.ts("dve", lam[:, 3:4], lam[:, 2:3], -lam_init, None, ALU.add, r=[d_lp], w=[d_lp])
            gsub = A.alloc((128,), F32)
            d_gsub = Dep()
            mk.dma("sp", gsub, W["diff_subln_g"][0].partition_broadcast(128), w=[d_gsub])
            mk.ts("dve", gsub, gsub, 1.0 - lam_init, None, ALU.mult, r=[d_gsub], w=[d_gsub])
            kT = A.alloc((4, T), BF16)
            d_kT = Dep()
            vv = A.alloc((NT, 4, va), BF16)
            d_vv = Dep()
            o0 = [A.alloc((4, 128), F32) for _ in range(2)]
            d_o0 = [Dep() for _ in range(2)]
            junk = A.alloc((128,), F32)
            otd = [A.alloc((4, 512), BF16) for _ in range(2)]
            qtl = [(q * 4, 4) for q in range(8)] + ([(NTL, 2)] if ctx_out else [])
            oi = 0
            for hg in range(2):
                for c in range(4):
                    mk.dma("sp", kT[:, c, :], kT_d[(hg * 4 + c) * 128:(hg * 4 + c + 1) * 128, :], r=[d_kd[hg * 4 + c]], w=[d_kT])
                for tt in range(NT):
                    mk.dma("sp", vv[:, tt], v_d[tt * 128:(tt + 1) * 128, hg * 4 * va:(hg + 1) * 4 * va].rearrange(
                        "p (h d) -> p h d", d=va), r=[d_vd[tt]], w=[d_vv])
                for qi_, (qa, nq4) in enumerate(qtl):
                    nq = nq4 * 128
                    b = qi_ % 2
                    q_, dq_ = qt[b], d_qt[b]
                    mk.dma("sp", q_[:, 0:4, 0:nq],
                           qT_d.rearrange("(c p) t -> p c t", p=128)[:, hg * 4:hg * 4 + 4, qa * 128:qa * 128 + nq],
                           r=d_qd, w=[dq_])
                    o_, do_ = otd[b], d_ot[b]
                    keys = list(range(NT)) if qa < NTL else [NTL, NTL + 1]
                    for pr in range(4):
                        ob_, dob_ = o0[oi % 2], d_o0[oi % 2]
                        oi += 1
                        for ii in range(2):
                            psoA, dpsoA = mk.ps_rot("o", O_BANKS)
                            psoB, dpsoB = mk.ps_rot("o", O_BANKS)
                            pend = []

                            def emit_pv(ki, kt_, e_, de_):
                                for s4 in range(nq4):
                                    pb, dpb = (psoA, dpsoA) if s4 < 2 else (psoB, dpsoB)
                                    co = (s4 % 2) * 256
                                    mk.mm(pb[:, co:co + va], e_[:, s4 * 128:(s4 + 1) * 128], vv[:, kt_, pr, :],
                                          ki == 0 and s4 % 2 == 0, ki == len(keys) - 1, r=[de_, d_vv], w=[dpb], sgc=True)

                            for ki, kt_ in enumerate(keys):
                                pss, dpss = mk.ps_rot("s", S_BANKS)
                                mk.mm(pss[:, 0:nq], kT[ii * 64:(ii + 1) * 64, pr, kt_ * 128:(kt_ + 1) * 128],
                                      q_[ii * 64:(ii + 1) * 64, pr, 0:nq], True, True, r=[d_kT, dq_], w=[dpss])
                                e_, de_ = et[ei % 4], d_et[ei % 4]
                                ei += 1
                                mk.act(e_[:, 0:nq], pss[:, 0:nq], AF.Exp, scale=SCALE, r=[dpss], w=[de_])
                                pend.append((ki, kt_, e_, de_))
                                if len(pend) > 2:
                                    emit_pv(*pend.pop(0))
                            while pend:
                                emit_pv(*pend.pop(0))
                            for s4 in range(nq4):
                                pb, dpb = (psoA, dpsoA) if s4 < 2 else (psoB, dpsoB)
                                co = (s4 % 2) * 256
                                s_, ds_ = sm[si % 4], d_sm[si % 4]
                                si += 1
                                mk.op("dve", lambda E, s_=s_, pb=pb, co=co: E.reciprocal(s_[:, 0:1], pb[:, co + vd:co + va]),
                                      r=[dpb], w=[ds_])
                                if ii == 0:
                                    mk.ts("dve", ob_[:, s4, :], pb[:, co:co + vd], s_[:, 0:1], None, ALU.mult,
                                          r=[dpb, ds_], w=[dob_])
                                else:
                                    mk.ts("dve", s_[:, 1:2], s_[:, 0:1], lam[:, 3:4], None, ALU.mult, r=[ds_, d_lp], w=[ds_])
                                    mk.stt("dve", ob_[:, s4, :], pb[:, co:co + vd], s_[:, 1:2], ob_[:, s4, :], ALU.mult, ALU.add,
                                           r=[dpb, ds_, dob_], w=[dob_])
                                    mk.act(junk, ob_[:, s4, :], AF.Square, r=[dob_], w=[ds_], accum_out=s_[:, 2:3])
                                    mk.ts("dve", s_[:, 3:4], s_[:, 2:3], 1.0 / 128.0, RMS_EPS, ALU.mult, ALU.add, r=[ds_], w=[ds_])
                                    mk.act(s_[:, 3:4], s_[:, 3:4], AF.Sqrt, r=[ds_], w=[ds_])
                                    mk.op("dve", lambda E, s_=s_: E.reciprocal(s_[:, 4:5], s_[:, 3:4]), r=[ds_], w=[ds_])
                                    mk.stt("dve", o_[:, s4, pr * 128:(pr + 1) * 128], ob_[:, s4, :], s_[:, 4:5], gsub,
                                           ALU.mult, ALU.mult, r=[dob_, ds_, d_gsub], w=[do_])
                    for s4 in range(nq4):
                        tq = qa + s4
                        mk.dma("sp", o_d[tq * 128:(tq + 1) * 128, hg * 512:(hg + 1) * 512], o_[:, s4, :],
                               r=[do_], w=[d_od[tq]])
        A.release(m0)
        if _os.environ.get("KSTOP") == "p2":
            return
        m0 = A.mark()
        wo = A.alloc((8, 1024), BF16)
        d_wo = Dep()
        self.load_w_bf16(wo, d_wo, wo_ap.rearrange("(k p) n -> p k n", p=128))
        ybias, d_yb = None, None
        if swa:
            ybias = A.alloc((1024,), F32)
            d_yb = Dep()
            mk.dma("sp", ybias, W["swa_out_b"][0].partition_broadcast(128), w=[d_yb])
        oin = [A.alloc((1024,), BF16) for _ in range(2)]
        d_oin = [Dep() for _ in range(2)]
        oT = [A.alloc((8, 128), BF16) for _ in range(2)]
        d_oT = [Dep() for _ in range(2)]
        for qi_, tt in enumerate(qtiles):
            b = qi_ % 2
            mk.dma("sp", oin[b], o_d[tt * 128:(tt + 1) * 128, :], r=[d_od[tt]], w=[d_oin[b]])
            ps, dps = mk.next_ps()
            psb = ps.bitcast(BF16)
            for k in range(8):
                mk.tr(psb[:, k * 128:(k + 1) * 128], oin[b][:, k * 128:(k + 1) * 128], self.identb,
                      r=[d_oin[b], self.d_identb], w=[dps])
            mk.copy("act", oT[b], psb[:, 0:1024].rearrange("p (k t) -> p k t", k=8), r=[dps], w=[d_oT[b]])
            ys, dys = [], []
            for hf in range(2):
                ps2, dps2 = mk.next_ps()
                for k in range(8):
                    mk.mm(ps2[:, :], oT[b][:, k, :], wo[:, k, hf * 512:(hf + 1) * 512], k == 0, k == 7,
                          r=[d_oT[b], d_wo], w=[dps2])
                ys.append(ps2[:, :])
                dys.append(dps2)
            self.post_tile(tt, ys, dys, 0, self.xres[tt * 128:(tt + 1) * 128, :], self.xdep[tt], ybias=ybias, d_ybias=d_yb)
        self.set_src_xres(qtiles)
        A.release(m0)

    class _Pool:
        def __init__(self, A, shape, dtype, n):
            self.t = [A.alloc(shape, dtype) for _ in range(n)]
            self.d = [Dep() for _ in range(n)]
            self.i = 0

        def get(self):
            k = self.i % len(self.t)
            self.i += 1
            return self.t[k], self.d[k]

    def mixer_delta(self, i):
        mk, A, W, C = self.mk, self.A, self.W, self.C
        nc = self.nc
        P = Prog._Pool
        def _dt(name, shape, dtype):
            if self.test_mode:
                self.dbg_names.append("dbg_" + name)
                return nc.dram_tensor("dbg_" + name, shape, dtype, kind="ExternalOutput").ap()
            return nc.dram_tensor(name, shape, dtype).ap()
        qT_d = _dt("dn_qT", [1024, T], BF16)
        kT_d = _dt("dn_kT", [1024, T], BF16)
        k_d = _dt("dn_k", [T, 1024], BF16)
        v_d = _dt("dn_v", [T, 2048], BF16)
        z_d = _dt("dn_z", [SEQ, 2048], BF16)
        of_d = _dt("dn_of", [SEQ, 2048], F32)
        d_qTd = [Dep() for _ in range(8)]
        d_kTd = [Dep() for _ in range(8)]
        d_kd = [Dep() for _ in range(8)]
        d_vd = [Dep() for _ in range(16)]
        d_zd = [Dep() for _ in range(NTL)]
        d_ofd = [Dep() for _ in range(NTL)]
        mg = A.mark()
        gb = A.alloc((NT, 64), F32)
        d_gb = [Dep() for _ in range(NT)]
        wsrc = W["delta_qkvz_w"][0].rearrange("(k p) n -> p k n", p=128)
        m0 = A.mark()
        hT = A.alloc((8, T), BF16)
        d_hT = [Dep() for _ in range(NT)]
        m1 = A.mark()
        h32 = A.alloc((8, 128), F32)
        d_h32 = Dep()
        wba = A.alloc((8, 64), F32)
        d_wba = Dep()
        mk.dma("sp", wba, W["delta_ba_w"][0].rearrange("(k p) n -> p k n", p=128), w=[d_wba])
        dtb = A.alloc((32,), F32)
        nea = A.alloc((32,), F32)
        d_cst = Dep()
        mk.dma("sp", dtb, W["delta_dt_bias"][0].rearrange("d h -> (d h)").partition_broadcast(128), w=[d_cst])
        mk.dma("sp", nea, W["delta_a_log"][0].rearrange("d h -> (d h)").partition_broadcast(128), w=[d_cst])
        mk.act(nea, nea, AF.Exp, r=[d_cst], w=[d_cst])
        mk.ts("dve", nea, nea, -1.0, None, ALU.mult, r=[d_cst], w=[d_cst])
        v3 = lambda ap: ap.rearrange("p (d h) -> p d h", d=2)
        for tt in range(NT):
            self.prep_tile(tt, 0, hT, d_hT[tt], tt * 128, h32=h32, d_h32=d_h32)
            ps, dps = mk.next_ps()
            for kk in range(8):
                mk.mm(ps[:, 0:64], h32[:, kk, :], wba[:, kk, :], kk == 0, kk == 7, r=[d_h32, d_wba], w=[dps])
            bav = ps[:, 0:64].rearrange("p (d s h) -> p d s h", d=2, s=2)
            gv = v3(gb[:, tt, 0:32])
            mk.tt("dve", gv, bav[:, :, 1, :], v3(dtb), ALU.add, r=[dps, d_cst], w=[d_gb[tt]])
            mk.act(gb[:, tt, 0:32], gb[:, tt, 0:32], AF.Exp, r=[d_gb[tt]], w=[d_gb[tt]])
            mk.act(gb[:, tt, 0:32], gb[:, tt, 0:32], AF.Ln, bias=self.ones[:, 0:1], r=[d_gb[tt], self.d_ones], w=[d_gb[tt]])
            mk.tt("dve", gb[:, tt, 0:32], gb[:, tt, 0:32], nea, ALU.mult, r=[d_cst], w=[d_gb[tt]])
            mk.act(v3(gb[:, tt, 32:64]), bav[:, :, 0, :], AF.Sigmoid, r=[dps], w=[d_gb[tt]])
        self.dbg("dn_gb", gb, d_gb)
        A.release(m1)
        m1 = A.mark()
        wc5 = A.alloc((32, 5), F32)
        d_wc5 = Dep()
        wtmp = A.alloc((32,), F32)
        d_wtmp = Dep()
        for k in range(5):
            self.load_featmajor(wtmp, d_wtmp, W["delta_conv_w"][0, k], 32)
            mk.copy("dve", wc5[:, :, k], wtmp, r=[d_wtmp], w=[d_wc5])
        onesb = A.alloc((128,), BF16)
        d_onesb = Dep()
        mk.memset("dve", onesb, 1.0, w=[d_onesb])
        LP = SEQ + 4 + CTX + 4
        pbuf = A.alloc((LP,), F32)
        d_pbuf = Dep()
        mk.memset("dve", pbuf, 0.0, w=[d_pbuf])
        acc = A.alloc((T,), F32)
        d_acc = Dep()
        sq = A.alloc((T,), BF16)
        d_sq = Dep()
        obp = P(A, (T,), BF16, 2)
        wcp = P(A, (8, 128), BF16, 2)
        rnp = P(A, (512,), F32, 2)
        stp = P(A, (8, 128), BF16, 2)
        tts = [(q * 512, 512, 2 + q * 512) for q in range(8)] + [(SEQ, CTX, SEQ + 6)]
        QS = 128.0 ** -0.5
        for c in range(32):
            wcb, dwc = wcp.get()
            self.load_w_bf16(wcb, dwc, wsrc[:, :, c * 128:(c + 1) * 128])
            for (t0, n, co) in tts:
                ps, dps = mk.next_ps()
                for kk in range(8):
                    mk.mm(ps[:, 0:n], wcb[:, kk, :], hT[:, kk, t0:t0 + n], kk == 0, kk == 7,
                          r=[dwc] + d_hT[t0 // 128:(t0 + n) // 128], w=[dps])
                mk.copy("act", pbuf[:, co:co + n], ps[:, 0:n], r=[dps], w=[d_pbuf])
            for (s0, L, base) in ((0, SEQ, 0), (SEQ, CTX, SEQ + 4)):
                for (a0, a1, eng) in ((0, L, "dve"),):
                    av = acc[:, s0 + a0:s0 + a1]
                    n_ = a1 - a0
                    mk.ts(eng, av, pbuf[:, base + a0:base + a0 + n_], wc5[:, c, 0:1], None, ALU.mult,
                          r=[d_pbuf, d_wc5], w=[d_acc])
                    for k in range(1, 5):
                        mk.stt(eng, av, pbuf[:, base + a0 + k:base + a0 + k + n_], wc5[:, c, k:k + 1], av, ALU.mult, ALU.add,
                               r=[d_pbuf, d_wc5], w=[d_acc])
            ob, dob = obp.get()
            if c < 16:
                mk.act(acc, acc, AF.Silu, r=[d_acc], w=[d_acc])
                mk.act(sq, acc, AF.Square, r=[d_acc], w=[d_sq])
                for (t0, n, co) in tts:
                    ps, dps = mk.next_ps()
                    mk.mm(ps[:, 0:n], onesb, sq[:, t0:t0 + n], True, True, r=[d_onesb, d_sq], w=[dps])
                    rn, drn = rnp.get()
                    mk.ts("dve", rn[:, 0:n], ps[:, 0:n], RMS_EPS, None, ALU.add, r=[dps], w=[drn])
                    mk.act(rn[:, 0:n], rn[:, 0:n], AF.Sqrt, r=[drn], w=[drn])
                    mk.op("dve", lambda E, rn=rn, n=n: E.reciprocal(rn[:, 0:n], rn[:, 0:n]), r=[drn], w=[drn])
                    mk.stt("dve", ob[:, t0:t0 + n], acc[:, t0:t0 + n], QS if c < 8 else 1.0, rn[:, 0:n], ALU.mult, ALU.mult,
                           r=[d_acc, drn], w=[dob])
                if c < 8:
                    mk.dma("sp", qT_d[c * 128:(c + 1) * 128, :], ob, r=[dob], w=[d_qTd[c]])
                else:
                    mk.dma("sp", kT_d[(c - 8) * 128:(c - 7) * 128, :], ob, r=[dob], w=[d_kTd[c - 8]])
            else:
                mk.act(ob, acc, AF.Silu, r=[d_acc], w=[dob])
            if c >= 8:
                dst, ddst, cc = (k_d, d_kd[c - 8], c - 8) if c < 16 else (v_d, d_vd[c - 16], c - 16)
                dview = dst.rearrange("(t p) f -> p t f", p=128)
                for t8 in range(0, NT, 8):
                    nt8 = min(8, NT - t8)
                    ps, dps = mk.next_ps()
                    psb = ps.bitcast(BF16)
                    for q in range(nt8):
                        mk.tr(psb[:, q * 128:(q + 1) * 128], ob[:, (t8 + q) * 128:(t8 + q + 1) * 128], self.identb,
                              r=[dob, self.d_identb], w=[dps])
                    st, dst_ = stp.get()
                    mk.copy("act", st[:, 0:nt8, :], psb[:, 0:nt8 * 128].rearrange("p (t f) -> p t f", f=128), r=[dps], w=[dst_])
                    mk.dma("sp", dview[:, t8:t8 + nt8, cc * 128:(cc + 1) * 128], st[:, 0:nt8, :], r=[dst_], w=[ddst])
        A.release(m1)
        m1 = A.mark()
        wz = A.alloc((8, 2048), BF16)
        d_wz = Dep()
        self.load_w_bf16(wz, d_wz, wsrc[:, :, 4096:6144])
        ngb = A.alloc((4, 128), F32)
        d_ngb = Dep()
        for q in range(4):
            mk.dma("sp", ngb[:, q, :], W["delta_norm_g"][0].partition_broadcast(128), w=[d_ngb])
        zsp = P(A, (512,), F32, 2)
        ztp = P(A, (2048,), BF16, 2)
        for tt in range(NTL):
            zt, dzt = ztp.get()
            for cg in range(4):
                ps, dps = mk.next_ps()
                for kk in range(8):
                    mk.mm(ps[:, :], hT[:, kk, tt * 128:(tt + 1) * 128], wz[:, kk, cg * 512:(cg + 1) * 512], kk == 0, kk == 7,
                          r=[d_wz, d_hT[tt]], w=[dps])
                zs, dzs = zsp.get()
                mk.act(zs, ps[:, :], AF.Silu, r=[dps], w=[dzs])
                mk.tt("dve", zt[:, cg * 512:(cg + 1) * 512], zs, ngb.rearrange("p a b -> p (a b)"), ALU.mult,
                      r=[dzs, d_ngb], w=[dzt])
            mk.dma("sp", z_d[tt * 128:(tt + 1) * 128, :], zt, r=[dzt], w=[d_zd[tt]])
        A.release(m0)
        m0 = A.mark()
        cst = {}
        d_c2 = Dep()
        for nm in ("triF", "triB", "m2F", "m2B", "selF", "selB"):
            cst[nm] = A.alloc((128,), F32)
            mk.dma("sp", cst[nm], C[nm][:, :], w=[d_c2])
        S = A.alloc((16, 128), F32)
        Sb = A.alloc((16, 128), BF16)
        d_S = [Dep() for _ in range(16)]
        wo = A.alloc((16, 1024), BF16)
        d_wo = Dep()
        self.load_w_bf16(wo, d_wo, W["delta_out_w"][0].rearrange("(k p) n -> p k n", p=128))
        ldq = P(A, (8, 128), BF16, 2)
        ldkT = P(A, (8, 128), BF16, 2)
        ldk = P(A, (8, 128), BF16, 2)
        ldv = P(A, (16, 128), BF16, 2)
        smp = P(A, (8, 16), F32, 2)
        kkp = P(A, (128,), F32, 2)
        qkp = P(A, (128,), F32, 2)
        bmp = P(A, (128,), F32, 2)
        dmp = P(A, (128,), F32, 2)
        dtp = P(A, (128,), F32, 2)
        abp = P(A, (128,), F32, 8)
        ttp = P(A, (128,), F32, 3)
        tbp = P(A, (128,), BF16, 2)
        vbp = P(A, (128,), BF16, 2)
        kbp = P(A, (128,), BF16, 2)
        usp = P(A, (128,), F32, 2)
        wtp = P(A, (128,), BF16, 2)
        vnp = P(A, (128,), BF16, 2)
        itp = P(A, (128,), BF16, 2)
        o1p = P(A, (128,), F32, 2)
        kdp = P(A, (128,), BF16, 2)
        otp = P(A, (16, 128), F32, 1)
        ofp = P(A, (16, 128), F32, 1)
        zgp = P(A, (16, 128), BF16, 1)
        onp = P(A, (16, 128), BF16, 1)
        oTp = P(A, (16, 128), BF16, 1)
        s16p = P(A, (4, 16), F32, 2)
        sqt = A.alloc((16, 128), F32)
        d_sqt = Dep()
        kTv = kT_d.rearrange("(h p) t -> p h t", p=128)
        qTv = qT_d.rearrange("(h p) t -> p h t", p=128)
        evi = [0]

        def evac(out, ps, dps, dout):
            evi[0] += 1
            mk.copy("act" if evi[0] % 2 == 0 else "dve", out, ps, r=[dps], w=[dout])

        for d in range(2):
            order = ([NTL, NTL + 1] + list(range(NTL))) if d == 0 else ([NTL + 1, NTL] + list(range(NTL - 1, -1, -1)))
            tri, m2, sel = (cst["triF"], cst["m2F"], cst["selF"]) if d == 0 else (cst["triB"], cst["m2B"], cst["selB"])
            mk.memset("dve", S, 0.0, w=d_S)
            mk.memset("dve", Sb, 0.0, w=d_S)
            for tt in order:
                lat = tt < NTL
                tok = slice(tt * 128, (tt + 1) * 128)
                kTt, dkTt = ldkT.get()
                mk.dma("sp", kTt, kTv[:, :, tok], r=d_kTd, w=[dkTt])
                kt, dkt = ldk.get()
                mk.dma("sp", kt, k_d[tok, :].rearrange("p (h f) -> p h f", f=128), r=d_kd, w=[dkt])
                vt, dvt = ldv.get()
                mk.dma("sp", vt, v_d[tok, :].rearrange("p (h f) -> p h f", f=128), r=d_vd, w=[dvt])
                if lat:
                    qTt, dqTt = ldq.get()
                    mk.dma("sp", qTt, qTv[:, :, tok], r=d_qTd, w=[dqTt])
                g_d = gb[:, tt, d * 16:(d + 1) * 16]
                b_d = gb[:, tt, 32 + d * 16:32 + (d + 1) * 16]
                sm_, dsm = smp.get()
                gc, gl, eg, egl, kds, negb, beg = [sm_[:, q, :] for q in range(7)]
                ps, dps = mk.next_ps()
                mk.mm(ps[:, 0:16], tri, g_d, True, True, r=[d_c2, d_gb[tt]], w=[dps])
                mk.copy("dve", gc, ps[:, 0:16], r=[dps], w=[dsm])
                ps, dps = mk.next_ps()
                mk.mm(ps[:, 0:16], sel, gc, True, True, r=[d_c2, dsm], w=[dps])
                mk.copy("dve", gl, ps[:, 0:16], r=[dps], w=[dsm])
                mk.act(eg, gc, AF.Exp, r=[dsm], w=[dsm])
                mk.act(egl, gl, AF.Exp, r=[dsm], w=[dsm])
                mk.tt("dve", kds, gl, gc, ALU.subtract, r=[dsm], w=[dsm])
                mk.act(kds, kds, AF.Exp, r=[dsm], w=[dsm])
                mk.ts("dve", negb, b_d, -1.0, None, ALU.mult, r=[d_gb[tt]], w=[dsm])
                mk.tt("dve", beg, b_d, eg, ALU.mult, r=[d_gb[tt], dsm], w=[dsm])
                if lat:
                    ot, dot = otp.get()
                    if d == 1:
                        oft, doft = ofp.get()
                        mk.dma("sp", oft, of_d[tok, :].rearrange("p (h f) -> p h f", f=128), r=[d_ofd[tt]], w=[doft])
                for hq in range(8):
                    ps, dps = mk.next_ps()
                    mk.mm(ps[:, 0:128], kTt[:, hq, :], kTt[:, hq, :], True, True, r=[dkTt], w=[dps])
                    kkm, dkkm = kkp.get()
                    mk.tt("dve", kkm, ps[:, 0:128], m2, ALU.mult, r=[dps, d_c2], w=[dkkm])
                    if lat:
                        ps, dps = mk.next_ps()
                        mk.mm(ps[:, 0:128], kTt[:, hq, :], qTt[:, hq, :], True, True, r=[dkTt, dqTt], w=[dps])
                        qkm, dqkm = qkp.get()
                        mk.tt("dve", qkm, ps[:, 0:128], tri, ALU.mult, r=[dps, d_c2], w=[dqkm])
                    for hv in (2 * hq, 2 * hq + 1):
                        bm, dbm = bmp.get()
                        mk.ts("dve", bm, m2, g_d[:, hv:hv + 1], None, ALU.mult, r=[d_c2, d_gb[tt]], w=[dbm])
                        ps, dps = mk.next_ps()
                        mk.mm(ps[:, 0:128], tri, bm, True, True, r=[d_c2, dbm], w=[dps])
                        dm, ddm = dmp.get()
                        mk.act(dm, ps[:, 0:128], AF.Exp, r=[dps], w=[ddm])
                        a_k, da_k = abp.get()
                        mk.stt("dve", a_k, kkm, negb[:, hv:hv + 1], dm, ALU.mult, ALU.mult, r=[dkkm, dsm, ddm], w=[da_k])
                        ps, dps = mk.next_ps()
                        mk.tr(ps[:, 0:128], a_k, self.ident, r=[da_k, self.d_ident], w=[dps])
                        b_k, db_k = abp.get()
                        evac(b_k, ps[:, 0:128], dps, db_k)
                        tT, dtT = ttp.get()
                        mk.tt("dve", tT, b_k, self.ident, ALU.add, r=[db_k, self.d_ident], w=[dtT])
                        for lvl in range(1, 7):
                            ps, dps = mk.next_ps()
                            mk.mm(ps[:, 0:128], b_k, a_k, True, True, r=[db_k, da_k], w=[dps])
                            a_n, da_n = abp.get()
                            evac(a_n, ps[:, 0:128], dps, da_n)
                            if lvl < 6:
                                ps, dps = mk.next_ps()
                                mk.mm(ps[:, 0:128], a_k, b_k, True, True, r=[db_k, da_k], w=[dps])
                                b_n, db_n = abp.get()
                                evac(b_n, ps[:, 0:128], dps, db_n)
                            ps, dps = mk.next_ps()
                            mk.mm(ps[:, 0:128], a_n, tT, True, True, r=[da_n, dtT], w=[dps])
                            tN, dtN = ttp.get()
                            mk.tt("dve", tN, ps[:, 0:128], tT, ALU.add, r=[dps, dtT], w=[dtN])
                            tT, dtT = tN, dtN
                            a_k, da_k = a_n, da_n
                            if lvl < 6:
                                b_k, db_k = b_n, db_n
                        tTf, dtTf = tT, dtT
                        tT, dtT = tbp.get()
                        mk.copy("act", tT, tTf, r=[dtTf], w=[dtT])
                        vb, dvb = vbp.get()
                        mk.ts("dve", vb, vt[:, hv, :], b_d[:, hv:hv + 1], None, ALU.mult, r=[dvt, d_gb[tt]], w=[dvb])
                        kb, dkb = kbp.get()
                        mk.ts("dve", kb, kt[:, hq, :], beg[:, hv:hv + 1], None, ALU.mult, r=[dkt, dsm], w=[dkb])
                        ps, dps = mk.next_ps()
                        mk.mm(ps[:, 0:128], tT, vb, True, True, r=[dtT, dvb], w=[dps])
                        us, dus = usp.get()
                        evac(us, ps[:, 0:128], dps, dus)
                        ps, dps = mk.next_ps()
                        mk.mm(ps[:, 0:128], kb, tT, True, True, r=[dkb, dtT], w=[dps])
                        wT, dwT = wtp.get()
                        evac(wT, ps[:, 0:128], dps, dwT)
                        ps, dps = mk.next_ps()
                        mk.mm(ps[:, 0:128], wT, Sb[:, hv, :], True, True, r=[dwT, d_S[hv]], w=[dps])
                        vn, dvn = vnp.get()
                        mk.tt("dve", vn, us, ps[:, 0:128], ALU.subtract, r=[dus, dps], w=[dvn])
                        if lat:
                            ps, dps = mk.next_ps()
                            mk.mm(ps[:, 0:128], bm, tri, True, True, r=[d_c2, dbm], w=[dps])
                            dT, ddT = dtp.get()
                            mk.act(dT, ps[:, 0:128], AF.Exp, r=[dps], w=[ddT])
                            it, dit = itp.get()
                            mk.tt("pool", it, qkm, dT, ALU.mult, r=[dqkm, ddT], w=[dit])
                            ps, dps = mk.next_ps()
                            mk.mm(ps[:, 0:128], qTt[:, hq, :], Sb[:, hv, :], True, True, r=[dqTt, d_S[hv]], w=[dps])
                            o1, do1 = o1p.get()
                            mk.ts("dve", o1, ps[:, 0:128], eg[:, hv:hv + 1], None, ALU.mult, r=[dps, dsm], w=[do1])
                            ps, dps = mk.next_ps()
                            mk.mm(ps[:, 0:128], it, vn, True, True, r=[dit, dvn], w=[dps])
                            if d == 0:
                                mk.tt("dve", ot[:, hv, :], ps[:, 0:128], o1, ALU.add, r=[dps, do1], w=[dot])
                            else:
                                mk.tt("dve", o1, ps[:, 0:128], o1, ALU.add, r=[dps], w=[do1])
                                mk.tt("pool", ot[:, hv, :], o1, oft[:, hv, :], ALU.add, r=[do1, doft], w=[dot])
                        kd, dkd = kdp.get()
                        mk.ts("dve", kd, kt[:, hq, :], kds[:, hv:hv + 1], None, ALU.mult, r=[dkt, dsm], w=[dkd])
                        ps, dps = mk.next_ps()
                        mk.mm(ps[:, 0:128], kd, vn, True, True, r=[dkd, dvn], w=[dps])
                        mk.stt("dve", S[:, hv, :], S[:, hv, :], egl[:, hv:hv + 1], ps[:, 0:128], ALU.mult, ALU.add,
                               r=[dps, dsm], w=[d_S[hv]])
                        mk.copy("act", Sb[:, hv, :], S[:, hv, :], r=[], w=[d_S[hv]])
                if lat and d == 0:
                    mk.dma("sp", of_d[tok, :].rearrange("p (h f) -> p h f", f=128), ot, r=[dot], w=[d_ofd[tt]])
                if lat and d == 1:
                    zg, dzg = zgp.get()
                    mk.dma("sp", zg, z_d[tok, :].rearrange("p (h f) -> p h f", f=128), r=[d_zd[tt]], w=[dzg])
                    s16, ds16 = s16p.get()
                    mk.tt("pool", sqt, ot, ot, ALU.mult, r=[dot], w=[d_sqt])
                    mk.op("dve", lambda E, s16=s16: E.reduce_sum(s16[:, 0, :], sqt, mybir.AxisListType.X), r=[d_sqt], w=[ds16])
                    mk.ts("dve", s16[:, 1, :], s16[:, 0, :], 1.0 / 128.0, RMS_EPS, ALU.mult, ALU.add, r=[ds16], w=[ds16])
                    mk.act(s16[:, 1, :], s16[:, 1, :], AF.Sqrt, r=[ds16], w=[ds16])
                    mk.op("dve", lambda E, s16=s16: E.reciprocal(s16[:, 2, :], s16[:, 1, :]), r=[ds16], w=[ds16])
                    on, don = onp.get()
                    for hv in range(16):
                        mk.stt("dve", on[:, hv, :], ot[:, hv, :], s16[:, 2, hv:hv + 1], zg[:, hv, :],
                               ALU.mult, ALU.mult, r=[dot, ds16, dzg], w=[don])
                    oT, doT = oTp.get()
                    for hf in range(2):
                        ps, dps = mk.next_ps()
                        psb = ps.bitcast(BF16)
                        for q in range(8):
                            mk.tr(psb[:, q * 128:(q + 1) * 128], on[:, hf * 8 + q, :], self.identb,
                                  r=[don, self.d_identb], w=[dps])
                        mk.copy("act", oT[:, hf * 8:(hf + 1) * 8, :], psb[:, 0:1024].rearrange("p (k t) -> p k t", k=8),
                                r=[dps], w=[doT])
                    ys, dys = [], []
                    for hf in range(2):
                        ps2, dps2 = mk.next_ps()
                        for k in range(16):
                            mk.mm(ps2[:, :], oT[:, k, :], wo[:, k, hf * 512:(hf + 1) * 512], k == 0, k == 15,
                                  r=[doT, d_wo], w=[dps2])
                        ys.append(ps2[:, :])
                        dys.append(dps2)
                    self.post_tile(tt, ys, dys, 0, self.xres[tt * 128:(tt + 1) * 128, :], self.xdep[tt])
        self.set_src_xres(range(NTL))
        A.release(mg)

    def moe(self, i, last, final):
        mk, A, W = self.mk, self.A, self.W
        ntile = NTL if last else NT
        GT = 9
        groups = [(0, 9), (9, 18), (18, 26), (26, 34)] if not last else [(0, 8), (8, 16), (16, 24), (24, 32)]
        m0 = A.mark()
        wr = A.alloc((8, 32), F32)
        d_wr = Dep()
        mk.dma("sp", wr, W["router_w"][i].rearrange("(k p) n -> p k n", p=128), w=[d_wr])
        brbc = A.alloc((32,), F32)
        d_br = Dep()
        mk.dma("sp", brbc, W["router_b"][i].partition_broadcast(128), w=[d_br])
        b2 = A.alloc((1024,), F32, parts=32)
        d_b2 = Dep()
        mk.dma("sp", b2, W["exp_b2"][i], w=[d_b2])
        b1F = A.alloc((NE, 16), F32)
        d_b1F = Dep()
        for e in range(NE):
            self.load_featmajor(b1F[:, e, :], d_b1F, W["exp_b1"][i, e], 16)
        w1b = A.alloc((8, 2048), BF16)
        d_w1 = Dep()
        w2b = A.alloc((8, 1024), BF16)
        d_w2 = Dep()
        hT = A.alloc((8, GT * 128), BF16)
        acc = A.alloc((GT, 1024), F32)
        gw = A.alloc((GT, 32), F32)
        actT = A.alloc((8, GT * 128), BF16)
        h32 = A.alloc((8, 128), F32)
        d_h32 = Dep()
        lg = A.alloc((32,), F32)
        top8 = A.alloc((8,), F32)
        sm = A.alloc((4,), F32)
        ex = A.alloc((32,), F32)
        d_lg = Dep()
        gwT = A.alloc((128,), F32, parts=32)
        d_gwT = Dep()
        NTB = 3
        tg = [A.alloc((512,), F32) for _ in range(NTB)]
        tsg = [A.alloc((512,), F32) for _ in range(NTB)]
        tu = [A.alloc((512,), F32) for _ in range(NTB)]
        d_tg = [Dep() for _ in range(NTB)]
        d_tsg = [Dep() for _ in range(NTB)]
        d_tu = [Dep() for _ in range(NTB)]
        mk.ts("dve", b1F[:, :, 8:16], b1F[:, :, 8:16], 1.0, None, ALU.add, r=[d_b1F], w=[d_b1F])
        w1src = W["exp_w1"][i]
        w2src = W["exp_w2"][i]
        ti = 0
        for (ga, gb) in groups:
            ng = gb - ga
            d_hT = [Dep() for _ in range(ng)]
            d_acc = [Dep() for _ in range(ng)]
            d_gw = [Dep() for _ in range(ng)]
            d_actT = [Dep() for _ in range(ng)]
            for lt in range(ng):
                tt = ga + lt
                self.prep_tile(tt, 3, hT, d_hT[lt], lt * 128, h32=h32, d_h32=d_h32)
                ps, dps = mk.next_ps()
                for kk in range(8):
                    mk.mm(ps[:, 0:32], h32[:, kk, :], wr[:, kk, :], kk == 0, kk == 7, r=[d_h32, d_wr], w=[dps])
                mk.tt("dve", lg, ps[:, 0:32], brbc, ALU.add, r=[dps, d_br], w=[d_lg])
                mk.op("dve", lambda E: E.max(top8, lg), r=[], w=[d_lg])
                mk.ts("dve", sm[:, 0:1], top8[:, 0:1], -1.0, None, ALU.mult, r=[], w=[d_lg])
                mk.act(ex, lg, AF.Exp, bias=sm[:, 0:1], scale=1.0, r=[d_lg], w=[d_lg])
                mk.stt("dve", ex, lg, top8[:, 3:4], ex, ALU.is_ge, ALU.mult, r=[d_lg], w=[d_lg])
                mk.op("dve", lambda E: E.reduce_sum(sm[:, 1:2], ex, mybir.AxisListType.X), r=[], w=[d_lg])
                mk.op("dve", lambda E: E.reciprocal(sm[:, 2:3], sm[:, 1:2]), r=[], w=[d_lg])
                g_ap = gw[:, lt, :]
                mk.ts("dve", g_ap, ex, sm[:, 2:3], None, ALU.mult, r=[d_lg], w=[d_gw[lt]])
                ps, dps = mk.next_ps()
                mk.tr(ps[0:32, 0:128], g_ap, self.ident, r=[d_gw[lt], self.d_ident], w=[dps])
                mk.copy("dve", gwT, ps[0:32, 0:128], r=[dps], w=[d_gwT])
                for hf in range(2):
                    ps, dps = mk.next_ps()
                    mk.mm(ps[:, :], gwT, b2[:, hf * 512:(hf + 1) * 512], True, True, r=[d_gwT, d_b2], w=[dps])
                    mk.copy("act", acc[:, lt, hf * 512:(hf + 1) * 512], ps[:, :], r=[dps], w=[d_acc[lt]])
            t5 = [(a, min(4, ng - a)) for a in range(0, ng, 4)]
            for e in range(NE):
                self.load_w_bf16(w1b, d_w1, w1src[e].rearrange("(k p) n -> p k n", p=128))
                self.load_w_bf16(w2b, d_w2, w2src[e].rearrange("(k p) n -> p k n", p=128))
                for (a, nt4) in t5:
                    n = nt4 * 128
                    c0 = a * 128
                    rd = d_hT[a:a + nt4]
                    for j in range(8):
                        psg, dpsg = mk.next_ps()
                        for kk in range(8):
                            mk.mm(psg[:, 0:n], w1b[:, kk, j * 128:(j + 1) * 128], hT[:, kk, c0:c0 + n], kk == 0, kk == 7,
                                  r=[d_w1] + rd, w=[dpsg])
                        psu, dpsu = mk.next_ps()
                        for kk in range(8):
                            mk.mm(psu[:, 0:n], w1b[:, kk, 1024 + j * 128:1024 + (j + 1) * 128], hT[:, kk, c0:c0 + n],
                                  kk == 0, kk == 7, r=[d_w1] + rd, w=[dpsu])
                        tb = ti % NTB
                        ti += 1
                        g_, s_, u_ = tg[tb], tsg[tb], tu[tb]
                        dg_, ds_, du_ = d_tg[tb], d_tsg[tb], d_tu[tb]
                        mk.ts("dve", g_[:, 0:n], psg[:, 0:n], b1F[:, e, j:j + 1], 7.0, ALU.add, ALU.min,
                              r=[dpsg, d_b1F], w=[dg_])
                        mk.act(s_[:, 0:n], g_[:, 0:n], AF.Sigmoid, scale=1.702, r=[dg_], w=[ds_])
                        mk.ts("dve", u_[:, 0:n], psu[:, 0:n], b1F[:, e, 8 + j:9 + j], -6.0, ALU.add, ALU.max,
                              r=[dpsu, d_b1F], w=[du_])
                        mk.tt("pool", s_[:, 0:n], g_[:, 0:n], s_[:, 0:n], ALU.mult, r=[dg_, ds_], w=[ds_])
                        mk.stt("dve", actT[:, j, c0:c0 + n], u_[:, 0:n], 8.0, s_[:, 0:n], ALU.min, ALU.mult,
                               r=[du_, ds_], w=d_actT[a:a + nt4])
                for lt in range(ng):
                    for hf in range(2):
                        ps, dps = mk.next_ps()
                        for j in range(8):
                            mk.mm(ps[:, :], actT[:, j, lt * 128:(lt + 1) * 128], w2b[:, j, hf * 512:(hf + 1) * 512],
                                  j == 0, j == 7, r=[d_actT[lt], d_w2], w=[dps])
                        av = acc[:, lt, hf * 512:(hf + 1) * 512]
                        mk.stt("dve", av, ps[:, :], gw[:, lt, e:e + 1], av, ALU.mult, ALU.add,
                               r=[dps, d_gw[lt]], w=[d_acc[lt]])
            for lt in range(ng):
                tt = ga + lt
                if final:
                    dst, ddst = self.out_d[tt * 128:(tt + 1) * 128, :], self.outdep[tt]
                else:
                    dst, ddst = self.xres[tt * 128:(tt + 1) * 128, :], self.xdep[tt]
                self.post_tile(tt, [acc[:, lt, 0:512], acc[:, lt, 512:1024]], [d_acc[lt]], 1, dst, ddst)
            if not final:
                self.set_src_xres(range(ga, gb))
        A.release(m0)

    def build(self):
        for li, i in enumerate(self.layers):
            last = (i == DEPTH - 1)
            final = (li == len(self.layers) - 1)
            self.phase_mod(i)
            kind = i % 4
            if kind == 0:
                self.mixer_conv(i, not last)
            elif kind in (1, 2):
                self.mixer_attn(i, kind, not last)
            else:
                self.mixer_delta(i)
            if self.do_moe:
                self.moe(i, last, final)
            else:
                self.copy_xres_to_out()
        self.mk.finish()

    def copy_xres_to_out(self):
        for tt in range(NT):
            b = self.xt_i % 2
            self.xt_i += 1
            xt, dxt = self.xt[b], self.d_xt[b]
            self.mk.dma("sp", xt, self.src[tt], r=[self.xdep[tt]], w=[dxt])
            self.mk.dma("sp", self.out_d[tt * 128:(tt + 1) * 128, :], xt, r=[dxt], w=[self.outdep[tt]])


def build_program(layers=(0, 1, 2, 3), test_mode=False, do_moe=True):
    nc = bass.Bass("TRN2", target_bir_lowering=False)
    p = Prog(nc, layers, test_mode=test_mode, do_moe=do_moe)
    p.build()
    return nc, p


def kernel(**inputs):
    nc, p = build_program()
    consts = host_constants()
    in_maps = []
    for b in range(8):
        m = {"x": np.ascontiguousarray(inputs["x"][b]), "ctx": np.ascontiguousarray(inputs["ctx"][b]),
             "c": np.ascontiguousarray(inputs["c"][b]), "c_ctx": np.ascontiguousarray(inputs["c_ctx"])}
        for name, _ in W_SPECS:
            m[name] = np.ascontiguousarray(inputs[name])
        m.update(consts)
        in_maps.append(m)
    res = run_bass_kernel_spmd(nc, in_maps, core_ids=list(range(8)))
    return np.stack([np.asarray(r["out"]) for r in res.results], axis=0).astype(np.float32)
```
